# Optimizing a Trainium2 kernel written in Bass

```python
import math
import jax, jax.numpy as jnp
from jax import lax
import numpy as np

D_MODEL = 2048
BATCH = 2
SEQ = 16384
DEPTH = 1

CHUNK = 64
CONV_WIDTH = D_MODEL // 2
CONV_K = 3
SGU_WIDTH = D_MODEL // 2
SGU_BLOCK = 128
SGU_GROUP_CH = 128
N_SGU_GROUPS = SGU_WIDTH // SGU_GROUP_CH
IN_COLS = 3 * CONV_WIDTH + 2 * SGU_WIDTH + 2 * D_MODEL
N_EXPERTS = 32
TOP_K = 4
D_FF = D_MODEL
SWIGLU_ALPHA = 1.702
SWIGLU_LIMIT = 7.0
MOE_BLOCK = 512
LN_EPS = 1e-5
DEEPNORM_ALPHA = (2.0 * DEPTH) ** 0.25
DEEPNORM_BETA = (8.0 * DEPTH) ** -0.25

kernel_name = "hybrid_shortconv_sgu_moe_deepnorm"


def layer_norm(x, g, b):
    xf = x.astype(jnp.float32)
    mu = jnp.mean(xf, axis=-1, keepdims=True)
    var = jnp.mean(jnp.square(xf - mu), axis=-1, keepdims=True)
    y = (xf - mu) * lax.rsqrt(var + LN_EPS) * g.astype(jnp.float32) + b.astype(jnp.float32)
    return y.astype(x.dtype)


def short_conv_mixer(pre_gate, hidden, post_gate, conv_w, w_out):
    S = hidden.shape[1]
    z = pre_gate * hidden
    zp = jnp.pad(z, ((0, 0), (CONV_K - 1, 0), (0, 0)))
    conv = sum(conv_w[k] * zp[:, k:k + S] for k in range(CONV_K))
    return (post_gate * conv) @ w_out


def spatial_gating_mixer(zb, ln_g, ln_b, w_s, b_s, w_out):
    Bsz, S, _ = zb.shape
    z = jax.nn.gelu(zb, approximate=False)
    u, v = jnp.split(z, 2, axis=-1)
    v = layer_norm(v, ln_g, ln_b)
    v = v.reshape(Bsz, S // SGU_BLOCK, SGU_BLOCK, N_SGU_GROUPS, SGU_GROUP_CH)
    pos = jnp.arange(SGU_BLOCK)
    mask = (pos[None, :] // CHUNK) <= (pos[:, None] // CHUNK)
    w = jnp.where(mask[None], w_s, jnp.zeros_like(w_s))
    mixed = jnp.einsum('gpq,bnqgc->bnpgc', w, v) + b_s.T[None, None, :, :, None]
    mixed = mixed.reshape(Bsz, S, SGU_WIDTH)
    return (u * mixed) @ w_out


def clamped_swiglu(a):
    glu, lin = jnp.split(a, 2, axis=-1)
    glu = jnp.minimum(glu, SWIGLU_LIMIT)
    lin = jnp.clip(lin, -SWIGLU_LIMIT, SWIGLU_LIMIT)
    return glu * jax.nn.sigmoid(SWIGLU_ALPHA * glu) * (lin + 1.0)


def moe(h, w_router, b_router, w_up, b_up, w_down, b_down):
    Bsz, S, D = h.shape
    T = Bsz * S
    hf = h.reshape(T, D)
    logits = (hf @ w_router + b_router).astype(jnp.float32)
    top_vals, top_idx = lax.top_k(logits, TOP_K)
    gates = jax.nn.softmax(top_vals, axis=-1)
    n_assign = T * TOP_K
    e_flat = top_idx.reshape(-1)
    tok_flat = jnp.arange(n_assign, dtype=jnp.int32) // TOP_K
    g_flat = gates.reshape(-1)
    order = jnp.argsort(e_flat)
    e_sorted = e_flat[order]
    counts = jnp.bincount(e_flat, length=N_EXPERTS)
    padded = ((counts + MOE_BLOCK - 1) // MOE_BLOCK) * MOE_BLOCK
    starts = jnp.cumsum(counts) - counts
    pends = jnp.cumsum(padded)
    pstarts = pends - padded
    rank = jnp.arange(n_assign, dtype=jnp.int32) - starts[e_sorted]
    dest = pstarts[e_sorted] + rank
    n_blocks = -(-n_assign // MOE_BLOCK) + N_EXPERTS
    n_rows = n_blocks * MOE_BLOCK
    row_tok = jnp.full((n_rows,), T, dtype=jnp.int32).at[dest].set(tok_flat[order])
    row_gate = jnp.zeros((n_rows,), h.dtype).at[dest].set(g_flat[order].astype(h.dtype))
    block_start = jnp.arange(n_blocks, dtype=jnp.int32) * MOE_BLOCK
    block_expert = jnp.minimum(jnp.searchsorted(pends, block_start, side='right'), N_EXPERTS - 1)
    h_pad = jnp.concatenate([hf, jnp.zeros((1, D), hf.dtype)], axis=0)

    def body(y, blk):
        toks, gts, e = blk
        xb = h_pad[toks]
        a = xb @ w_up[e] + b_up[e]
        o = clamped_swiglu(a) @ w_down[e] + b_down[e]
        return y.at[toks].add(o * gts[:, None]), None

    y0 = jnp.zeros((T + 1, D), h.dtype)
    y, _ = lax.scan(body, y0, (row_tok.reshape(n_blocks, MOE_BLOCK),
                               row_gate.reshape(n_blocks, MOE_BLOCK),
                               block_expert))
    return y[:T].reshape(Bsz, S, D)


def setup_inputs(seed: int = 0) -> dict:
    key = jax.random.key(seed)
    ks = jax.random.split(key, 24)
    f32 = jnp.float32
    L = DEPTH

    def nrm(k, shape, scale):
        return jax.random.normal(k, shape, f32) * scale

    return {
        "x": nrm(ks[0], (BATCH, SEQ, D_MODEL), 1.0),
        "w_in": nrm(ks[1], (L, D_MODEL, IN_COLS), D_MODEL ** -0.5),
        "conv_w": nrm(ks[2], (L, CONV_K, CONV_WIDTH), CONV_K ** -0.5),
        "w_a_out": nrm(ks[3], (L, CONV_WIDTH, D_MODEL), CONV_WIDTH ** -0.5 * DEEPNORM_BETA),
        "ln_v_g": 1.0 + nrm(ks[4], (L, SGU_WIDTH), 0.01),
        "ln_v_b": nrm(ks[5], (L, SGU_WIDTH), 0.01),
        "w_s": nrm(ks[6], (L, N_SGU_GROUPS, SGU_BLOCK, SGU_BLOCK), SGU_BLOCK ** -0.5),
        "b_s": 1.0 + nrm(ks[7], (L, N_SGU_GROUPS, SGU_BLOCK), 0.01),
        "w_b_out": nrm(ks[8], (L, SGU_WIDTH, D_MODEL), SGU_WIDTH ** -0.5 * DEEPNORM_BETA),
        "b_gate": nrm(ks[9], (L, 2 * D_MODEL), 0.01),
        "w_o": nrm(ks[10], (L, D_MODEL, D_MODEL), D_MODEL ** -0.5 * DEEPNORM_BETA),
        "ln1_g": 1.0 + nrm(ks[11], (L, D_MODEL), 0.01),
        "ln1_b": nrm(ks[12], (L, D_MODEL), 0.01),
        "w_router": nrm(ks[13], (L, D_MODEL, N_EXPERTS), D_MODEL ** -0.5),
        "b_router": nrm(ks[14], (L, N_EXPERTS), 0.01),
        "w_up": nrm(ks[15], (L, N_EXPERTS, D_MODEL, 2 * D_FF), D_MODEL ** -0.5 * DEEPNORM_BETA),
        "b_up": nrm(ks[16], (L, N_EXPERTS, 2 * D_FF), 0.01),
        "w_down": nrm(ks[17], (L, N_EXPERTS, D_FF, D_MODEL), D_FF ** -0.5 * DEEPNORM_BETA),
        "b_down": nrm(ks[18], (L, N_EXPERTS, D_MODEL), 0.01),
        "ln2_g": 1.0 + nrm(ks[19], (L, D_MODEL), 0.01),
        "ln2_b": nrm(ks[20], (L, D_MODEL), 0.01),
    }


def reference(x, w_in, conv_w, w_a_out, ln_v_g, ln_v_b, w_s, b_s, w_b_out, b_gate,
              w_o, ln1_g, ln1_b, w_router, b_router, w_up, b_up, w_down, b_down,
              ln2_g, ln2_b):
    c0 = CONV_WIDTH
    c1 = 2 * CONV_WIDTH
    c2 = 3 * CONV_WIDTH
    c3 = c2 + 2 * SGU_WIDTH
    for l in range(DEPTH):
        p = x @ w_in[l]
        pre_gate, hidden, post_gate = p[..., :c0], p[..., c0:c1], p[..., c1:c2]
        zb = p[..., c2:c3]
        g = jax.nn.sigmoid(p[..., c3:] + b_gate[l])
        g_a, g_b = g[..., :D_MODEL], g[..., D_MODEL:]
        y_a = short_conv_mixer(pre_gate, hidden, post_gate, conv_w[l], w_a_out[l])
        y_b = spatial_gating_mixer(zb, ln_v_g[l], ln_v_b[l], w_s[l], b_s[l], w_b_out[l])
        mix = (g_a * y_a + g_b * y_b) @ w_o[l]
        x = layer_norm(DEEPNORM_ALPHA * x + mix, ln1_g[l], ln1_b[l])
        f = moe(x, w_router[l], b_router[l], w_up[l], b_up[l], w_down[l], b_down[l])
        x = layer_norm(DEEPNORM_ALPHA * x + f, ln2_g[l], ln2_b[l])
    return x
```

```python
from contextlib import ExitStack
import numpy as np
import concourse.bass as bass
import concourse.mybir as mybir
from concourse.bass_utils import run_bass_kernel_spmd

F32 = mybir.dt.float32
BF16 = mybir.dt.bfloat16
I32 = mybir.dt.int32
AF = mybir.ActivationFunctionType
ALU = mybir.AluOpType

D = 2048
KD = D // 128
CW = 1024
E = 32
TOPK = 4
LN_EPS = 1e-5
ALPHA = (2.0 * 1) ** 0.25
SW_ALPHA = 1.702
SW_LIM = 7.0
IN_COLS = 3 * CW + 2 * CW + 2 * D


class Tracker:
    def __init__(self, nc, stack):
        self.nc = nc
        self.stack = stack
        self.eng = {"pe": nc.tensor, "act": nc.scalar, "dve": nc.vector, "pool": nc.gpsimd, "sp": nc.sync}
        self.sem = {}
        self.cnt = {}
        for e in ("pe", "act", "dve", "pool"):
            self.sem[e] = stack.enter_context(nc.semaphore("sem_" + e))
            self.cnt[e] = 0
        self.waited = {e: {} for e in self.eng}
        self.lastw = {}
        self.readers = {}
        self.chan = {}

    def _deps(self, reads, writes, order_writes=True):
        deps = []
        for r in reads:
            deps.extend(self.lastw.get(r, []))
        for w in writes:
            if order_writes:
                deps.extend(self.lastw.get(w, []))
            deps.extend(self.readers.get(w, []))
        return deps

    def _wait(self, e, deps):
        best = {}
        for (key, sem, val) in deps:
            if key not in best or best[key][1] < val:
                best[key] = (sem, val)
        for key, (sem, val) in best.items():
            if self.waited[e].get(key, 0) < val:
                self.eng[e].wait_ge(sem, val)
                self.waited[e][key] = val

    def _record(self, tok, reads, writes, order_writes=True):
        for r in reads:
            self.readers.setdefault(r, []).append(tok)
        for w in writes:
            if order_writes:
                self.lastw[w] = [tok]
                self.readers[w] = []
            else:
                self.lastw.setdefault(w, []).append(tok)

    def op(self, e, fn, reads=(), writes=()):
        self._wait(e, self._deps(reads, writes))
        ins = fn(self.eng[e])
        self.cnt[e] += 1
        ins.then_inc(self.sem[e], 1)
        tok = (e, self.sem[e], self.cnt[e])
        self._record(tok, reads, writes)
        return tok

    def mm_group(self, mms, reads_list, writes):
        tok = ("pe", self.sem["pe"], self.cnt["pe"] + 1)
        n = len(mms)
        for i, fn in enumerate(mms):
            self._wait("pe", self._deps(reads_list[i], writes if i == 0 else ()))
            ins = fn(self.eng["pe"])
            if i == n - 1:
                ins.then_inc(self.sem["pe"], 1)
        self.cnt["pe"] += 1
        allreads = set()
        for r in reads_list:
            allreads.update(r)
        self._record(tok, list(allreads), writes)
        return tok

    def dma(self, q, chan, fn, reads=(), writes=(), order_writes=True):
        if chan not in self.chan:
            self.chan[chan] = [self.stack.enter_context(self.nc.semaphore("ds_" + chan)), 0]
        self._wait(q, self._deps(reads, writes, order_writes))
        ins = fn(self.eng[q])
        c = self.chan[chan]
        c[1] += 16
        ins.then_inc(c[0], 16)
        tok = ("d_" + chan, c[0], c[1])
        self._record(tok, reads, writes, order_writes)
        return tok

    def barrier(self):
        toks = [(e, self.sem[e], self.cnt[e]) for e in self.sem if self.cnt[e] > 0]
        toks += [("d_" + c, sv[0], sv[1]) for c, sv in self.chan.items() if sv[1] > 0]
        for e in self.eng:
            self._wait(e, toks)

    def wait_all(self, e, resources):
        deps = []
        for r in resources:
            deps.extend(self.lastw.get(r, []))
            deps.extend(self.readers.get(r, []))
        self._wait(e, deps)


def build_nc(NT, F, CAP):
    NTT = NT // 512
    NTB = NT // 128
    KF = F // 128
    NB = CAP // 128
    NSLOT = E * CAP
    colr = [(0, min(CAP, 512))] + ([(512, CAP)] if CAP > 512 else [])

    nc = bass.Bass("TRN2", target_bir_lowering=False)

    def din(name, shape, dt=F32):
        return nc.dram_tensor(name, list(shape), dt, kind="ExternalInput").ap()

    xs = din("xs", [NT, D])
    xhT = din("xhT", [128, KD, 2])
    w_in = din("w_in", [D, IN_COLS])
    cw_d = din("cw", [128, 8, 3])
    w_a = din("w_a", [CW, D])
    w_b = din("w_b", [CW, D])
    lnv_d = din("lnv", [128, 2, CW])
    wsT_d = din("wsT", [128, 8, 128])
    bsb_d = din("bsb", [128, 8, 128])
    bg_d = din("bg", [128, 32])
    w_o = din("w_o", [D, D])
    ln1_d = din("ln1", [128, 2, D])
    ln2_d = din("ln2", [128, 2, D])
    wr_d = din("wr", [128, KD, E])
    brb_d = din("brb", [128, E])
    w_up = din("w_up", [E * D, 2 * F])
    bup_d = din("bup", [128, E, 2 * KF])
    w_dn = din("w_dn", [E * F, D])
    b_dn = din("b_dn", [E, D])
    cst_d = din("cst", [128, 128 + 128 + E])
    out = nc.dram_tensor("out", [NT, D], F32, kind="ExternalOutput").ap()

    MT = nc.dram_tensor("MT", [NTT, 128, KD * 512], BF16, kind="Internal").ap()
    X1 = nc.dram_tensor("X1", [NT + 1, D + E], F32, kind="Internal").ap()
    TT = nc.dram_tensor("TT", [NSLOT + 1, 2], I32, kind="Internal").ap()
    OD = nc.dram_tensor("OD", [NSLOT + 1, D], F32, kind="Internal").ap()

    with ExitStack() as top:
        T = Tracker(nc, top)
        sb = lambda st, name, shape, dt=F32: st.enter_context(nc.sbuf_tensor("s_" + name, list(shape), dt))
        ps_ = lambda st, name, shape, dt=F32: st.enter_context(nc.psum_tensor("p_" + name, list(shape), dt))

        cst = sb(top, "cst", [128, 128 + 128 + E])
        idb = sb(top, "idb", [128, 128], BF16)
        ones = sb(top, "ones", [128, 128])
        epsT = sb(top, "epsT", [128, 1])
        slots_all = sb(top, "slots_all", [128, NTB, TOPK], I32)
        tokid = sb(top, "tokid", [128, NTB, 2], I32)
        T.dma("sp", "c_cst", lambda q: q.dma_start(out=cst[:, :], in_=cst_d[:, :]), writes=["cst"])
        T.op("act", lambda e: e.copy(idb[:, :], cst[:, 0:128]), reads=["cst"], writes=["idb"])
        T.op("pool", lambda e: e.memset(ones[:, :], 1.0), writes=["ones"])
        T.op("pool", lambda e: e.memset(epsT[:, :], LN_EPS), writes=["epsT"])
        T.op("pool", lambda e: e.iota(tokid[:, :, :], pattern=[[128, NTB], [0, 2]], base=0, channel_multiplier=1),
             writes=["tokid"])
        idf = cst[:, 0:128]
        Uf = cst[:, 128:256]
        ecap = cst[:, 256:256 + E]

        with ExitStack() as st0:
            zrow = sb(st0, "zrow", [1, D + E])
            tinit = sb(st0, "tinit", [128, NSLOT * 2 // 128], I32)
            T.op("pool", lambda e: e.memset(zrow[:, :], 0.0), writes=["zrow"])
            T.op("pool", lambda e: e.memset(tinit[:, :], NT), writes=["tinit"])
            T.dma("sp", "i_x1", lambda q: q.dma_start(out=X1[NT:NT + 1, :], in_=zrow[:, :]), reads=["zrow"], writes=["X1z"])
            T.dma("sp", "i_od", lambda q: q.dma_start(out=OD[NSLOT:NSLOT + 1, :], in_=zrow[:, 0:D]), reads=["zrow"], writes=["ODz"])
            T.dma("sp", "i_tt", lambda q: q.dma_start(out=TT[0:NSLOT, :].rearrange("(p a) b -> p (a b)", p=128), in_=tinit[:, :]),
                  reads=["tinit"], writes=["TTinit"])
            T.barrier()

        with ExitStack() as st:
            NW = 5
            wp = [sb(st, f"wpA{i}", [128, 16, 512], BF16) for i in range(NW)]
            wctr = [0]

            def wload(src2d, K):
                s = wctr[0] % NW
                wctr[0] += 1
                T.dma("pool", f"wpA{s}",
                      lambda q: q.dma_start(out=wp[s][:, 0:K, :], in_=src2d.rearrange("(k p) c -> p k c", p=128)),
                      writes=[f"wpA{s}"])
                return wp[s], f"wpA{s}"

            NPA = 5
            psb = [ps_(st, f"psA{i}", [128, 512]) for i in range(NPA)]
            psH = ps_(st, "psH", [128, 512])
            psTA = [ps_(st, f"psTA{i}", [128, 8, 128], BF16) for i in range(2)]
            tctrA = [0]
            pctr = [0]

            def nextps():
                i = pctr[0] % NPA
                pctr[0] += 1
                return psb[i], f"psA{i}"

            xbs = [sb(st, f"xb{i}", [128, D], BF16) for i in range(2)]
            xissued = set()

            def xload(blk):
                if blk in xissued or blk >= NTB:
                    return
                xissued.add(blk)
                bb = blk % 2
                T.dma("pool", f"xb{bb}", lambda q: q.dma_start(out=xbs[bb][:, :], in_=xs[blk * 128:(blk + 1) * 128, :]),
                      writes=[f"xb{bb}"])
            xT = sb(st, "xT", [128, KD, 514], BF16)
            cw = sb(st, "cw", [128, 8, 3])
            lnv = sb(st, "lnv", [128, 2, CW])
            wsT = sb(st, "wsT", [128, 8, 128], BF16)
            bsb = sb(st, "bsb", [128, 8, 128])
            bg = sb(st, "bg", [128, 32])
            pre_sb = sb(st, "pre_sb", [128, 514])
            zb = sb(st, "zb", [128, 514])
            c1 = sb(st, "c1", [128, 512])
            c2 = sb(st, "c2", [128, 512])
            actA = sb(st, "actA", [128, 8, 512], BF16)
            actB = sb(st, "actB", [128, 8, 512], BF16)
            u = sb(st, "u", [128, 8, 512], BF16)
            v = sb(st, "v", [128, 4, CW], BF16)
            vg = sb(st, "vg", [128, CW])
            stt = sb(st, "stt", [128, 4, 6])
            mv = sb(st, "mv", [128, 8])
            tmpB = sb(st, "tmpB", [128, 512])
            gsb = [sb(st, f"gsb{i}", [128, 512]) for i in range(2)]
            m2 = sb(st, "m2", [128, 512])
            mtmp = sb(st, "mtmp", [128, 4, 512])
            mst = [sb(st, f"mst{i}", [128, 4, 512], BF16) for i in range(2)]

            T.dma("sp", "c_cw", lambda q: q.dma_start(out=cw[:, :, :], in_=cw_d[:, :, :]), writes=["cw"])
            T.dma("sp", "c_lnv", lambda q: q.dma_start(out=lnv[:, :, :], in_=lnv_d[:, :, :]), writes=["lnv"])
            T.dma("sp", "c_bsb", lambda q: q.dma_start(out=bsb[:, :, :], in_=bsb_d[:, :, :]), writes=["bsb"])
            T.dma("sp", "c_bg", lambda q: q.dma_start(out=bg[:, :], in_=bg_d[:, :]), writes=["bg"])
            T.dma("pool", "c_ws", lambda q: q.dma_start(out=wsT[:, :, :], in_=wsT_d[:, :, :]), writes=["wsT"])
            T.op("pool", lambda e: e.memset(wsT[64:128, :, 0:64], 0.0), reads=["wsT"], writes=["wsT"])

            mctr = [0]
            for ti in range(NTT):
                if ti == 0:
                    T.dma("pool", "c_xh", lambda q: q.dma_start(out=xT[:, :, 0:2], in_=xhT[:, :, :]), writes=["xT"])
                else:
                    T.op("dve", lambda e: e.tensor_copy(xT[:, :, 0:2], xT[:, :, 512:514]), reads=["xT"], writes=["xT"])
                for b in range(4):
                    blk = ti * 4 + b
                    xload(blk)
                    xb, xbr = xbs[blk % 2], f"xb{blk % 2}"
                    for half in range(2):
                        tp = tctrA[0] % 2
                        tctrA[0] += 1
                        psT, ptr_ = psTA[tp], f"psTA{tp}"
                        mms = [(lambda pe, a=a, half=half: pe.transpose(psT[:, a, :], xb[:, (half * 8 + a) * 128:(half * 8 + a + 1) * 128], idb[:, :]))
                               for a in range(8)]
                        T.mm_group(mms, [[xbr, "idb"]] * 8, [ptr_])
                        if half == 0:
                            T.op("dve", lambda e: e.tensor_copy(xT[:, 0:8, 2 + b * 128:2 + (b + 1) * 128], psT[:, :, :]), reads=[ptr_], writes=["xT"])
                        else:
                            T.op("act", lambda e: e.copy(xT[:, 8:16, 2 + b * 128:2 + (b + 1) * 128], psT[:, :, :]), reads=[ptr_], writes=["xT"])
                    if b + 2 < 4:
                        xload(blk + 2)

                def lin_fm(W, wres, cc, rhs_of_k, K, rres, pst, pres, c0=0, c1_=512):
                    mms = [(lambda pe, k=k: pe.matmul(pst[:, c0:c1_], lhsT=W[:, k, cc * 128:(cc + 1) * 128], rhs=rhs_of_k(k),
                                                      start=(k == 0), stop=(k == K - 1))) for k in range(K)]
                    T.mm_group(mms, [[wres] + rres] * K, [pres])

                for h in range(2):
                    Wpre, rpre = wload(w_in[:, h * 512:(h + 1) * 512], KD)
                    Whid, rhid = wload(w_in[:, CW + h * 512:CW + (h + 1) * 512], KD)
                    Wpost, rpost = wload(w_in[:, 2 * CW + h * 512:2 * CW + (h + 1) * 512], KD)
                    for cc in range(4):
                        j = 4 * h + cc
                        pP, rP = nextps()
                        pH, rH = nextps()
                        pO, rO = nextps()
                        pX, rX = psH, "psH"
                        main = lambda k: xT[:, k, 2:514]
                        halo = lambda k: xT[:, k, 0:2]
                        lin_fm(Wpre, rpre, cc, main, KD, ["xT"], pP, rP)
                        lin_fm(Wpre, rpre, cc, halo, KD, ["xT"], pX, rX, 0, 2)
                        lin_fm(Whid, rhid, cc, main, KD, ["xT"], pH, rH)
                        lin_fm(Whid, rhid, cc, halo, KD, ["xT"], pX, rX, 2, 4)
                        lin_fm(Wpost, rpost, cc, main, KD, ["xT"], pO, rO)
                        T.op("act", lambda e: e.copy(pre_sb[:, 2:514], pP[:, :]), reads=[rP], writes=["pre_sb"])
                        T.op("act", lambda e: e.copy(pre_sb[:, 0:2], pX[:, 0:2]), reads=[rX], writes=["pre_sb"])
                        T.op("dve", lambda e: e.tensor_tensor(zb[:, 2:514], pre_sb[:, 2:514], pH[:, :], ALU.mult),
                             reads=["pre_sb", rH], writes=["zb"])
                        T.op("dve", lambda e: e.tensor_tensor(zb[:, 0:2], pre_sb[:, 0:2], pX[:, 2:4], ALU.mult),
                             reads=["pre_sb", rX], writes=["zb"])
                        T.op("dve", lambda e: e.tensor_scalar(c1[:, :], zb[:, 0:512], cw[:, j, 0:1], None, ALU.mult),
                             reads=["zb", "cw"], writes=["c1"])
                        T.op("dve", lambda e: e.scalar_tensor_tensor(c2[:, :], zb[:, 1:513], cw[:, j, 1:2], c1[:, :], ALU.mult, ALU.add),
                             reads=["zb", "cw", "c1"], writes=["c2"])
                        T.op("dve", lambda e: e.scalar_tensor_tensor(c1[:, :], zb[:, 2:514], cw[:, j, 2:3], c2[:, :], ALU.mult, ALU.add),
                             reads=["zb", "cw", "c2"], writes=["c1"])
                        T.op("dve", lambda e: e.tensor_tensor(actA[:, j, :], c1[:, :], pO[:, :], ALU.mult),
                             reads=["c1", rO], writes=["actA"])

                Wv = [wload(w_in[:, 4 * CW + hh * 512:4 * CW + (hh + 1) * 512], KD) for hh in range(2)]
                for b in range(4):
                    for hh in range(2):
                        pV, rV = nextps()
                        Wt, rw = Wv[hh]
                        mms = [(lambda pe, k=k: pe.matmul(pV[:, :], lhsT=xT[:, k, 2 + b * 128:2 + (b + 1) * 128], rhs=Wt[:, k, :],
                                                          start=(k == 0), stop=(k == KD - 1))) for k in range(KD)]
                        T.mm_group(mms, [["xT", rw]] * KD, [rV])
                        T.op("act", lambda e: e.activation(vg[:, hh * 512:(hh + 1) * 512], pV[:, :], AF.Gelu), reads=[rV], writes=["vg"])
                    for hh in range(2):
                        T.op("dve", lambda e, hh=hh: e.bn_stats(stt[:, hh, :], vg[:, hh * 512:(hh + 1) * 512]), reads=["vg"], writes=["stt"])
                    T.op("dve", lambda e: e.bn_aggr(mv[:, 0:2], stt[:, 0:2, :].rearrange("p a b -> p (a b)")), reads=["stt"], writes=["mv"])
                    T.op("act", lambda e: e.activation(mv[:, 2:3], mv[:, 1:2], AF.Sqrt, bias=epsT[:, 0:1], scale=1.0),
                         reads=["mv", "epsT"], writes=["mv"])
                    T.op("dve", lambda e: e.reciprocal(mv[:, 3:4], mv[:, 2:3]), reads=["mv"], writes=["mv"])
                    T.op("dve", lambda e: e.tensor_scalar(vg[:, :], vg[:, :], mv[:, 0:1], mv[:, 3:4], ALU.subtract, ALU.mult),
                         reads=["vg", "mv"], writes=["vg"])
                    T.op("dve", lambda e: e.tensor_tensor(vg[:, :], vg[:, :], lnv[:, 0, :], ALU.mult), reads=["vg", "lnv"], writes=["vg"])
                    T.op("dve", lambda e: e.tensor_tensor(v[:, b, :], vg[:, :], lnv[:, 1, :], ALU.add), reads=["vg", "lnv"], writes=["v"])

                for h in range(2):
                    Wu, ru = wload(w_in[:, 3 * CW + h * 512:3 * CW + (h + 1) * 512], KD)
                    for cc in range(4):
                        j = 4 * h + cc
                        pU, rU = nextps()
                        lin_fm(Wu, ru, cc, lambda k: xT[:, k, 2:514], KD, ["xT"], pU, rU)
                        T.op("act", lambda e: e.activation(u[:, j, :], pU[:, :], AF.Gelu), reads=[rU], writes=["u"])

                for g in range(8):
                    pS, rS = nextps()
                    mms = [(lambda pe, b=b: pe.matmul(pS[:, b * 128:(b + 1) * 128], lhsT=v[:, b, g * 128:(g + 1) * 128], rhs=wsT[:, g, :],
                                                      start=True, stop=True)) for b in range(4)]
                    T.mm_group(mms, [["v", "wsT"]] * 4, [rS])
                    for b in range(4):
                        T.op("dve", lambda e, b=b: e.tensor_tensor(tmpB[:, b * 128:(b + 1) * 128], pS[:, b * 128:(b + 1) * 128], bsb[:, g, :], ALU.add),
                             reads=[rS, "bsb"], writes=["tmpB"])
                    T.op("dve", lambda e: e.tensor_tensor(actB[:, g, :], tmpB[:, :], u[:, g, :], ALU.mult),
                         reads=["tmpB", "u"], writes=["actB"])

                xload((ti + 1) * 4)
                xload((ti + 1) * 4 + 1)
                for q in range(4):
                    ms = mst[mctr[0] % 2]
                    mres = f"mst{mctr[0] % 2}"
                    for br in range(2):
                        Wg, rg = wload(w_in[:, 5 * CW + br * D + q * 512:5 * CW + br * D + (q + 1) * 512], KD)
                        Wy, ry = wload((w_a if br == 0 else w_b)[:, q * 512:(q + 1) * 512], 8)
                        act_in, ares = (actA, "actA") if br == 0 else (actB, "actB")
                        for cc in range(4):
                            c = 4 * q + cc
                            pG, rG = nextps()
                            pY, rY = nextps()
                            lin_fm(Wg, rg, cc, lambda k: xT[:, k, 2:514], KD, ["xT"], pG, rG)
                            lin_fm(Wy, ry, cc, lambda k: act_in[:, k, :], 8, [ares], pY, rY)
                            gs = gsb[(c + br) % 2]
                            gres = f"gsb{(c + br) % 2}"
                            T.op("act", lambda e: e.activation(gs[:, :], pG[:, :], AF.Sigmoid, bias=bg[:, br * 16 + c:br * 16 + c + 1], scale=1.0),
                                 reads=[rG, "bg"], writes=[gres])
                            if br == 0:
                                T.op("dve", lambda e: e.tensor_tensor(mtmp[:, cc, :], gs[:, :], pY[:, :], ALU.mult),
                                     reads=[gres, rY], writes=["mtmp"])
                            else:
                                T.op("dve", lambda e: e.tensor_tensor(m2[:, :], gs[:, :], pY[:, :], ALU.mult),
                                     reads=[gres, rY], writes=["m2"])
                                T.op("dve", lambda e: e.tensor_tensor(ms[:, cc, :], mtmp[:, cc, :], m2[:, :], ALU.add),
                                     reads=["mtmp", "m2"], writes=[mres])
                    T.dma("sp", mres, lambda q_, q=q, ti=ti: q_.dma_start(
                        out=MT[ti, :, q * 4 * 512:(q + 1) * 4 * 512], in_=ms[:, :, :].rearrange("p a b -> p (a b)")),
                        reads=[mres], writes=["MT"], order_writes=False)
                    mctr[0] += 1
            T.barrier()

        with ExitStack() as st:
            wo = sb(st, "wo", [128, KD, D], BF16)
            for n in range(4):
                T.dma("pool", f"wo{n}", lambda q, n=n: q.dma_start(
                    out=wo[:, :, n * 512:(n + 1) * 512], in_=w_o[:, n * 512:(n + 1) * 512].rearrange("(k p) c -> p k c", p=128)),
                    writes=[f"wo{n}"])
            ln1 = sb(st, "ln1", [128, 2, D])
            wr = sb(st, "wr", [128, KD, E])
            brb = sb(st, "brb", [128, E])
            T.dma("sp", "c_ln1", lambda q: q.dma_start(out=ln1[:, :, :], in_=ln1_d[:, :, :]), writes=["ln1"])
            T.dma("sp", "c_wr", lambda q: q.dma_start(out=wr[:, :, :], in_=wr_d[:, :, :]), writes=["wr"])
            T.dma("sp", "c_brb", lambda q: q.dma_start(out=brb[:, :], in_=brb_d[:, :]), writes=["brb"])
            mt = [sb(st, f"mt{i}", [128, KD, 512], BF16) for i in range(2)]
            xin2 = [sb(st, f"xin2_{i}", [128, D]) for i in range(3)]
            r = [sb(st, f"r{i}", [128, D]) for i in range(2)]
            xrow = [sb(st, f"xrow{i}", [128, D + E]) for i in range(2)]
            x1T = sb(st, "x1T", [128, KD, 128])
            st1 = sb(st, "st1", [128, 4, 6])
            mv1 = sb(st, "mv1", [128, 8])
            lg = sb(st, "lg", [128, E])
            top8 = sb(st, "top8", [128, 8])
            mask = sb(st, "mask", [128, E])
            ex = sb(st, "ex", [128, E])
            sm = sb(st, "sm", [128, 4])
            run = sb(st, "run", [128, E])
            aa = sb(st, "aa", [128, E])
            ok = sb(st, "ok", [128, E])
            svn = sb(st, "svn", [128, E])
            top8s = sb(st, "top8s", [128, 8])
            psm = [ps_(st, f"psM{i}", [128, 512]) for i in range(4)]
            pst = [ps_(st, f"psX{i}", [128, 4, 128]) for i in range(2)]
            psl = ps_(st, "psL", [128, 512])
            psc = ps_(st, "psC", [128, 512])
            T.op("pool", lambda e: e.memset(run[:, :], 0.0), writes=["run"])
            BIG = float(NSLOT)

            negcap = sb(st, "negcap", [128, E])
            T.op("dve", lambda e: e.tensor_scalar(negcap[:, :], ecap, -1.0, BIG, ALU.mult, ALU.add), reads=["cst"], writes=["negcap"])

            def load_mt(ti):
                mtt, mtr = mt[ti % 2], f"mt{ti % 2}"
                T.dma("sp", mtr, lambda q: q.dma_start(out=mtt[:, :, :].rearrange("p a b -> p (a b)"), in_=MT[ti, :, :]),
                      reads=["MT"], writes=[mtr])

            def loads(blk):
                par = blk % 3
                xi, xir = xin2[par], f"xin2_{par}"
                T.dma("sp", xir, lambda q: q.dma_start(out=xi[:, :], in_=xs[blk * 128:(blk + 1) * 128, :]), writes=[xir])

            def s1a(blk):
                ti, b = blk // 4, blk % 4
                mtt, mtr = mt[ti % 2], f"mt{ti % 2}"
                par = blk % 2
                xi, xir = xin2[blk % 3], f"xin2_{blk % 3}"
                rr, rres = r[par], f"r{par}"
                for n in range(4):
                    mms = [(lambda pe, k=k: pe.matmul(psm[n][:, :], lhsT=mtt[:, k, b * 128:(b + 1) * 128], rhs=wo[:, k, n * 512:(n + 1) * 512],
                                                      start=(k == 0), stop=(k == KD - 1))) for k in range(KD)]
                    T.mm_group(mms, [[mtr, f"wo{n}"]] * KD, [f"psM{n}"])
                    T.op("dve", lambda e, n=n: e.scalar_tensor_tensor(rr[:, n * 512:(n + 1) * 512], xi[:, n * 512:(n + 1) * 512], ALPHA,
                                                                     psm[n][:, :], ALU.mult, ALU.add),
                         reads=[xir, f"psM{n}"], writes=[rres])

            def s1b_stats(blk):
                par = blk % 2
                rr, rres = r[par], f"r{par}"
                for n in range(4):
                    T.op("dve", lambda e, n=n: e.bn_stats(st1[:, n, :], rr[:, n * 512:(n + 1) * 512]), reads=[rres], writes=["st1"])
                T.op("dve", lambda e: e.bn_aggr(mv1[:, 0:2], st1[:, :, :].rearrange("p a b -> p (a b)")), reads=["st1"], writes=["mv1"])
                T.op("act", lambda e: e.activation(mv1[:, 2:3], mv1[:, 1:2], AF.Ln, bias=epsT[:, 0:1], scale=1.0),
                     reads=["mv1", "epsT"], writes=["mv1"])
                T.op("act", lambda e: e.activation(mv1[:, 3:4], mv1[:, 2:3], AF.Exp, scale=-0.5), reads=["mv1"], writes=["mv1"])

            def s1b_norm(blk):
                par = blk % 2
                rr, rres = r[par], f"r{par}"
                xr, xres = xrow[par], f"xrow{par}"
                T.op("dve", lambda e: e.tensor_scalar(mv1[:, 4:5], mv1[:, 0:1], -1.0, mv1[:, 3:4], ALU.mult, ALU.mult),
                     reads=["mv1"], writes=["mv1"])
                T.op("act", lambda e: e.activation(rr[:, :], rr[:, :], AF.Identity, bias=mv1[:, 4:5], scale=mv1[:, 3:4]),
                     reads=[rres, "mv1"], writes=[rres])
                T.op("dve", lambda e: e.tensor_tensor(rr[:, :], rr[:, :], ln1[:, 0, :], ALU.mult), reads=[rres, "ln1"], writes=[rres])
                T.op("dve", lambda e: e.tensor_tensor(xr[:, 0:D], rr[:, :], ln1[:, 1, :], ALU.add), reads=[rres, "ln1"], writes=[xres])

            def s2(blk):
                par = blk % 2
                xr, xres = xrow[par], f"xrow{par}"
                for qd in range(4):
                    pt, ptr = pst[qd % 2], f"psX{qd % 2}"
                    mms = [(lambda pe, a=a: pe.transpose(pt[:, a, :], xr[:, (qd * 4 + a) * 128:(qd * 4 + a + 1) * 128], idf))
                           for a in range(4)]
                    T.mm_group(mms, [[xres, "cst"]] * 4, [ptr])
                    T.op("act", lambda e: e.copy(x1T[:, qd * 4:(qd + 1) * 4, :], pt[:, :, :]), reads=[ptr], writes=["x1T"])
                mms = [(lambda pe, k=k: pe.matmul(psl[:, 0:E], lhsT=x1T[:, k, :], rhs=wr[:, k, :], start=(k == 0), stop=(k == KD - 1)))
                       for k in range(KD)]
                T.mm_group(mms, [["x1T", "wr"]] * KD, ["psL"])

            def s3a(blk):
                T.op("dve", lambda e: e.tensor_tensor(lg[:, :], psl[:, 0:E], brb[:, :], ALU.add), reads=["psL", "brb"], writes=["lg"])
                T.op("dve", lambda e: e.max(out=top8[:, :], in_=lg[:, :]), reads=["lg"], writes=["top8"])
                T.op("dve", lambda e: e.tensor_scalar(mask[:, :], lg[:, :], top8[:, 3:4], None, ALU.is_ge), reads=["lg", "top8"], writes=["mask"])
                T.op("dve", lambda e: e.tensor_scalar(sm[:, 0:1], top8[:, 0:1], -1.0, None, ALU.mult), reads=["top8"], writes=["sm"])
                T.op("act", lambda e: e.activation(ex[:, :], lg[:, :], AF.Exp, bias=sm[:, 0:1], scale=1.0), reads=["lg", "sm"], writes=["ex"])
                mms = [lambda pe: pe.matmul(psc[:, 0:E], lhsT=Uf, rhs=mask[:, :], start=True, stop=False),
                       lambda pe: pe.matmul(psc[:, 0:E], lhsT=ones[:, :], rhs=run[:, :], start=False, stop=True)]
                T.mm_group(mms, [["cst", "mask"], ["ones", "run"]], ["psC"])

            def s3b(blk):
                par = blk % 2
                xr, xres = xrow[par], f"xrow{par}"
                T.op("dve", lambda e: e.scalar_tensor_tensor(aa[:, :], psc[:, 0:E], -1.0, negcap[:, :], ALU.mult, ALU.add),
                     reads=["psC", "negcap"], writes=["aa"])
                T.op("dve", lambda e: e.scalar_tensor_tensor(ok[:, :], psc[:, 0:E], float(CAP), mask[:, :], ALU.is_lt, ALU.mult),
                     reads=["psC", "mask"], writes=["ok"])
                T.op("dve", lambda e: e.tensor_tensor(run[:, :], run[:, :], mask[:, :], ALU.add), reads=["run", "mask"], writes=["run"])
                T.op("dve", lambda e: e.tensor_tensor(svn[:, :], aa[:, :], ok[:, :], ALU.mult), reads=["aa", "ok"], writes=["svn"])
                T.op("dve", lambda e: e.max(out=top8s[:, :], in_=svn[:, :]), reads=["svn"], writes=["top8s"])
                T.op("dve", lambda e: e.tensor_scalar(slots_all[:, blk, :], top8s[:, 0:TOPK], -1.0, BIG, ALU.mult, ALU.add),
                     reads=["top8s"], writes=[f"slots{blk % 2}"])
                for k in range(TOPK):
                    T.dma("pool", "scat", lambda q, k=k: q.indirect_dma_start(
                        out=TT[:, :], out_offset=bass.IndirectOffsetOnAxis(ap=slots_all[:, blk, k:k + 1], axis=0),
                        in_=tokid[:, blk, :], in_offset=None),
                        reads=[f"slots{blk % 2}", "tokid", "TTinit"], writes=["TT"], order_writes=False)
                T.op("dve", lambda e: e.tensor_tensor(ex[:, :], ex[:, :], mask[:, :], ALU.mult), reads=["ex", "mask"], writes=["ex"])
                T.op("dve", lambda e: e.reduce_sum(sm[:, 1:2], ex[:, :], axis=mybir.AxisListType.X), reads=["ex"], writes=["sm"])
                T.op("dve", lambda e: e.reciprocal(sm[:, 2:3], sm[:, 1:2]), reads=["sm"], writes=["sm"])
                T.op("dve", lambda e: e.tensor_scalar(xr[:, D:D + E], ex[:, :], sm[:, 2:3], None, ALU.mult), reads=["ex", "sm"], writes=[xres])
                T.dma("sp", f"x1s{par}", lambda q: q.dma_start(out=X1[blk * 128:(blk + 1) * 128, :], in_=xr[:, :]),
                      reads=[xres], writes=["X1"], order_writes=False)

            load_mt(0)
            loads(0)
            if NTB > 1:
                loads(1)
            for blk in range(NTB):
                if blk + 2 < NTB:
                    loads(blk + 2)
                if blk % 4 == 1 and blk // 4 + 1 < NTT:
                    load_mt(blk // 4 + 1)
                s1a(blk)
                if blk > 0:
                    s2(blk - 1)
                s1b_stats(blk)
                if blk > 0:
                    s3a(blk - 1)
                s1b_norm(blk)
                if blk > 0:
                    s3b(blk - 1)
            s2(NTB - 1)
            s3a(NTB - 1)
            s3b(NTB - 1)
            T.barrier()

        with ExitStack() as st:
            NW = 5
            wp = [sb(st, f"wpB{i}", [128, 16, 512], BF16) for i in range(NW)]
            wctr = [0]

            def wloadB(src2d, K):
                s = wctr[0] % NW
                wctr[0] += 1
                T.dma("pool", f"wpB{s}",
                      lambda q: q.dma_start(out=wp[s][:, 0:K, :], in_=src2d.rearrange("(k p) c -> p k c", p=128)),
                      writes=[f"wpB{s}"])
                return wp[s], f"wpB{s}"

            psb = [ps_(st, f"psB{i}", [128, 512]) for i in range(6)]
            psTs = [ps_(st, f"psTB{i}", [128, 8, 128], BF16) for i in range(2)]
            pctr = [0]
            tctr = [0]

            def nextpsB():
                i = pctr[0] % 6
                pctr[0] += 1
                return psb[i], f"psB{i}"

            bup = sb(st, "bup", [128, E, 2 * KF])
            T.dma("sp", "c_bup", lambda q: q.dma_start(out=bup[:, :, :], in_=bup_d[:, :, :]), writes=["bup"])
            idx = [sb(st, f"idx{i}", [128, NB, 2], I32) for i in range(2)]
            xg = [sb(st, f"xg{i}", [128, D + E]) for i in range(2)]
            xgbs = [sb(st, f"xgb{i}", [128, D], BF16) for i in range(NB)]
            xgT = sb(st, "xgT", [128, KD, CAP], BF16)
            gcol = [sb(st, f"gcol{i}", [128, NB]) for i in range(2)]
            hT = sb(st, "hT", [128, KF, CAP], BF16)
            gl = sb(st, "gl", [128, CAP])
            sg = sb(st, "sg", [128, CAP])
            ln_ = sb(st, "ln_", [128, CAP])
            bdn = [sb(st, f"bdn{i}", [128, D]) for i in range(2)]
            osb = [sb(st, f"osb{i}", [128, 512]) for i in range(2)]
            osc = [sb(st, f"osc{i}", [128, 512]) for i in range(2)]
            octr = [0]
            gctr = [0]

            def prep_head(e_):
                ep = e_ % 2
                ix, ixr = idx[ep], f"idx{ep}"
                bd, bdr = bdn[ep], f"bdn{ep}"
                T.dma("sp", ixr, lambda q: q.dma_start(out=ix[:, :, :], in_=TT[e_ * CAP:(e_ + 1) * CAP, :].rearrange("(j p) b -> p j b", p=128)),
                      reads=["TT", "TTinit"], writes=[ixr])
                T.dma("sp", bdr, lambda q: q.dma_start(out=bd[:, :], in_=b_dn[e_:e_ + 1, :].to_broadcast([128, D])), writes=[bdr])

            def prep_gather(e_, j):
                ep = e_ % 2
                ix, ixr = idx[ep], f"idx{ep}"
                gc, gcr = gcol[ep], f"gcol{ep}"
                gp_ = gctr[0] % 2
                gctr[0] += 1
                xgt, xgr = xg[gp_], f"xg{gp_}"
                xgb, xbr = xgbs[j], f"xgb{j}"
                T.dma("pool", xgr, lambda q: q.indirect_dma_start(
                    out=xgt[:, :], out_offset=None, in_=X1[:, :],
                    in_offset=bass.IndirectOffsetOnAxis(ap=ix[:, j, 0:1], axis=0)),
                    reads=[ixr, "X1", "X1z"], writes=[xgr])
                T.op("act", lambda e: e.copy(xgb[:, :], xgt[:, 0:D]), reads=[xgr], writes=[xbr])
                T.op("dve", lambda e: e.tensor_copy(gc[:, j:j + 1], xgt[:, D + e_:D + e_ + 1]), reads=[xgr], writes=[gcr])

            def prep_transpose(e_, j):
                xgb, xbr = xgbs[j], f"xgb{j}"
                for half in range(2):
                    tp = tctr[0] % 2
                    tctr[0] += 1
                    psT, ptr_ = psTs[tp], f"psTB{tp}"
                    mms = [(lambda pe, a=a: pe.transpose(psT[:, a, :], xgb[:, (half * 8 + a) * 128:(half * 8 + a + 1) * 128], idb[:, :]))
                           for a in range(8)]
                    T.mm_group(mms, [[xbr, "idb"]] * 8, [ptr_])
                    if half == 0:
                        T.op("dve", lambda e: e.tensor_copy(xgT[:, 0:8, j * 128:(j + 1) * 128], psT[:, :, :]), reads=[ptr_], writes=["xgT"])
                    else:
                        T.op("act", lambda e: e.copy(xgT[:, 8:16, j * 128:(j + 1) * 128], psT[:, :, :]), reads=[ptr_], writes=["xgT"])

            def up(e_, hook=None):
                for g in range(F // 512):
                    Wg_, rg_ = wloadB(w_up[e_ * D:(e_ + 1) * D, g * 512:(g + 1) * 512], KD)
                    Wl_, rl_ = wloadB(w_up[e_ * D:(e_ + 1) * D, F + g * 512:F + (g + 1) * 512], KD)
                    for cc in range(4):
                        c = 4 * g + cc
                        pA, rA = nextpsB()
                        pB, rB = nextpsB()
                        pTl, rTl = (nextpsB() if len(colr) > 1 else (None, None))
                        for part, (W_, wr_, pm) in enumerate(((Wg_, rg_, pA), (Wl_, rl_, pB))):
                            for ci, (a0, a1) in enumerate(colr):
                                if ci == 0:
                                    dst, dres = pm[:, 0:a1 - a0], (rA if part == 0 else rB)
                                else:
                                    w_ = a1 - a0
                                    dst, dres = pTl[:, part * w_:(part + 1) * w_], rTl
                                mms = [(lambda pe, k=k, dst=dst, W_=W_, a0=a0, a1=a1: pe.matmul(
                                    dst, lhsT=W_[:, k, cc * 128:(cc + 1) * 128], rhs=xgT[:, k, a0:a1],
                                    start=(k == 0), stop=(k == KD - 1))) for k in range(KD)]
                                T.mm_group(mms, [[wr_, "xgT"]] * KD, [dres])
                        for ci, (a0, a1) in enumerate(colr):
                            w_ = a1 - a0
                            srcg = pA[:, 0:w_] if ci == 0 else pTl[:, 0:w_]
                            rgs = rA if ci == 0 else rTl
                            T.op("dve", lambda e, srcg=srcg, a0=a0, a1=a1: e.tensor_scalar(
                                gl[:, a0:a1], srcg, bup[:, e_, c:c + 1], SW_LIM, ALU.add, ALU.min),
                                reads=[rgs, "bup"], writes=["gl"])
                        T.op("act", lambda e: e.activation(sg[:, :], gl[:, :], AF.Silu, scale=SW_ALPHA), reads=["gl"], writes=["sg"])
                        for ci, (a0, a1) in enumerate(colr):
                            w_ = a1 - a0
                            srcl = pB[:, 0:w_] if ci == 0 else pTl[:, w_:2 * w_]
                            rls = rB if ci == 0 else rTl
                            T.op("dve", lambda e, srcl=srcl, a0=a0, a1=a1: e.tensor_scalar(
                                ln_[:, a0:a1], srcl, bup[:, e_, KF + c:KF + c + 1], -SW_LIM, ALU.add, ALU.max),
                                reads=[rls, "bup"], writes=["ln_"])
                        T.op("dve", lambda e: e.tensor_scalar(ln_[:, :], ln_[:, :], SW_LIM, 1.0, ALU.min, ALU.add), reads=["ln_"], writes=["ln_"])
                        T.op("dve", lambda e: e.scalar_tensor_tensor(hT[:, c, :], sg[:, :], 1.0 / SW_ALPHA, ln_[:, :], ALU.mult, ALU.mult),
                             reads=["sg", "ln_"], writes=["hT"])
                        if hook is not None:
                            hook(c)

            def down(e_, hook=None):
                ep = e_ % 2
                gc, gcr = gcol[ep], f"gcol{ep}"
                bd, bdr = bdn[ep], f"bdn{ep}"
                for n in range(D // 512):
                    Wd_, rd_ = wloadB(w_dn[e_ * F:(e_ + 1) * F, n * 512:(n + 1) * 512], KF)
                    for sbk in range(NB):
                        pO, rO = nextpsB()
                        mms = [(lambda pe, k=k: pe.matmul(pO[:, :], lhsT=hT[:, k, sbk * 128:(sbk + 1) * 128], rhs=Wd_[:, k, :],
                                                          start=(k == 0), stop=(k == KF - 1))) for k in range(KF)]
                        T.mm_group(mms, [["hT", rd_]] * KF, [rO])
                        op_ = octr[0] % 2
                        octr[0] += 1
                        o1, o1r = osb[op_], f"osb{op_}"
                        o2, o2r = osc[op_], f"osc{op_}"
                        T.op("dve", lambda e: e.tensor_tensor(o1[:, :], pO[:, :], bd[:, n * 512:(n + 1) * 512], ALU.add),
                             reads=[rO, bdr], writes=[o1r])
                        T.op("act", lambda e: e.mul(o2[:, :], o1[:, :], gc[:, sbk:sbk + 1]), reads=[o1r, gcr], writes=[o2r])
                        T.dma("sp", o2r, lambda q: q.dma_start(
                            out=OD[e_ * CAP + sbk * 128:e_ * CAP + (sbk + 1) * 128, n * 512:(n + 1) * 512], in_=o2[:, :]),
                            reads=[o2r], writes=["OD"], order_writes=False)
                    if hook is not None:
                        hook(n)

            NCH = KF
            gsched = {}
            for j in range(NB):
                gsched.setdefault(min(NCH - 1, (j * NCH) // NB), []).append(j)
            NDG = D // 512
            tsched = {}
            for j in range(NB):
                tsched.setdefault(min(NDG - 1, (j * NDG) // NB), []).append(j)
            prep_head(0)
            for j in range(NB):
                prep_gather(0, j)
            for j in range(NB):
                prep_transpose(0, j)
            for e_ in range(E):
                nxt = e_ + 1
                if nxt < E:
                    prep_head(nxt)
                    up(e_, hook=lambda c: [prep_gather(nxt, j) for j in gsched.get(c, [])])
                    down(e_, hook=lambda n: [prep_transpose(nxt, j) for j in tsched.get(n, [])])
                else:
                    up(e_)
                    down(e_)
            T.barrier()

        with ExitStack() as st:
            ln2 = sb(st, "ln2", [128, 2, D])
            T.dma("sp", "c_ln2", lambda q: q.dma_start(out=ln2[:, :, :], in_=ln2_d[:, :, :]), writes=["ln2"])
            og = [[sb(st, f"og{p}_{k}", [128, D]) for k in range(TOPK)] for p in range(2)]
            xr2 = [sb(st, f"xr2_{p}", [128, D]) for p in range(2)]
            acc = [sb(st, f"acc{p}", [128, D]) for p in range(2)]
            acc2 = sb(st, "acc2", [128, D])
            yy = [sb(st, f"yy{p}", [128, D]) for p in range(2)]
            st2 = sb(st, "st2", [128, 4, 6])
            mv2s = [sb(st, f"mv2_{i}", [128, 8]) for i in range(4)]

            def cL(i):
                p = i % 2
                T.dma("sp", f"xr2_{p}", lambda q: q.dma_start(out=xr2[p][:, :], in_=X1[i * 128:(i + 1) * 128, 0:D]),
                      reads=["X1"], writes=[f"xr2_{p}"])
                for k in range(TOPK):
                    T.dma("pool", f"og{p}_{k}", lambda q, k=k: q.indirect_dma_start(
                        out=og[p][k][:, :], out_offset=None, in_=OD[:, :],
                        in_offset=bass.IndirectOffsetOnAxis(ap=slots_all[:, i, k:k + 1], axis=0)),
                        reads=["slots0", "slots1", "OD", "ODz"], writes=[f"og{p}_{k}"])

            def cA(i):
                p = i % 2
                mvt, mvr = mv2s[i % 4], f"mv2_{i % 4}"
                a_, ar = acc[p], f"acc{p}"
                T.op("dve", lambda e: e.scalar_tensor_tensor(a_[:, :], xr2[p][:, :], ALPHA, og[p][0][:, :], ALU.mult, ALU.add),
                     reads=[f"xr2_{p}", f"og{p}_0"], writes=[ar])
                T.op("dve", lambda e: e.tensor_tensor(acc2[:, :], og[p][1][:, :], og[p][2][:, :], ALU.add),
                     reads=[f"og{p}_1", f"og{p}_2"], writes=["acc2"])
                T.op("dve", lambda e: e.tensor_tensor(a_[:, :], a_[:, :], og[p][3][:, :], ALU.add), reads=[ar, f"og{p}_3"], writes=[ar])
                T.op("dve", lambda e: e.tensor_tensor(a_[:, :], a_[:, :], acc2[:, :], ALU.add), reads=[ar, "acc2"], writes=[ar])
                for n in range(4):
                    T.op("dve", lambda e, n=n: e.bn_stats(st2[:, n, :], a_[:, n * 512:(n + 1) * 512]), reads=[ar], writes=["st2"])
                T.op("dve", lambda e: e.bn_aggr(mvt[:, 0:2], st2[:, :, :].rearrange("p a b -> p (a b)")), reads=["st2"], writes=[mvr])
                T.op("act", lambda e: e.activation(mvt[:, 2:3], mvt[:, 1:2], AF.Ln, bias=epsT[:, 0:1], scale=1.0),
                     reads=[mvr, "epsT"], writes=[mvr])
                T.op("act", lambda e: e.activation(mvt[:, 3:4], mvt[:, 2:3], AF.Exp, scale=-0.5), reads=[mvr], writes=[mvr])

            def cB(i):
                p = i % 2
                mvt, mvr = mv2s[i % 4], f"mv2_{i % 4}"
                a_, ar = acc[p], f"acc{p}"
                y_, yr = yy[p], f"yy{p}"
                T.op("dve", lambda e: e.tensor_scalar(mvt[:, 4:5], mvt[:, 0:1], -1.0, mvt[:, 3:4], ALU.mult, ALU.mult),
                     reads=[mvr], writes=[mvr])
                T.op("act", lambda e: e.activation(y_[:, :], a_[:, :], AF.Identity, bias=mvt[:, 4:5], scale=mvt[:, 3:4]),
                     reads=[ar, mvr], writes=[yr])

            def cC(i):
                p = i % 2
                y_, yr = yy[p], f"yy{p}"
                T.op("dve", lambda e: e.tensor_tensor(y_[:, :], y_[:, :], ln2[:, 0, :], ALU.mult), reads=[yr, "ln2"], writes=[yr])
                T.op("dve", lambda e: e.tensor_tensor(y_[:, :], y_[:, :], ln2[:, 1, :], ALU.add), reads=[yr, "ln2"], writes=[yr])
                T.dma("sp", f"out{p}", lambda q: q.dma_start(out=out[i * 128:(i + 1) * 128, :], in_=y_[:, :]),
                      reads=[yr], writes=["out"], order_writes=False)

            cL(0)
            for t in range(NTB + 2):
                if t + 1 < NTB:
                    cL(t + 1)
                if t < NTB:
                    cA(t)
                if 0 <= t - 1 < NTB:
                    cB(t - 1)
                if 0 <= t - 2 < NTB:
                    cC(t - 2)
            T.barrier()
    return nc


def prep_shared(inp, CAP):
    f = lambda a: np.ascontiguousarray(np.asarray(a, dtype=np.float32))
    rep = lambda vec: np.broadcast_to(np.asarray(vec, np.float32)[None, :], (128, len(vec)))
    F = inp["w_down"].shape[2]
    KF = F // 128
    d = {}
    d["w_in"] = f(inp["w_in"][0])
    d["cw"] = f(np.transpose(np.asarray(inp["conv_w"][0]).reshape(3, 8, 128), (2, 1, 0)))
    d["w_a"] = f(inp["w_a_out"][0])
    d["w_b"] = f(inp["w_b_out"][0])
    d["lnv"] = f(np.stack([rep(inp["ln_v_g"][0]), rep(inp["ln_v_b"][0])], axis=1))
    d["wsT"] = f(np.transpose(np.asarray(inp["w_s"][0]), (2, 0, 1)))
    d["bsb"] = f(np.broadcast_to(np.asarray(inp["b_s"][0])[None], (128, 8, 128)))
    d["bg"] = f(np.asarray(inp["b_gate"][0]).reshape(32, 128).T)
    d["w_o"] = f(inp["w_o"][0])
    d["ln1"] = f(np.stack([rep(inp["ln1_g"][0]), rep(inp["ln1_b"][0])], axis=1))
    d["ln2"] = f(np.stack([rep(inp["ln2_g"][0]), rep(inp["ln2_b"][0])], axis=1))
    d["wr"] = f(np.transpose(np.asarray(inp["w_router"][0]).reshape(KD, 128, E), (1, 0, 2)))
    d["brb"] = f(rep(inp["b_router"][0]))
    d["w_up"] = f(np.asarray(inp["w_up"][0]).reshape(E * D, 2 * F))
    d["bup"] = f(np.transpose(np.asarray(inp["b_up"][0]).reshape(E, 2 * KF, 128), (2, 0, 1)))
    d["w_dn"] = f(np.asarray(inp["w_down"][0]).reshape(E * F, D))
    d["b_dn"] = f(inp["b_down"][0])
    cst = np.zeros((128, 256 + E), np.float32)
    cst[:, 0:128] = np.eye(128, dtype=np.float32)
    cst[:, 128:256] = np.triu(np.ones((128, 128), np.float32), k=1)
    cst[:, 256:] = (np.arange(E, dtype=np.float32) * CAP)[None, :]
    d["cst"] = cst
    return d


def run(inp, n_cores, CAP):
    x = np.asarray(inp["x"], dtype=np.float32)
    B, S, _ = x.shape
    xf = x.reshape(B * S, D)
    NT = (B * S) // n_cores
    F = inp["w_down"].shape[2]
    shared = prep_shared(inp, CAP)
    nc = build_nc(NT, F, CAP)
    in_maps = []
    for c in range(n_cores):
        t0 = c * NT
        m = dict(shared)
        m["xs"] = np.ascontiguousarray(xf[t0:t0 + NT])
        halo = np.zeros((2, D), np.float32)
        if t0 % S != 0:
            halo = xf[t0 - 2:t0]
        m["xhT"] = np.ascontiguousarray(np.transpose(halo.reshape(2, KD, 128), (2, 1, 0)))
        in_maps.append(m)
    res = run_bass_kernel_spmd(nc, in_maps, core_ids=list(range(n_cores)))
    outs = [np.asarray(res.results[c]["out"]) for c in range(n_cores)]
    return np.concatenate(outs, axis=0).reshape(B, S, D).astype(np.float32)


def kernel(**inputs):
    return run(inputs, 8, 640)
```

```python
from contextlib import ExitStack
import numpy as np
import concourse.bass as bass
import concourse.mybir as mybir
from concourse.bass_utils import run_bass_kernel_spmd

F32 = mybir.dt.float32
BF16 = mybir.dt.bfloat16
I32 = mybir.dt.int32
AF = mybir.ActivationFunctionType
ALU = mybir.AluOpType

D = 2048
KD = D // 128
CW = 1024
E = 32
TOPK = 4
LN_EPS = 1e-5
ALPHA = (2.0 * 1) ** 0.25
SW_ALPHA = 1.702
SW_LIM = 7.0
IN_COLS = 3 * CW + 2 * CW + 2 * D


class Tracker:
    def __init__(self, nc, stack):
        self.nc = nc
        self.stack = stack
        self.eng = {"pe": nc.tensor, "act": nc.scalar, "dve": nc.vector, "pool": nc.gpsimd, "sp": nc.sync}
        self.sem = {}
        self.cnt = {}
        for e in ("pe", "act", "dve", "pool"):
            self.sem[e] = stack.enter_context(nc.semaphore("sem_" + e))
            self.cnt[e] = 0
        self.waited = {e: {} for e in self.eng}
        self.lastw = {}
        self.readers = {}
        self.chan = {}

    def _deps(self, reads, writes, order_writes=True):
        deps = []
        for r in reads:
            deps.extend(self.lastw.get(r, []))
        for w in writes:
            if order_writes:
                deps.extend(self.lastw.get(w, []))
            deps.extend(self.readers.get(w, []))
        return deps

    def _wait(self, e, deps):
        best = {}
        for (key, sem, val) in deps:
            if key not in best or best[key][1] < val:
                best[key] = (sem, val)
        for key, (sem, val) in best.items():
            if self.waited[e].get(key, 0) < val:
                self.eng[e].wait_ge(sem, val)
                self.waited[e][key] = val

    def _record(self, tok, reads, writes, order_writes=True):
        for r in reads:
            self.readers.setdefault(r, []).append(tok)
        for w in writes:
            if order_writes:
                self.lastw[w] = [tok]
                self.readers[w] = []
            else:
                self.lastw.setdefault(w, []).append(tok)

    def op(self, e, fn, reads=(), writes=()):
        self._wait(e, self._deps(reads, writes))
        ins = fn(self.eng[e])
        self.cnt[e] += 1
        ins.then_inc(self.sem[e], 1)
        tok = (e, self.sem[e], self.cnt[e])
        self._record(tok, reads, writes)
        return tok

    def mm_group(self, mms, reads_list, writes):
        tok = ("pe", self.sem["pe"], self.cnt["pe"] + 1)
        n = len(mms)
        for i, fn in enumerate(mms):
            self._wait("pe", self._deps(reads_list[i], writes if i == 0 else ()))
            ins = fn(self.eng["pe"])
            if i == n - 1:
                ins.then_inc(self.sem["pe"], 1)
        self.cnt["pe"] += 1
        allreads = set()
        for r in reads_list:
            allreads.update(r)
        self._record(tok, list(allreads), writes)
        return tok

    def dma(self, q, chan, fn, reads=(), writes=(), order_writes=True):
        if chan not in self.chan:
            self.chan[chan] = [self.stack.enter_context(self.nc.semaphore("ds_" + chan)), 0]
        self._wait(q, self._deps(reads, writes, order_writes))
        ins = fn(self.eng[q])
        c = self.chan[chan]
        c[1] += 16
        ins.then_inc(c[0], 16)
        tok = ("d_" + chan, c[0], c[1])
        self._record(tok, reads, writes, order_writes)
        return tok

    def barrier(self):
        toks = [(e, self.sem[e], self.cnt[e]) for e in self.sem if self.cnt[e] > 0]
        toks += [("d_" + c, sv[0], sv[1]) for c, sv in self.chan.items() if sv[1] > 0]
        for e in self.eng:
            self._wait(e, toks)

    def wait_all(self, e, resources):
        deps = []
        for r in resources:
            deps.extend(self.lastw.get(r, []))
            deps.extend(self.readers.get(r, []))
        self._wait(e, deps)


def build_nc(NT, F, CAP):
    NTT = NT // 512
    NTB = NT // 128
    KF = F // 128
    NB = CAP // 128
    NSLOT = E * CAP
    colr = [(0, min(CAP, 512))] + ([(512, CAP)] if CAP > 512 else [])

    nc = bass.Bass("TRN2", target_bir_lowering=False)

    def din(name, shape, dt=F32):
        return nc.dram_tensor(name, list(shape), dt, kind="ExternalInput").ap()

    xs = din("xs", [NT, D])
    xhT = din("xhT", [128, KD, 2])
    w_in = din("w_in", [D, IN_COLS])
    cw_d = din("cw", [128, 8, 3])
    w_a = din("w_a", [CW, D])
    w_b = din("w_b", [CW, D])
    lnv_d = din("lnv", [128, 2, CW])
    wsT_d = din("wsT", [128, 8, 128])
    bsb_d = din("bsb", [128, 8, 128])
    bg_d = din("bg", [128, 32])
    w_o = din("w_o", [D, D])
    ln1_d = din("ln1", [128, 2, D])
    ln2_d = din("ln2", [128, 2, D])
    wr_d = din("wr", [128, KD, E])
    brb_d = din("brb", [128, E])
    w_up = din("w_up", [E * D, 2 * F])
    bup_d = din("bup", [128, E, 2 * KF])
    w_dn = din("w_dn", [E * F, D])
    b_dn = din("b_dn", [E, D])
    cst_d = din("cst", [128, 128 + 128 + E])
    out = nc.dram_tensor("out", [NT, D], F32, kind="ExternalOutput").ap()

    MT = nc.dram_tensor("MT", [NTT, 128, KD * 512], BF16, kind="Internal").ap()
    X1 = nc.dram_tensor("X1", [NT + 1, D + E], F32, kind="Internal").ap()
    TT = nc.dram_tensor("TT", [NSLOT + 1, 2], I32, kind="Internal").ap()
    OD = nc.dram_tensor("OD", [NSLOT + 1, D], F32, kind="Internal").ap()

    with ExitStack() as top:
        T = Tracker(nc, top)
        sb = lambda st, name, shape, dt=F32: st.enter_context(nc.sbuf_tensor("s_" + name, list(shape), dt))
        ps_ = lambda st, name, shape, dt=F32: st.enter_context(nc.psum_tensor("p_" + name, list(shape), dt))

        cst = sb(top, "cst", [128, 128 + 128 + E])
        idb = sb(top, "idb", [128, 128], BF16)
        ones = sb(top, "ones", [128, 128])
        epsT = sb(top, "epsT", [128, 1])
        slots_all = sb(top, "slots_all", [128, NTB, TOPK], I32)
        tokid = sb(top, "tokid", [128, NTB, 2], I32)
        T.dma("sp", "c_cst", lambda q: q.dma_start(out=cst[:, :], in_=cst_d[:, :]), writes=["cst"])
        T.op("act", lambda e: e.copy(idb[:, :], cst[:, 0:128]), reads=["cst"], writes=["idb"])
        T.op("pool", lambda e: e.memset(ones[:, :], 1.0), writes=["ones"])
        T.op("pool", lambda e: e.memset(epsT[:, :], LN_EPS), writes=["epsT"])
        T.op("pool", lambda e: e.iota(tokid[:, :, :], pattern=[[128, NTB], [0, 2]], base=0, channel_multiplier=1),
             writes=["tokid"])
        idf = cst[:, 0:128]
        Uf = cst[:, 128:256]
        ecap = cst[:, 256:256 + E]

        with ExitStack() as st0:
            zrow = sb(st0, "zrow", [1, D + E])
            tinit = sb(st0, "tinit", [128, NSLOT * 2 // 128], I32)
            T.op("pool", lambda e: e.memset(zrow[:, :], 0.0), writes=["zrow"])
            T.op("pool", lambda e: e.memset(tinit[:, :], NT), writes=["tinit"])
            T.dma("sp", "i_x1", lambda q: q.dma_start(out=X1[NT:NT + 1, :], in_=zrow[:, :]), reads=["zrow"], writes=["X1z"])
            T.dma("sp", "i_od", lambda q: q.dma_start(out=OD[NSLOT:NSLOT + 1, :], in_=zrow[:, 0:D]), reads=["zrow"], writes=["ODz"])
            T.dma("sp", "i_tt", lambda q: q.dma_start(out=TT[0:NSLOT, :].rearrange("(p a) b -> p (a b)", p=128), in_=tinit[:, :]),
                  reads=["tinit"], writes=["TTinit"])
            T.barrier()

        with ExitStack() as st:
            NW = 5
            wp = [sb(st, f"wpA{i}", [128, 16, 512], BF16) for i in range(NW)]
            wctr = [0]

            def wload(src2d, K):
                s = wctr[0] % NW
                wctr[0] += 1
                T.dma("pool", f"wpA{s}",
                      lambda q: q.dma_start(out=wp[s][:, 0:K, :], in_=src2d.rearrange("(k p) c -> p k c", p=128)),
                      writes=[f"wpA{s}"])
                return wp[s], f"wpA{s}"

            NPA = 5
            psb = [ps_(st, f"psA{i}", [128, 512]) for i in range(NPA)]
            psH = ps_(st, "psH", [128, 512])
            psTA = [ps_(st, f"psTA{i}", [128, 8, 128], BF16) for i in range(2)]
            tctrA = [0]
            pctr = [0]

            def nextps():
                i = pctr[0] % NPA
                pctr[0] += 1
                return psb[i], f"psA{i}"

            xbs = [sb(st, f"xb{i}", [128, D], BF16) for i in range(2)]
            xissued = set()

            def xload(blk):
                if blk in xissued or blk >= NTB:
                    return
                xissued.add(blk)
                bb = blk % 2
                T.dma("pool", f"xb{bb}", lambda q: q.dma_start(out=xbs[bb][:, :], in_=xs[blk * 128:(blk + 1) * 128, :]),
                      writes=[f"xb{bb}"])
            xT = sb(st, "xT", [128, KD, 514], BF16)
            cw = sb(st, "cw", [128, 8, 3])
            lnv = sb(st, "lnv", [128, 2, CW])
            wsT = sb(st, "wsT", [128, 8, 128], BF16)
            bsb = sb(st, "bsb", [128, 8, 128])
            bg = sb(st, "bg", [128, 32])
            pre_sb = sb(st, "pre_sb", [128, 514])
            zb = sb(st, "zb", [128, 514])
            zc = sb(st, "zc", [128, 8, 2])
            c1 = sb(st, "c1", [128, 512])
            c2 = sb(st, "c2", [128, 512])
            actA = sb(st, "actA", [128, 8, 512], BF16)
            actB = sb(st, "actB", [128, 8, 512], BF16)
            u = sb(st, "u", [128, 8, 512], BF16)
            v = sb(st, "v", [128, 4, CW], BF16)
            vg = sb(st, "vg", [128, CW])
            stt = sb(st, "stt", [128, 4, 6])
            mv = sb(st, "mv", [128, 8])
            tmpB = sb(st, "tmpB", [128, 512])
            gsb = [sb(st, f"gsb{i}", [128, 512]) for i in range(2)]
            m2 = sb(st, "m2", [128, 512])
            mtmp = sb(st, "mtmp", [128, 4, 512])
            mst = [sb(st, f"mst{i}", [128, 4, 512], BF16) for i in range(2)]

            T.dma("sp", "c_cw", lambda q: q.dma_start(out=cw[:, :, :], in_=cw_d[:, :, :]), writes=["cw"])
            T.dma("sp", "c_lnv", lambda q: q.dma_start(out=lnv[:, :, :], in_=lnv_d[:, :, :]), writes=["lnv"])
            T.dma("sp", "c_bsb", lambda q: q.dma_start(out=bsb[:, :, :], in_=bsb_d[:, :, :]), writes=["bsb"])
            T.dma("sp", "c_bg", lambda q: q.dma_start(out=bg[:, :], in_=bg_d[:, :]), writes=["bg"])
            T.dma("pool", "c_ws", lambda q: q.dma_start(out=wsT[:, :, :], in_=wsT_d[:, :, :]), writes=["wsT"])
            T.op("pool", lambda e: e.memset(wsT[64:128, :, 0:64], 0.0), reads=["wsT"], writes=["wsT"])

            mctr = [0]
            for ti in range(NTT):
                if ti == 0:
                    T.dma("pool", "c_xh", lambda q: q.dma_start(out=xT[:, :, 0:2], in_=xhT[:, :, :]), writes=["xT"])
                for b in range(4):
                    blk = ti * 4 + b
                    xload(blk)
                    xb, xbr = xbs[blk % 2], f"xb{blk % 2}"
                    for half in range(2):
                        tp = tctrA[0] % 2
                        tctrA[0] += 1
                        psT, ptr_ = psTA[tp], f"psTA{tp}"
                        mms = [(lambda pe, a=a, half=half: pe.transpose(psT[:, a, :], xb[:, (half * 8 + a) * 128:(half * 8 + a + 1) * 128], idb[:, :]))
                               for a in range(8)]
                        T.mm_group(mms, [[xbr, "idb"]] * 8, [ptr_])
                        if half == 0:
                            T.op("dve", lambda e: e.tensor_copy(xT[:, 0:8, 2 + b * 128:2 + (b + 1) * 128], psT[:, :, :]), reads=[ptr_], writes=["xT"])
                        else:
                            T.op("act", lambda e: e.copy(xT[:, 8:16, 2 + b * 128:2 + (b + 1) * 128], psT[:, :, :]), reads=[ptr_], writes=["xT"])
                    if b + 2 < 4:
                        xload(blk + 2)

                def lin_fm(W, wres, cc, rhs_of_k, K, rres, pst, pres, c0=0, c1_=512):
                    mms = [(lambda pe, k=k: pe.matmul(pst[:, c0:c1_], lhsT=W[:, k, cc * 128:(cc + 1) * 128], rhs=rhs_of_k(k),
                                                      start=(k == 0), stop=(k == K - 1))) for k in range(K)]
                    T.mm_group(mms, [[wres] + rres] * K, [pres])

                for h in range(2):
                    Wpre, rpre = wload(w_in[:, h * 512:(h + 1) * 512], KD)
                    Whid, rhid = wload(w_in[:, CW + h * 512:CW + (h + 1) * 512], KD)
                    Wpost, rpost = wload(w_in[:, 2 * CW + h * 512:2 * CW + (h + 1) * 512], KD)
                    for cc in range(4):
                        j = 4 * h + cc
                        pP, rP = nextps()
                        pH, rH = nextps()
                        pO, rO = nextps()
                        pX, rX = psH, "psH"
                        main = lambda k: xT[:, k, 2:514]
                        halo = lambda k: xT[:, k, 0:2]
                        lin_fm(Wpre, rpre, cc, main, KD, ["xT"], pP, rP)
                        if ti == 0:
                            lin_fm(Wpre, rpre, cc, halo, KD, ["xT"], pX, rX, 0, 2)
                        lin_fm(Whid, rhid, cc, main, KD, ["xT"], pH, rH)
                        if ti == 0:
                            lin_fm(Whid, rhid, cc, halo, KD, ["xT"], pX, rX, 2, 4)
                        lin_fm(Wpost, rpost, cc, main, KD, ["xT"], pO, rO)
                        T.op("act", lambda e: e.copy(pre_sb[:, 2:514], pP[:, :]), reads=[rP], writes=["pre_sb"])
                        if ti == 0:
                            T.op("act", lambda e: e.copy(pre_sb[:, 0:2], pX[:, 0:2]), reads=[rX], writes=["pre_sb"])
                            T.op("dve", lambda e: e.tensor_tensor(zb[:, 0:2], pre_sb[:, 0:2], pX[:, 2:4], ALU.mult),
                                 reads=["pre_sb", rX], writes=["zb"])
                        else:
                            T.op("dve", lambda e: e.tensor_copy(zb[:, 0:2], zc[:, j, :]), reads=["zc"], writes=["zb"])
                        T.op("dve", lambda e: e.tensor_tensor(zb[:, 2:514], pre_sb[:, 2:514], pH[:, :], ALU.mult),
                             reads=["pre_sb", rH], writes=["zb"])
                        T.op("dve", lambda e: e.tensor_copy(zc[:, j, :], zb[:, 512:514]), reads=["zb"], writes=["zc"])
                        T.op("dve", lambda e: e.tensor_scalar(c1[:, :], zb[:, 0:512], cw[:, j, 0:1], None, ALU.mult),
                             reads=["zb", "cw"], writes=["c1"])
                        T.op("dve", lambda e: e.scalar_tensor_tensor(c2[:, :], zb[:, 1:513], cw[:, j, 1:2], c1[:, :], ALU.mult, ALU.add),
                             reads=["zb", "cw", "c1"], writes=["c2"])
                        T.op("dve", lambda e: e.scalar_tensor_tensor(c1[:, :], zb[:, 2:514], cw[:, j, 2:3], c2[:, :], ALU.mult, ALU.add),
                             reads=["zb", "cw", "c2"], writes=["c1"])
                        T.op("dve", lambda e: e.tensor_tensor(actA[:, j, :], c1[:, :], pO[:, :], ALU.mult),
                             reads=["c1", rO], writes=["actA"])

                Wv = [wload(w_in[:, 4 * CW + hh * 512:4 * CW + (hh + 1) * 512], KD) for hh in range(2)]
                for b in range(4):
                    for hh in range(2):
                        pV, rV = nextps()
                        Wt, rw = Wv[hh]
                        mms = [(lambda pe, k=k: pe.matmul(pV[:, :], lhsT=xT[:, k, 2 + b * 128:2 + (b + 1) * 128], rhs=Wt[:, k, :],
                                                          start=(k == 0), stop=(k == KD - 1))) for k in range(KD)]
                        T.mm_group(mms, [["xT", rw]] * KD, [rV])
                        T.op("act", lambda e: e.activation(vg[:, hh * 512:(hh + 1) * 512], pV[:, :], AF.Gelu), reads=[rV], writes=["vg"])
                    for hh in range(2):
                        T.op("dve", lambda e, hh=hh: e.bn_stats(stt[:, hh, :], vg[:, hh * 512:(hh + 1) * 512]), reads=["vg"], writes=["stt"])
                    T.op("dve", lambda e: e.bn_aggr(mv[:, 0:2], stt[:, 0:2, :].rearrange("p a b -> p (a b)")), reads=["stt"], writes=["mv"])
                    T.op("act", lambda e: e.activation(mv[:, 2:3], mv[:, 1:2], AF.Sqrt, bias=epsT[:, 0:1], scale=1.0),
                         reads=["mv", "epsT"], writes=["mv"])
                    T.op("dve", lambda e: e.reciprocal(mv[:, 3:4], mv[:, 2:3]), reads=["mv"], writes=["mv"])
                    T.op("dve", lambda e: e.tensor_scalar(vg[:, :], vg[:, :], mv[:, 0:1], mv[:, 3:4], ALU.subtract, ALU.mult),
                         reads=["vg", "mv"], writes=["vg"])
                    T.op("dve", lambda e: e.tensor_tensor(vg[:, :], vg[:, :], lnv[:, 0, :], ALU.mult), reads=["vg", "lnv"], writes=["vg"])
                    T.op("dve", lambda e: e.tensor_tensor(v[:, b, :], vg[:, :], lnv[:, 1, :], ALU.add), reads=["vg", "lnv"], writes=["v"])

                for h in range(2):
                    Wu, ru = wload(w_in[:, 3 * CW + h * 512:3 * CW + (h + 1) * 512], KD)
                    for cc in range(4):
                        j = 4 * h + cc
                        pU, rU = nextps()
                        lin_fm(Wu, ru, cc, lambda k: xT[:, k, 2:514], KD, ["xT"], pU, rU)
                        T.op("act", lambda e: e.activation(u[:, j, :], pU[:, :], AF.Gelu), reads=[rU], writes=["u"])

                for g in range(8):
                    pS, rS = nextps()
                    mms = [(lambda pe, b=b: pe.matmul(pS[:, b * 128:(b + 1) * 128], lhsT=v[:, b, g * 128:(g + 1) * 128], rhs=wsT[:, g, :],
                                                      start=True, stop=True)) for b in range(4)]
                    T.mm_group(mms, [["v", "wsT"]] * 4, [rS])
                    for b in range(4):
                        T.op("dve", lambda e, b=b: e.tensor_tensor(tmpB[:, b * 128:(b + 1) * 128], pS[:, b * 128:(b + 1) * 128], bsb[:, g, :], ALU.add),
                             reads=[rS, "bsb"], writes=["tmpB"])
                    T.op("dve", lambda e: e.tensor_tensor(actB[:, g, :], tmpB[:, :], u[:, g, :], ALU.mult),
                         reads=["tmpB", "u"], writes=["actB"])

                xload((ti + 1) * 4)
                xload((ti + 1) * 4 + 1)
                for q in range(4):
                    ms = mst[mctr[0] % 2]
                    mres = f"mst{mctr[0] % 2}"
                    for br in range(2):
                        Wg, rg = wload(w_in[:, 5 * CW + br * D + q * 512:5 * CW + br * D + (q + 1) * 512], KD)
                        Wy, ry = wload((w_a if br == 0 else w_b)[:, q * 512:(q + 1) * 512], 8)
                        act_in, ares = (actA, "actA") if br == 0 else (actB, "actB")
                        for cc in range(4):
                            c = 4 * q + cc
                            pG, rG = nextps()
                            pY, rY = nextps()
                            lin_fm(Wg, rg, cc, lambda k: xT[:, k, 2:514], KD, ["xT"], pG, rG)
                            lin_fm(Wy, ry, cc, lambda k: act_in[:, k, :], 8, [ares], pY, rY)
                            gs = gsb[(c + br) % 2]
                            gres = f"gsb{(c + br) % 2}"
                            T.op("act", lambda e: e.activation(gs[:, :], pG[:, :], AF.Sigmoid, bias=bg[:, br * 16 + c:br * 16 + c + 1], scale=1.0),
                                 reads=[rG, "bg"], writes=[gres])
                            if br == 0:
                                T.op("dve", lambda e: e.tensor_tensor(mtmp[:, cc, :], gs[:, :], pY[:, :], ALU.mult),
                                     reads=[gres, rY], writes=["mtmp"])
                            else:
                                T.op("dve", lambda e: e.tensor_tensor(m2[:, :], gs[:, :], pY[:, :], ALU.mult),
                                     reads=[gres, rY], writes=["m2"])
                                T.op("dve", lambda e: e.tensor_tensor(ms[:, cc, :], mtmp[:, cc, :], m2[:, :], ALU.add),
                                     reads=["mtmp", "m2"], writes=[mres])
                    T.dma("sp", mres, lambda q_, q=q, ti=ti: q_.dma_start(
                        out=MT[ti, :, q * 4 * 512:(q + 1) * 4 * 512], in_=ms[:, :, :].rearrange("p a b -> p (a b)")),
                        reads=[mres], writes=["MT"], order_writes=False)
                    mctr[0] += 1
            T.barrier()

        with ExitStack() as st:
            wo = sb(st, "wo", [128, KD, D], BF16)
            for n in range(4):
                T.dma("pool", f"wo{n}", lambda q, n=n: q.dma_start(
                    out=wo[:, :, n * 512:(n + 1) * 512], in_=w_o[:, n * 512:(n + 1) * 512].rearrange("(k p) c -> p k c", p=128)),
                    writes=[f"wo{n}"])
            ln1 = sb(st, "ln1", [128, 2, D])
            wr = sb(st, "wr", [128, KD, E])
            brb = sb(st, "brb", [128, E])
            T.dma("sp", "c_ln1", lambda q: q.dma_start(out=ln1[:, :, :], in_=ln1_d[:, :, :]), writes=["ln1"])
            T.dma("sp", "c_wr", lambda q: q.dma_start(out=wr[:, :, :], in_=wr_d[:, :, :]), writes=["wr"])
            T.dma("sp", "c_brb", lambda q: q.dma_start(out=brb[:, :], in_=brb_d[:, :]), writes=["brb"])
            mt = [sb(st, f"mt{i}", [128, KD, 512], BF16) for i in range(2)]
            xin2 = [sb(st, f"xin2_{i}", [128, D]) for i in range(3)]
            r = [sb(st, f"r{i}", [128, D]) for i in range(2)]
            xrow = [sb(st, f"xrow{i}", [128, D + E]) for i in range(2)]
            x1T = sb(st, "x1T", [128, KD, 128])
            st1 = sb(st, "st1", [128, 4, 6])
            mv1 = sb(st, "mv1", [128, 8])
            lg = sb(st, "lg", [128, E])
            top8 = sb(st, "top8", [128, 8])
            mask = sb(st, "mask", [128, E])
            ex = sb(st, "ex", [128, E])
            sm = sb(st, "sm", [128, 4])
            run = sb(st, "run", [128, E])
            aa = sb(st, "aa", [128, E])
            ok = sb(st, "ok", [128, E])
            svn = sb(st, "svn", [128, E])
            top8s = sb(st, "top8s", [128, 8])
            psm = [ps_(st, f"psM{i}", [128, 512]) for i in range(4)]
            pst = [ps_(st, f"psX{i}", [128, 4, 128]) for i in range(2)]
            psl = ps_(st, "psL", [128, 512])
            psc = ps_(st, "psC", [128, 512])
            T.op("pool", lambda e: e.memset(run[:, :], 0.0), writes=["run"])
            BIG = float(NSLOT)

            negcap = sb(st, "negcap", [128, E])
            T.op("dve", lambda e: e.tensor_scalar(negcap[:, :], ecap, -1.0, BIG, ALU.mult, ALU.add), reads=["cst"], writes=["negcap"])

            def load_mt(ti):
                mtt, mtr = mt[ti % 2], f"mt{ti % 2}"
                T.dma("sp", mtr, lambda q: q.dma_start(out=mtt[:, :, :].rearrange("p a b -> p (a b)"), in_=MT[ti, :, :]),
                      reads=["MT"], writes=[mtr])

            def loads(blk):
                par = blk % 3
                xi, xir = xin2[par], f"xin2_{par}"
                T.dma("sp", xir, lambda q: q.dma_start(out=xi[:, :], in_=xs[blk * 128:(blk + 1) * 128, :]), writes=[xir])

            def s1a(blk):
                ti, b = blk // 4, blk % 4
                mtt, mtr = mt[ti % 2], f"mt{ti % 2}"
                par = blk % 2
                xi, xir = xin2[blk % 3], f"xin2_{blk % 3}"
                rr, rres = r[par], f"r{par}"
                for n in range(4):
                    mms = [(lambda pe, k=k: pe.matmul(psm[n][:, :], lhsT=mtt[:, k, b * 128:(b + 1) * 128], rhs=wo[:, k, n * 512:(n + 1) * 512],
                                                      start=(k == 0), stop=(k == KD - 1))) for k in range(KD)]
                    T.mm_group(mms, [[mtr, f"wo{n}"]] * KD, [f"psM{n}"])
                    T.op("dve", lambda e, n=n: e.scalar_tensor_tensor(rr[:, n * 512:(n + 1) * 512], xi[:, n * 512:(n + 1) * 512], ALPHA,
                                                                     psm[n][:, :], ALU.mult, ALU.add),
                         reads=[xir, f"psM{n}"], writes=[rres])

            def s1b_stats(blk):
                par = blk % 2
                rr, rres = r[par], f"r{par}"
                for n in range(4):
                    T.op("dve", lambda e, n=n: e.bn_stats(st1[:, n, :], rr[:, n * 512:(n + 1) * 512]), reads=[rres], writes=["st1"])
                T.op("dve", lambda e: e.bn_aggr(mv1[:, 0:2], st1[:, :, :].rearrange("p a b -> p (a b)")), reads=["st1"], writes=["mv1"])
                T.op("act", lambda e: e.activation(mv1[:, 2:3], mv1[:, 1:2], AF.Ln, bias=epsT[:, 0:1], scale=1.0),
                     reads=["mv1", "epsT"], writes=["mv1"])
                T.op("act", lambda e: e.activation(mv1[:, 3:4], mv1[:, 2:3], AF.Exp, scale=-0.5), reads=["mv1"], writes=["mv1"])

            def s1b_norm(blk):
                par = blk % 2
                rr, rres = r[par], f"r{par}"
                xr, xres = xrow[par], f"xrow{par}"
                T.op("dve", lambda e: e.tensor_scalar(mv1[:, 4:5], mv1[:, 0:1], -1.0, mv1[:, 3:4], ALU.mult, ALU.mult),
                     reads=["mv1"], writes=["mv1"])
                T.op("act", lambda e: e.activation(rr[:, :], rr[:, :], AF.Identity, bias=mv1[:, 4:5], scale=mv1[:, 3:4]),
                     reads=[rres, "mv1"], writes=[rres])
                T.op("dve", lambda e: e.tensor_tensor(rr[:, :], rr[:, :], ln1[:, 0, :], ALU.mult), reads=[rres, "ln1"], writes=[rres])
                T.op("dve", lambda e: e.tensor_tensor(xr[:, 0:D], rr[:, :], ln1[:, 1, :], ALU.add), reads=[rres, "ln1"], writes=[xres])

            def s2(blk):
                par = blk % 2
                xr, xres = xrow[par], f"xrow{par}"
                for qd in range(4):
                    pt, ptr = pst[qd % 2], f"psX{qd % 2}"
                    mms = [(lambda pe, a=a: pe.transpose(pt[:, a, :], xr[:, (qd * 4 + a) * 128:(qd * 4 + a + 1) * 128], idf))
                           for a in range(4)]
                    T.mm_group(mms, [[xres, "cst"]] * 4, [ptr])
                    T.op("act", lambda e: e.copy(x1T[:, qd * 4:(qd + 1) * 4, :], pt[:, :, :]), reads=[ptr], writes=["x1T"])
                mms = [(lambda pe, k=k: pe.matmul(psl[:, 0:E], lhsT=x1T[:, k, :], rhs=wr[:, k, :], start=(k == 0), stop=(k == KD - 1)))
                       for k in range(KD)]
                T.mm_group(mms, [["x1T", "wr"]] * KD, ["psL"])

            def s3a(blk):
                T.op("dve", lambda e: e.tensor_tensor(lg[:, :], psl[:, 0:E], brb[:, :], ALU.add), reads=["psL", "brb"], writes=["lg"])
                T.op("dve", lambda e: e.max(out=top8[:, :], in_=lg[:, :]), reads=["lg"], writes=["top8"])
                T.op("dve", lambda e: e.tensor_scalar(mask[:, :], lg[:, :], top8[:, 3:4], None, ALU.is_ge), reads=["lg", "top8"], writes=["mask"])
                T.op("dve", lambda e: e.tensor_scalar(sm[:, 0:1], top8[:, 0:1], -1.0, None, ALU.mult), reads=["top8"], writes=["sm"])
                T.op("act", lambda e: e.activation(ex[:, :], lg[:, :], AF.Exp, bias=sm[:, 0:1], scale=1.0), reads=["lg", "sm"], writes=["ex"])
                mms = [lambda pe: pe.matmul(psc[:, 0:E], lhsT=Uf, rhs=mask[:, :], start=True, stop=False),
                       lambda pe: pe.matmul(psc[:, 0:E], lhsT=ones[:, :], rhs=run[:, :], start=False, stop=True)]
                T.mm_group(mms, [["cst", "mask"], ["ones", "run"]], ["psC"])

            def s3b(blk):
                par = blk % 2
                xr, xres = xrow[par], f"xrow{par}"
                T.op("dve", lambda e: e.scalar_tensor_tensor(aa[:, :], psc[:, 0:E], -1.0, negcap[:, :], ALU.mult, ALU.add),
                     reads=["psC", "negcap"], writes=["aa"])
                T.op("dve", lambda e: e.scalar_tensor_tensor(ok[:, :], psc[:, 0:E], float(CAP), mask[:, :], ALU.is_lt, ALU.mult),
                     reads=["psC", "mask"], writes=["ok"])
                T.op("dve", lambda e: e.tensor_tensor(run[:, :], run[:, :], mask[:, :], ALU.add), reads=["run", "mask"], writes=["run"])
                T.op("dve", lambda e: e.tensor_tensor(svn[:, :], aa[:, :], ok[:, :], ALU.mult), reads=["aa", "ok"], writes=["svn"])
                T.op("dve", lambda e: e.max(out=top8s[:, :], in_=svn[:, :]), reads=["svn"], writes=["top8s"])
                T.op("dve", lambda e: e.tensor_scalar(slots_all[:, blk, :], top8s[:, 0:TOPK], -1.0, BIG, ALU.mult, ALU.add),
                     reads=["top8s"], writes=[f"slots{blk % 2}"])
                for k in range(TOPK):
                    T.dma("pool", "scat", lambda q, k=k: q.indirect_dma_start(
                        out=TT[:, :], out_offset=bass.IndirectOffsetOnAxis(ap=slots_all[:, blk, k:k + 1], axis=0),
                        in_=tokid[:, blk, :], in_offset=None),
                        reads=[f"slots{blk % 2}", "tokid", "TTinit"], writes=["TT"], order_writes=False)
                T.op("dve", lambda e: e.tensor_tensor(ex[:, :], ex[:, :], mask[:, :], ALU.mult), reads=["ex", "mask"], writes=["ex"])
                T.op("dve", lambda e: e.reduce_sum(sm[:, 1:2], ex[:, :], axis=mybir.AxisListType.X), reads=["ex"], writes=["sm"])
                T.op("dve", lambda e: e.reciprocal(sm[:, 2:3], sm[:, 1:2]), reads=["sm"], writes=["sm"])
                T.op("dve", lambda e: e.tensor_scalar(xr[:, D:D + E], ex[:, :], sm[:, 2:3], None, ALU.mult), reads=["ex", "sm"], writes=[xres])
                T.dma("sp", f"x1s{par}", lambda q: q.dma_start(out=X1[blk * 128:(blk + 1) * 128, :], in_=xr[:, :]),
                      reads=[xres], writes=["X1"], order_writes=False)

            load_mt(0)
            loads(0)
            if NTB > 1:
                loads(1)
            for blk in range(NTB):
                if blk + 2 < NTB:
                    loads(blk + 2)
                if blk % 4 == 1 and blk // 4 + 1 < NTT:
                    load_mt(blk // 4 + 1)
                s1a(blk)
                if blk > 0:
                    s2(blk - 1)
                s1b_stats(blk)
                if blk > 0:
                    s3a(blk - 1)
                s1b_norm(blk)
                if blk > 0:
                    s3b(blk - 1)
            s2(NTB - 1)
            s3a(NTB - 1)
            s3b(NTB - 1)
            T.barrier()

        with ExitStack() as st:
            NW = 5
            wp = [sb(st, f"wpB{i}", [128, 16, 512], BF16) for i in range(NW)]
            wctr = [0]

            def wloadB(src2d, K):
                s = wctr[0] % NW
                wctr[0] += 1
                T.dma("pool", f"wpB{s}",
                      lambda q: q.dma_start(out=wp[s][:, 0:K, :], in_=src2d.rearrange("(k p) c -> p k c", p=128)),
                      writes=[f"wpB{s}"])
                return wp[s], f"wpB{s}"

            psb = [ps_(st, f"psB{i}", [128, 512]) for i in range(6)]
            psTs = [ps_(st, f"psTB{i}", [128, 8, 128], BF16) for i in range(2)]
            pctr = [0]
            tctr = [0]

            def nextpsB():
                i = pctr[0] % 6
                pctr[0] += 1
                return psb[i], f"psB{i}"

            bup = sb(st, "bup", [128, E, 2 * KF])
            T.dma("sp", "c_bup", lambda q: q.dma_start(out=bup[:, :, :], in_=bup_d[:, :, :]), writes=["bup"])
            idx = [sb(st, f"idx{i}", [128, NB, 2], I32) for i in range(2)]
            xg = [sb(st, f"xg{i}", [128, D + E]) for i in range(2)]
            xgbs = [sb(st, f"xgb{i}", [128, D], BF16) for i in range(NB)]
            xgT = sb(st, "xgT", [128, KD, CAP], BF16)
            gcol = [sb(st, f"gcol{i}", [128, NB]) for i in range(2)]
            hT = sb(st, "hT", [128, KF, CAP], BF16)
            gl = sb(st, "gl", [128, CAP])
            sg = sb(st, "sg", [128, CAP])
            ln_ = sb(st, "ln_", [128, CAP])
            bdn = [sb(st, f"bdn{i}", [128, D]) for i in range(2)]
            osb = [sb(st, f"osb{i}", [128, 512]) for i in range(2)]
            osc = [sb(st, f"osc{i}", [128, 512]) for i in range(2)]
            octr = [0]
            gctr = [0]

            def prep_head(e_):
                ep = e_ % 2
                ix, ixr = idx[ep], f"idx{ep}"
                bd, bdr = bdn[ep], f"bdn{ep}"
                T.dma("sp", ixr, lambda q: q.dma_start(out=ix[:, :, :], in_=TT[e_ * CAP:(e_ + 1) * CAP, :].rearrange("(j p) b -> p j b", p=128)),
                      reads=["TT", "TTinit"], writes=[ixr])
                T.dma("sp", bdr, lambda q: q.dma_start(out=bd[:, :], in_=b_dn[e_:e_ + 1, :].to_broadcast([128, D])), writes=[bdr])

            def prep_gather(e_, j):
                ep = e_ % 2
                ix, ixr = idx[ep], f"idx{ep}"
                gc, gcr = gcol[ep], f"gcol{ep}"
                gp_ = gctr[0] % 2
                gctr[0] += 1
                xgt, xgr = xg[gp_], f"xg{gp_}"
                xgb, xbr = xgbs[j], f"xgb{j}"
                T.dma("pool", xgr, lambda q: q.indirect_dma_start(
                    out=xgt[:, :], out_offset=None, in_=X1[:, :],
                    in_offset=bass.IndirectOffsetOnAxis(ap=ix[:, j, 0:1], axis=0)),
                    reads=[ixr, "X1", "X1z"], writes=[xgr])
                T.op("act", lambda e: e.copy(xgb[:, :], xgt[:, 0:D]), reads=[xgr], writes=[xbr])
                T.op("dve", lambda e: e.tensor_copy(gc[:, j:j + 1], xgt[:, D + e_:D + e_ + 1]), reads=[xgr], writes=[gcr])

            def prep_transpose(e_, j):
                xgb, xbr = xgbs[j], f"xgb{j}"
                for half in range(2):
                    tp = tctr[0] % 2
                    tctr[0] += 1
                    psT, ptr_ = psTs[tp], f"psTB{tp}"
                    mms = [(lambda pe, a=a: pe.transpose(psT[:, a, :], xgb[:, (half * 8 + a) * 128:(half * 8 + a + 1) * 128], idb[:, :]))
                           for a in range(8)]
                    T.mm_group(mms, [[xbr, "idb"]] * 8, [ptr_])
                    if half == 0:
                        T.op("dve", lambda e: e.tensor_copy(xgT[:, 0:8, j * 128:(j + 1) * 128], psT[:, :, :]), reads=[ptr_], writes=["xgT"])
                    else:
                        T.op("act", lambda e: e.copy(xgT[:, 8:16, j * 128:(j + 1) * 128], psT[:, :, :]), reads=[ptr_], writes=["xgT"])

            def up(e_, hook=None):
                for g in range(F // 512):
                    Wg_, rg_ = wloadB(w_up[e_ * D:(e_ + 1) * D, g * 512:(g + 1) * 512], KD)
                    Wl_, rl_ = wloadB(w_up[e_ * D:(e_ + 1) * D, F + g * 512:F + (g + 1) * 512], KD)
                    for cc in range(4):
                        c = 4 * g + cc
                        pA, rA = nextpsB()
                        pB, rB = nextpsB()
                        pTl, rTl = (nextpsB() if len(colr) > 1 else (None, None))
                        for part, (W_, wr_, pm) in enumerate(((Wg_, rg_, pA), (Wl_, rl_, pB))):
                            for ci, (a0, a1) in enumerate(colr):
                                if ci == 0:
                                    dst, dres = pm[:, 0:a1 - a0], (rA if part == 0 else rB)
                                else:
                                    w_ = a1 - a0
                                    dst, dres = pTl[:, part * w_:(part + 1) * w_], rTl
                                mms = [(lambda pe, k=k, dst=dst, W_=W_, a0=a0, a1=a1: pe.matmul(
                                    dst, lhsT=W_[:, k, cc * 128:(cc + 1) * 128], rhs=xgT[:, k, a0:a1],
                                    start=(k == 0), stop=(k == KD - 1))) for k in range(KD)]
                                T.mm_group(mms, [[wr_, "xgT"]] * KD, [dres])
                        for ci, (a0, a1) in enumerate(colr):
                            w_ = a1 - a0
                            srcg = pA[:, 0:w_] if ci == 0 else pTl[:, 0:w_]
                            rgs = rA if ci == 0 else rTl
                            T.op("dve", lambda e, srcg=srcg, a0=a0, a1=a1: e.tensor_scalar(
                                gl[:, a0:a1], srcg, bup[:, e_, c:c + 1], SW_LIM, ALU.add, ALU.min),
                                reads=[rgs, "bup"], writes=["gl"])
                        T.op("act", lambda e: e.activation(sg[:, :], gl[:, :], AF.Silu, scale=SW_ALPHA), reads=["gl"], writes=["sg"])
                        for ci, (a0, a1) in enumerate(colr):
                            w_ = a1 - a0
                            srcl = pB[:, 0:w_] if ci == 0 else pTl[:, w_:2 * w_]
                            rls = rB if ci == 0 else rTl
                            T.op("dve", lambda e, srcl=srcl, a0=a0, a1=a1: e.tensor_scalar(
                                ln_[:, a0:a1], srcl, bup[:, e_, KF + c:KF + c + 1], -SW_LIM, ALU.add, ALU.max),
                                reads=[rls, "bup"], writes=["ln_"])
                        T.op("dve", lambda e: e.tensor_scalar(ln_[:, :], ln_[:, :], SW_LIM, 1.0, ALU.min, ALU.add), reads=["ln_"], writes=["ln_"])
                        T.op("dve", lambda e: e.scalar_tensor_tensor(hT[:, c, :], sg[:, :], 1.0 / SW_ALPHA, ln_[:, :], ALU.mult, ALU.mult),
                             reads=["sg", "ln_"], writes=["hT"])
                        if hook is not None:
                            hook(c)

            def down(e_, hook=None):
                ep = e_ % 2
                gc, gcr = gcol[ep], f"gcol{ep}"
                bd, bdr = bdn[ep], f"bdn{ep}"
                for n in range(D // 512):
                    Wd_, rd_ = wloadB(w_dn[e_ * F:(e_ + 1) * F, n * 512:(n + 1) * 512], KF)
                    for sbk in range(NB):
                        pO, rO = nextpsB()
                        mms = [(lambda pe, k=k: pe.matmul(pO[:, :], lhsT=hT[:, k, sbk * 128:(sbk + 1) * 128], rhs=Wd_[:, k, :],
                                                          start=(k == 0), stop=(k == KF - 1))) for k in range(KF)]
                        T.mm_group(mms, [["hT", rd_]] * KF, [rO])
                        op_ = octr[0] % 2
                        octr[0] += 1
                        o1, o1r = osb[op_], f"osb{op_}"
                        o2, o2r = osc[op_], f"osc{op_}"
                        T.op("dve", lambda e: e.tensor_tensor(o1[:, :], pO[:, :], bd[:, n * 512:(n + 1) * 512], ALU.add),
                             reads=[rO, bdr], writes=[o1r])
                        T.op("act", lambda e: e.mul(o2[:, :], o1[:, :], gc[:, sbk:sbk + 1]), reads=[o1r, gcr], writes=[o2r])
                        T.dma("sp", o2r, lambda q: q.dma_start(
                            out=OD[e_ * CAP + sbk * 128:e_ * CAP + (sbk + 1) * 128, n * 512:(n + 1) * 512], in_=o2[:, :]),
                            reads=[o2r], writes=["OD"], order_writes=False)
                    if hook is not None:
                        hook(n)

            NCH = KF
            gsched = {}
            for j in range(NB):
                gsched.setdefault(min(NCH - 1, (j * NCH) // NB), []).append(j)
            NDG = D // 512
            tsched = {}
            for j in range(NB):
                tsched.setdefault(min(NDG - 1, (j * NDG) // NB), []).append(j)
            prep_head(0)
            for j in range(NB):
                prep_gather(0, j)
            for j in range(NB):
                prep_transpose(0, j)
            for e_ in range(E):
                nxt = e_ + 1
                if nxt < E:
                    prep_head(nxt)
                    up(e_, hook=lambda c: [prep_gather(nxt, j) for j in gsched.get(c, [])])
                    down(e_, hook=lambda n: [prep_transpose(nxt, j) for j in tsched.get(n, [])])
                else:
                    up(e_)
                    down(e_)
            T.barrier()

        with ExitStack() as st:
            ln2 = sb(st, "ln2", [128, 2, D])
            T.dma("sp", "c_ln2", lambda q: q.dma_start(out=ln2[:, :, :], in_=ln2_d[:, :, :]), writes=["ln2"])
            og = [[sb(st, f"og{p}_{k}", [128, D]) for k in range(TOPK)] for p in range(2)]
            xr2 = [sb(st, f"xr2_{p}", [128, D]) for p in range(2)]
            acc = [sb(st, f"acc{p}", [128, D]) for p in range(2)]
            acc2 = sb(st, "acc2", [128, D])
            yy = [sb(st, f"yy{p}", [128, D]) for p in range(2)]
            st2 = sb(st, "st2", [128, 4, 6])
            mv2s = [sb(st, f"mv2_{i}", [128, 8]) for i in range(4)]

            def cL(i):
                p = i % 2
                T.dma("sp", f"xr2_{p}", lambda q: q.dma_start(out=xr2[p][:, :], in_=X1[i * 128:(i + 1) * 128, 0:D]),
                      reads=["X1"], writes=[f"xr2_{p}"])
                for k in range(TOPK):
                    T.dma("pool", f"og{p}_{k}", lambda q, k=k: q.indirect_dma_start(
                        out=og[p][k][:, :], out_offset=None, in_=OD[:, :],
                        in_offset=bass.IndirectOffsetOnAxis(ap=slots_all[:, i, k:k + 1], axis=0)),
                        reads=["slots0", "slots1", "OD", "ODz"], writes=[f"og{p}_{k}"])

            def cA(i):
                p = i % 2
                mvt, mvr = mv2s[i % 4], f"mv2_{i % 4}"
                a_, ar = acc[p], f"acc{p}"
                T.op("dve", lambda e: e.scalar_tensor_tensor(a_[:, :], xr2[p][:, :], ALPHA, og[p][0][:, :], ALU.mult, ALU.add),
                     reads=[f"xr2_{p}", f"og{p}_0"], writes=[ar])
                T.op("dve", lambda e: e.tensor_tensor(acc2[:, :], og[p][1][:, :], og[p][2][:, :], ALU.add),
                     reads=[f"og{p}_1", f"og{p}_2"], writes=["acc2"])
                T.op("dve", lambda e: e.tensor_tensor(a_[:, :], a_[:, :], og[p][3][:, :], ALU.add), reads=[ar, f"og{p}_3"], writes=[ar])
                T.op("dve", lambda e: e.tensor_tensor(a_[:, :], a_[:, :], acc2[:, :], ALU.add), reads=[ar, "acc2"], writes=[ar])
                for n in range(4):
                    T.op("dve", lambda e, n=n: e.bn_stats(st2[:, n, :], a_[:, n * 512:(n + 1) * 512]), reads=[ar], writes=["st2"])
                T.op("dve", lambda e: e.bn_aggr(mvt[:, 0:2], st2[:, :, :].rearrange("p a b -> p (a b)")), reads=["st2"], writes=[mvr])
                T.op("act", lambda e: e.activation(mvt[:, 2:3], mvt[:, 1:2], AF.Ln, bias=epsT[:, 0:1], scale=1.0),
                     reads=[mvr, "epsT"], writes=[mvr])
                T.op("act", lambda e: e.activation(mvt[:, 3:4], mvt[:, 2:3], AF.Exp, scale=-0.5), reads=[mvr], writes=[mvr])

            def cB(i):
                p = i % 2
                mvt, mvr = mv2s[i % 4], f"mv2_{i % 4}"
                a_, ar = acc[p], f"acc{p}"
                y_, yr = yy[p], f"yy{p}"
                T.op("dve", lambda e: e.tensor_scalar(mvt[:, 4:5], mvt[:, 0:1], -1.0, mvt[:, 3:4], ALU.mult, ALU.mult),
                     reads=[mvr], writes=[mvr])
                T.op("act", lambda e: e.activation(y_[:, :], a_[:, :], AF.Identity, bias=mvt[:, 4:5], scale=mvt[:, 3:4]),
                     reads=[ar, mvr], writes=[yr])

            def cC(i):
                p = i % 2
                y_, yr = yy[p], f"yy{p}"
                T.op("dve", lambda e: e.tensor_tensor(y_[:, :], y_[:, :], ln2[:, 0, :], ALU.mult), reads=[yr, "ln2"], writes=[yr])
                T.op("dve", lambda e: e.tensor_tensor(y_[:, :], y_[:, :], ln2[:, 1, :], ALU.add), reads=[yr, "ln2"], writes=[yr])
                T.dma("sp", f"out{p}", lambda q: q.dma_start(out=out[i * 128:(i + 1) * 128, :], in_=y_[:, :]),
                      reads=[yr], writes=["out"], order_writes=False)

            cL(0)
            for t in range(NTB + 2):
                if t + 1 < NTB:
                    cL(t + 1)
                if t < NTB:
                    cA(t)
                if 0 <= t - 1 < NTB:
                    cB(t - 1)
                if 0 <= t - 2 < NTB:
                    cC(t - 2)
            T.barrier()
    return nc


def prep_shared(inp, CAP):
    f = lambda a: np.ascontiguousarray(np.asarray(a, dtype=np.float32))
    rep = lambda vec: np.broadcast_to(np.asarray(vec, np.float32)[None, :], (128, len(vec)))
    F = inp["w_down"].shape[2]
    KF = F // 128
    d = {}
    d["w_in"] = f(inp["w_in"][0])
    d["cw"] = f(np.transpose(np.asarray(inp["conv_w"][0]).reshape(3, 8, 128), (2, 1, 0)))
    d["w_a"] = f(inp["w_a_out"][0])
    d["w_b"] = f(inp["w_b_out"][0])
    d["lnv"] = f(np.stack([rep(inp["ln_v_g"][0]), rep(inp["ln_v_b"][0])], axis=1))
    d["wsT"] = f(np.transpose(np.asarray(inp["w_s"][0]), (2, 0, 1)))
    d["bsb"] = f(np.broadcast_to(np.asarray(inp["b_s"][0])[None], (128, 8, 128)))
    d["bg"] = f(np.asarray(inp["b_gate"][0]).reshape(32, 128).T)
    d["w_o"] = f(inp["w_o"][0])
    d["ln1"] = f(np.stack([rep(inp["ln1_g"][0]), rep(inp["ln1_b"][0])], axis=1))
    d["ln2"] = f(np.stack([rep(inp["ln2_g"][0]), rep(inp["ln2_b"][0])], axis=1))
    d["wr"] = f(np.transpose(np.asarray(inp["w_router"][0]).reshape(KD, 128, E), (1, 0, 2)))
    d["brb"] = f(rep(inp["b_router"][0]))
    d["w_up"] = f(np.asarray(inp["w_up"][0]).reshape(E * D, 2 * F))
    d["bup"] = f(np.transpose(np.asarray(inp["b_up"][0]).reshape(E, 2 * KF, 128), (2, 0, 1)))
    d["w_dn"] = f(np.asarray(inp["w_down"][0]).reshape(E * F, D))
    d["b_dn"] = f(inp["b_down"][0])
    cst = np.zeros((128, 256 + E), np.float32)
    cst[:, 0:128] = np.eye(128, dtype=np.float32)
    cst[:, 128:256] = np.triu(np.ones((128, 128), np.float32), k=1)
    cst[:, 256:] = (np.arange(E, dtype=np.float32) * CAP)[None, :]
    d["cst"] = cst
    return d


def run(inp, n_cores, CAP):
    x = np.asarray(inp["x"], dtype=np.float32)
    B, S, _ = x.shape
    xf = x.reshape(B * S, D)
    NT = (B * S) // n_cores
    F = inp["w_down"].shape[2]
    shared = prep_shared(inp, CAP)
    nc = build_nc(NT, F, CAP)
    in_maps = []
    for c in range(n_cores):
        t0 = c * NT
        m = dict(shared)
        m["xs"] = np.ascontiguousarray(xf[t0:t0 + NT])
        halo = np.zeros((2, D), np.float32)
        if t0 % S != 0:
            halo = xf[t0 - 2:t0]
        m["xhT"] = np.ascontiguousarray(np.transpose(halo.reshape(2, KD, 128), (2, 1, 0)))
        in_maps.append(m)
    res = run_bass_kernel_spmd(nc, in_maps, core_ids=list(range(n_cores)))
    outs = [np.asarray(res.results[c]["out"]) for c in range(n_cores)]
    return np.concatenate(outs, axis=0).reshape(B, S, D).astype(np.float32)


def kernel(**inputs):
    return run(inputs, 8, 640)
```

```python
from contextlib import ExitStack
import numpy as np
import concourse.bass as bass
import concourse.mybir as mybir
from concourse.bass_utils import run_bass_kernel_spmd

F32 = mybir.dt.float32
BF16 = mybir.dt.bfloat16
I32 = mybir.dt.int32
AF = mybir.ActivationFunctionType
ALU = mybir.AluOpType

D = 2048
KD = D // 128
CW = 1024
E = 32
TOPK = 4
LN_EPS = 1e-5
ALPHA = (2.0 * 1) ** 0.25
SW_ALPHA = 1.702
SW_LIM = 7.0
IN_COLS = 3 * CW + 2 * CW + 2 * D


class Tracker:
    def __init__(self, nc, stack):
        self.nc = nc
        self.stack = stack
        self.eng = {"pe": nc.tensor, "act": nc.scalar, "dve": nc.vector, "pool": nc.gpsimd, "sp": nc.sync}
        self.sem = {}
        self.cnt = {}
        for e in ("pe", "act", "dve", "pool"):
            self.sem[e] = stack.enter_context(nc.semaphore("sem_" + e))
            self.cnt[e] = 0
        self.waited = {e: {} for e in self.eng}
        self.lastw = {}
        self.readers = {}
        self.chan = {}

    def _deps(self, reads, writes, order_writes=True):
        deps = []
        for r in reads:
            deps.extend(self.lastw.get(r, []))
        for w in writes:
            if order_writes:
                deps.extend(self.lastw.get(w, []))
            deps.extend(self.readers.get(w, []))
        return deps

    def _wait(self, e, deps):
        best = {}
        for (key, sem, val) in deps:
            if key not in best or best[key][1] < val:
                best[key] = (sem, val)
        for key, (sem, val) in best.items():
            if self.waited[e].get(key, 0) < val:
                self.eng[e].wait_ge(sem, val)
                self.waited[e][key] = val

    def _record(self, tok, reads, writes, order_writes=True):
        for r in reads:
            self.readers.setdefault(r, []).append(tok)
        for w in writes:
            if order_writes:
                self.lastw[w] = [tok]
                self.readers[w] = []
            else:
                self.lastw.setdefault(w, []).append(tok)

    def op(self, e, fn, reads=(), writes=()):
        self._wait(e, self._deps(reads, writes))
        ins = fn(self.eng[e])
        self.cnt[e] += 1
        ins.then_inc(self.sem[e], 1)
        tok = (e, self.sem[e], self.cnt[e])
        self._record(tok, reads, writes)
        return tok

    def mm_group(self, mms, reads_list, writes):
        tok = ("pe", self.sem["pe"], self.cnt["pe"] + 1)
        n = len(mms)
        for i, fn in enumerate(mms):
            self._wait("pe", self._deps(reads_list[i], writes if i == 0 else ()))
            ins = fn(self.eng["pe"])
            if i == n - 1:
                ins.then_inc(self.sem["pe"], 1)
        self.cnt["pe"] += 1
        allreads = set()
        for r in reads_list:
            allreads.update(r)
        self._record(tok, list(allreads), writes)
        return tok

    def dma(self, q, chan, fn, reads=(), writes=(), order_writes=True):
        if chan not in self.chan:
            self.chan[chan] = [self.stack.enter_context(self.nc.semaphore("ds_" + chan)), 0]
        self._wait(q, self._deps(reads, writes, order_writes))
        ins = fn(self.eng[q])
        c = self.chan[chan]
        c[1] += 16
        ins.then_inc(c[0], 16)
        tok = ("d_" + chan, c[0], c[1])
        self._record(tok, reads, writes, order_writes)
        return tok

    def barrier(self):
        toks = [(e, self.sem[e], self.cnt[e]) for e in self.sem if self.cnt[e] > 0]
        toks += [("d_" + c, sv[0], sv[1]) for c, sv in self.chan.items() if sv[1] > 0]
        for e in self.eng:
            self._wait(e, toks)

    def wait_all(self, e, resources):
        deps = []
        for r in resources:
            deps.extend(self.lastw.get(r, []))
            deps.extend(self.readers.get(r, []))
        self._wait(e, deps)


def build_nc(NT, F, CAP):
    NTT = NT // 512
    NTB = NT // 128
    KF = F // 128
    NB = CAP // 128
    NSLOT = E * CAP
    colr = [(0, min(CAP, 512))] + ([(512, CAP)] if CAP > 512 else [])

    nc = bass.Bass("TRN2", target_bir_lowering=False)

    def din(name, shape, dt=F32):
        return nc.dram_tensor(name, list(shape), dt, kind="ExternalInput").ap()

    xs = din("xs", [NT, D])
    xhT = din("xhT", [128, KD, 2])
    w_in = din("w_in", [D, IN_COLS])
    cw_d = din("cw", [128, 8, 3])
    w_a = din("w_a", [CW, D])
    w_b = din("w_b", [CW, D])
    lnv_d = din("lnv", [128, 2, CW])
    wsT_d = din("wsT", [128, 8, 128])
    bsb_d = din("bsb", [128, 8, 128])
    bg_d = din("bg", [128, 32])
    w_o = din("w_o", [D, D])
    ln1_d = din("ln1", [128, 2, D])
    ln2_d = din("ln2", [128, 2, D])
    wr_d = din("wr", [128, KD, E])
    brb_d = din("brb", [128, E])
    w_up = din("w_up", [E * D, 2 * F])
    bup_d = din("bup", [128, E, 2 * KF])
    w_dn = din("w_dn", [E * F, D])
    b_dn = din("b_dn", [E, D])
    cst_d = din("cst", [128, 128 + 128 + E])
    out = nc.dram_tensor("out", [NT, D], F32, kind="ExternalOutput").ap()

    MT = nc.dram_tensor("MT", [NTT, 128, KD * 512], BF16, kind="Internal").ap()
    X1 = nc.dram_tensor("X1", [NT + 1, D + E], F32, kind="Internal").ap()
    TT = nc.dram_tensor("TT", [NSLOT + 1, 2], I32, kind="Internal").ap()
    OD = nc.dram_tensor("OD", [NSLOT + 1, D], F32, kind="Internal").ap()

    with ExitStack() as top:
        T = Tracker(nc, top)
        sb = lambda st, name, shape, dt=F32: st.enter_context(nc.sbuf_tensor("s_" + name, list(shape), dt))
        ps_ = lambda st, name, shape, dt=F32: st.enter_context(nc.psum_tensor("p_" + name, list(shape), dt))

        cst = sb(top, "cst", [128, 128 + 128 + E])
        idb = sb(top, "idb", [128, 128], BF16)
        ones = sb(top, "ones", [128, 128])
        epsT = sb(top, "epsT", [128, 1])
        slots_all = sb(top, "slots_all", [128, NTB, TOPK], I32)
        tokid = sb(top, "tokid", [128, NTB, 2], I32)
        T.dma("sp", "c_cst", lambda q: q.dma_start(out=cst[:, :], in_=cst_d[:, :]), writes=["cst"])
        T.op("act", lambda e: e.copy(idb[:, :], cst[:, 0:128]), reads=["cst"], writes=["idb"])
        T.op("pool", lambda e: e.memset(ones[:, :], 1.0), writes=["ones"])
        T.op("pool", lambda e: e.memset(epsT[:, :], LN_EPS), writes=["epsT"])
        T.op("pool", lambda e: e.iota(tokid[:, :, :], pattern=[[128, NTB], [0, 2]], base=0, channel_multiplier=1),
             writes=["tokid"])
        idf = cst[:, 0:128]
        Uf = cst[:, 128:256]
        ecap = cst[:, 256:256 + E]

        with ExitStack() as st0:
            zrow = sb(st0, "zrow", [1, D + E])
            tinit = sb(st0, "tinit", [128, NSLOT * 2 // 128], I32)
            T.op("pool", lambda e: e.memset(zrow[:, :], 0.0), writes=["zrow"])
            T.op("pool", lambda e: e.memset(tinit[:, :], NT), writes=["tinit"])
            T.dma("sp", "i_x1", lambda q: q.dma_start(out=X1[NT:NT + 1, :], in_=zrow[:, :]), reads=["zrow"], writes=["X1z"])
            T.dma("sp", "i_od", lambda q: q.dma_start(out=OD[NSLOT:NSLOT + 1, :], in_=zrow[:, 0:D]), reads=["zrow"], writes=["ODz"])
            T.dma("sp", "i_tt", lambda q: q.dma_start(out=TT[0:NSLOT, :].rearrange("(p a) b -> p (a b)", p=128), in_=tinit[:, :]),
                  reads=["tinit"], writes=["TTinit"])
            T.barrier()

        with ExitStack() as st:
            NW = 5
            wp = [sb(st, f"wpA{i}", [128, 16, 512], BF16) for i in range(NW)]
            wctr = [0]

            def wload(src2d, K):
                s = wctr[0] % NW
                wctr[0] += 1
                T.dma("pool", f"wpA{s}",
                      lambda q: q.dma_start(out=wp[s][:, 0:K, :], in_=src2d.rearrange("(k p) c -> p k c", p=128)),
                      writes=[f"wpA{s}"])
                return wp[s], f"wpA{s}"

            NPA = 5
            psb = [ps_(st, f"psA{i}", [128, 512]) for i in range(NPA)]
            psH = ps_(st, "psH", [128, 512])
            psTA = [ps_(st, f"psTA{i}", [128, 8, 128], BF16) for i in range(2)]
            tctrA = [0]
            pctr = [0]

            def nextps():
                i = pctr[0] % NPA
                pctr[0] += 1
                return psb[i], f"psA{i}"

            xbs = [sb(st, f"xb{i}", [128, D], BF16) for i in range(2)]
            xissued = set()

            def xload(blk):
                if blk in xissued or blk >= NTB:
                    return
                xissued.add(blk)
                bb = blk % 2
                T.dma("pool", f"xb{bb}", lambda q: q.dma_start(out=xbs[bb][:, :], in_=xs[blk * 128:(blk + 1) * 128, :]),
                      writes=[f"xb{bb}"])
            xT = sb(st, "xT", [128, KD, 514], BF16)
            cw = sb(st, "cw", [128, 8, 3])
            lnv = sb(st, "lnv", [128, 2, CW])
            wsT = sb(st, "wsT", [128, 8, 128], BF16)
            bsb = sb(st, "bsb", [128, 8, 128])
            bg = sb(st, "bg", [128, 32])
            pre_sb = sb(st, "pre_sb", [128, 514])
            zb = sb(st, "zb", [128, 514])
            zc = sb(st, "zc", [128, 8, 2])
            c1 = sb(st, "c1", [128, 512])
            c2 = sb(st, "c2", [128, 512])
            actA = sb(st, "actA", [128, 8, 512], BF16)
            actB = sb(st, "actB", [128, 8, 512], BF16)
            u = sb(st, "u", [128, 8, 512], BF16)
            v = sb(st, "v", [128, 4, CW], BF16)
            vg = sb(st, "vg", [128, CW])
            stt = sb(st, "stt", [128, 4, 6])
            mv = sb(st, "mv", [128, 8])
            tmpB = sb(st, "tmpB", [128, 512])
            gsb = [sb(st, f"gsb{i}", [128, 512]) for i in range(2)]
            m2 = sb(st, "m2", [128, 512])
            mtmp = sb(st, "mtmp", [128, 4, 512])
            mst = [sb(st, f"mst{i}", [128, 4, 512], BF16) for i in range(2)]

            T.dma("sp", "c_cw", lambda q: q.dma_start(out=cw[:, :, :], in_=cw_d[:, :, :]), writes=["cw"])
            T.dma("sp", "c_lnv", lambda q: q.dma_start(out=lnv[:, :, :], in_=lnv_d[:, :, :]), writes=["lnv"])
            T.dma("sp", "c_bsb", lambda q: q.dma_start(out=bsb[:, :, :], in_=bsb_d[:, :, :]), writes=["bsb"])
            T.dma("sp", "c_bg", lambda q: q.dma_start(out=bg[:, :], in_=bg_d[:, :]), writes=["bg"])
            T.dma("pool", "c_ws", lambda q: q.dma_start(out=wsT[:, :, :], in_=wsT_d[:, :, :]), writes=["wsT"])
            T.op("pool", lambda e: e.memset(wsT[64:128, :, 0:64], 0.0), reads=["wsT"], writes=["wsT"])

            mctr = [0]
            conv_pref = [None]
            for ti in range(NTT):
                if ti == 0:
                    T.dma("pool", "c_xh", lambda q: q.dma_start(out=xT[:, :, 0:2], in_=xhT[:, :, :]), writes=["xT"])
                for b in range(4):
                    blk = ti * 4 + b
                    xload(blk)
                    xb, xbr = xbs[blk % 2], f"xb{blk % 2}"
                    for half in range(2):
                        tp = tctrA[0] % 2
                        tctrA[0] += 1
                        psT, ptr_ = psTA[tp], f"psTA{tp}"
                        mms = [(lambda pe, a=a, half=half: pe.transpose(psT[:, a, :], xb[:, (half * 8 + a) * 128:(half * 8 + a + 1) * 128], idb[:, :]))
                               for a in range(8)]
                        T.mm_group(mms, [[xbr, "idb"]] * 8, [ptr_])
                        if half == 0:
                            T.op("dve", lambda e: e.tensor_copy(xT[:, 0:8, 2 + b * 128:2 + (b + 1) * 128], psT[:, :, :]), reads=[ptr_], writes=["xT"])
                        else:
                            T.op("act", lambda e: e.copy(xT[:, 8:16, 2 + b * 128:2 + (b + 1) * 128], psT[:, :, :]), reads=[ptr_], writes=["xT"])
                    if b + 2 < 4:
                        xload(blk + 2)

                def lin_fm(W, wres, cc, rhs_of_k, K, rres, pst, pres, c0=0, c1_=512):
                    mms = [(lambda pe, k=k: pe.matmul(pst[:, c0:c1_], lhsT=W[:, k, cc * 128:(cc + 1) * 128], rhs=rhs_of_k(k),
                                                      start=(k == 0), stop=(k == K - 1))) for k in range(K)]
                    T.mm_group(mms, [[wres] + rres] * K, [pres])

                for h in range(2):
                    if h == 0 and conv_pref[0] is not None:
                        (Wpre, rpre), (Whid, rhid), (Wpost, rpost) = conv_pref[0]
                        conv_pref[0] = None
                    else:
                        Wpre, rpre = wload(w_in[:, h * 512:(h + 1) * 512], KD)
                        Whid, rhid = wload(w_in[:, CW + h * 512:CW + (h + 1) * 512], KD)
                        Wpost, rpost = wload(w_in[:, 2 * CW + h * 512:2 * CW + (h + 1) * 512], KD)
                    for cc in range(4):
                        j = 4 * h + cc
                        pP, rP = nextps()
                        pH, rH = nextps()
                        pO, rO = nextps()
                        pX, rX = psH, "psH"
                        main = lambda k: xT[:, k, 2:514]
                        halo = lambda k: xT[:, k, 0:2]
                        lin_fm(Wpre, rpre, cc, main, KD, ["xT"], pP, rP)
                        if ti == 0:
                            lin_fm(Wpre, rpre, cc, halo, KD, ["xT"], pX, rX, 0, 2)
                        lin_fm(Whid, rhid, cc, main, KD, ["xT"], pH, rH)
                        if ti == 0:
                            lin_fm(Whid, rhid, cc, halo, KD, ["xT"], pX, rX, 2, 4)
                        lin_fm(Wpost, rpost, cc, main, KD, ["xT"], pO, rO)
                        T.op("act", lambda e: e.copy(pre_sb[:, 2:514], pP[:, :]), reads=[rP], writes=["pre_sb"])
                        if ti == 0:
                            T.op("act", lambda e: e.copy(pre_sb[:, 0:2], pX[:, 0:2]), reads=[rX], writes=["pre_sb"])
                            T.op("dve", lambda e: e.tensor_tensor(zb[:, 0:2], pre_sb[:, 0:2], pX[:, 2:4], ALU.mult),
                                 reads=["pre_sb", rX], writes=["zb"])
                        else:
                            T.op("dve", lambda e: e.tensor_copy(zb[:, 0:2], zc[:, j, :]), reads=["zc"], writes=["zb"])
                        T.op("dve", lambda e: e.tensor_tensor(zb[:, 2:514], pre_sb[:, 2:514], pH[:, :], ALU.mult),
                             reads=["pre_sb", rH], writes=["zb"])
                        T.op("dve", lambda e: e.tensor_copy(zc[:, j, :], zb[:, 512:514]), reads=["zb"], writes=["zc"])
                        T.op("dve", lambda e: e.tensor_scalar(c1[:, :], zb[:, 0:512], cw[:, j, 0:1], None, ALU.mult),
                             reads=["zb", "cw"], writes=["c1"])
                        T.op("dve", lambda e: e.scalar_tensor_tensor(c2[:, :], zb[:, 1:513], cw[:, j, 1:2], c1[:, :], ALU.mult, ALU.add),
                             reads=["zb", "cw", "c1"], writes=["c2"])
                        T.op("dve", lambda e: e.scalar_tensor_tensor(c1[:, :], zb[:, 2:514], cw[:, j, 2:3], c2[:, :], ALU.mult, ALU.add),
                             reads=["zb", "cw", "c2"], writes=["c1"])
                        T.op("dve", lambda e: e.tensor_tensor(actA[:, j, :], c1[:, :], pO[:, :], ALU.mult),
                             reads=["c1", rO], writes=["actA"])

                Wv = [wload(w_in[:, 4 * CW + hh * 512:4 * CW + (hh + 1) * 512], KD) for hh in range(2)]
                for b in range(4):
                    for hh in range(2):
                        pV, rV = nextps()
                        Wt, rw = Wv[hh]
                        mms = [(lambda pe, k=k: pe.matmul(pV[:, :], lhsT=xT[:, k, 2 + b * 128:2 + (b + 1) * 128], rhs=Wt[:, k, :],
                                                          start=(k == 0), stop=(k == KD - 1))) for k in range(KD)]
                        T.mm_group(mms, [["xT", rw]] * KD, [rV])
                        T.op("act", lambda e: e.activation(vg[:, hh * 512:(hh + 1) * 512], pV[:, :], AF.Gelu), reads=[rV], writes=["vg"])
                    for hh in range(2):
                        T.op("dve", lambda e, hh=hh: e.bn_stats(stt[:, hh, :], vg[:, hh * 512:(hh + 1) * 512]), reads=["vg"], writes=["stt"])
                    T.op("dve", lambda e: e.bn_aggr(mv[:, 0:2], stt[:, 0:2, :].rearrange("p a b -> p (a b)")), reads=["stt"], writes=["mv"])
                    T.op("act", lambda e: e.activation(mv[:, 2:3], mv[:, 1:2], AF.Sqrt, bias=epsT[:, 0:1], scale=1.0),
                         reads=["mv", "epsT"], writes=["mv"])
                    T.op("dve", lambda e: e.reciprocal(mv[:, 3:4], mv[:, 2:3]), reads=["mv"], writes=["mv"])
                    T.op("dve", lambda e: e.tensor_scalar(vg[:, :], vg[:, :], mv[:, 0:1], mv[:, 3:4], ALU.subtract, ALU.mult),
                         reads=["vg", "mv"], writes=["vg"])
                    T.op("dve", lambda e: e.tensor_tensor(vg[:, :], vg[:, :], lnv[:, 0, :], ALU.mult), reads=["vg", "lnv"], writes=["vg"])
                    T.op("dve", lambda e: e.tensor_tensor(v[:, b, :], vg[:, :], lnv[:, 1, :], ALU.add), reads=["vg", "lnv"], writes=["v"])

                for h in range(2):
                    Wu, ru = wload(w_in[:, 3 * CW + h * 512:3 * CW + (h + 1) * 512], KD)
                    for cc in range(4):
                        j = 4 * h + cc
                        pU, rU = nextps()
                        lin_fm(Wu, ru, cc, lambda k: xT[:, k, 2:514], KD, ["xT"], pU, rU)
                        T.op("act", lambda e: e.activation(u[:, j, :], pU[:, :], AF.Gelu), reads=[rU], writes=["u"])

                for g in range(8):
                    pS, rS = nextps()
                    mms = [(lambda pe, b=b: pe.matmul(pS[:, b * 128:(b + 1) * 128], lhsT=v[:, b, g * 128:(g + 1) * 128], rhs=wsT[:, g, :],
                                                      start=True, stop=True)) for b in range(4)]
                    T.mm_group(mms, [["v", "wsT"]] * 4, [rS])
                    for b in range(4):
                        T.op("dve", lambda e, b=b: e.tensor_tensor(tmpB[:, b * 128:(b + 1) * 128], pS[:, b * 128:(b + 1) * 128], bsb[:, g, :], ALU.add),
                             reads=[rS, "bsb"], writes=["tmpB"])
                    T.op("dve", lambda e: e.tensor_tensor(actB[:, g, :], tmpB[:, :], u[:, g, :], ALU.mult),
                         reads=["tmpB", "u"], writes=["actB"])

                xload((ti + 1) * 4)
                xload((ti + 1) * 4 + 1)
                for q in range(4):
                    ms = mst[mctr[0] % 2]
                    mres = f"mst{mctr[0] % 2}"
                    for br in range(2):
                        Wg, rg = wload(w_in[:, 5 * CW + br * D + q * 512:5 * CW + br * D + (q + 1) * 512], KD)
                        Wy, ry = wload((w_a if br == 0 else w_b)[:, q * 512:(q + 1) * 512], 8)
                        act_in, ares = (actA, "actA") if br == 0 else (actB, "actB")
                        for cc in range(4):
                            c = 4 * q + cc
                            pG, rG = nextps()
                            pY, rY = nextps()
                            lin_fm(Wg, rg, cc, lambda k: xT[:, k, 2:514], KD, ["xT"], pG, rG)
                            lin_fm(Wy, ry, cc, lambda k: act_in[:, k, :], 8, [ares], pY, rY)
                            gs = gsb[(c + br) % 2]
                            gres = f"gsb{(c + br) % 2}"
                            T.op("act", lambda e: e.activation(gs[:, :], pG[:, :], AF.Sigmoid, bias=bg[:, br * 16 + c:br * 16 + c + 1], scale=1.0),
                                 reads=[rG, "bg"], writes=[gres])
                            if br == 0:
                                T.op("dve", lambda e: e.tensor_tensor(mtmp[:, cc, :], gs[:, :], pY[:, :], ALU.mult),
                                     reads=[gres, rY], writes=["mtmp"])
                            else:
                                T.op("dve", lambda e: e.tensor_tensor(m2[:, :], gs[:, :], pY[:, :], ALU.mult),
                                     reads=[gres, rY], writes=["m2"])
                                T.op("dve", lambda e: e.tensor_tensor(ms[:, cc, :], mtmp[:, cc, :], m2[:, :], ALU.add),
                                     reads=["mtmp", "m2"], writes=[mres])
                    T.dma("sp", mres, lambda q_, q=q, ti=ti: q_.dma_start(
                        out=MT[ti, :, q * 4 * 512:(q + 1) * 4 * 512], in_=ms[:, :, :].rearrange("p a b -> p (a b)")),
                        reads=[mres], writes=["MT"], order_writes=False)
                    mctr[0] += 1
                if ti + 1 < NTT:
                    conv_pref[0] = [wload(w_in[:, 0:512], KD), wload(w_in[:, CW:CW + 512], KD), wload(w_in[:, 2 * CW:2 * CW + 512], KD)]
            T.barrier()

        with ExitStack() as st:
            wo = sb(st, "wo", [128, KD, D], BF16)
            for n in range(4):
                T.dma("pool", f"wo{n}", lambda q, n=n: q.dma_start(
                    out=wo[:, :, n * 512:(n + 1) * 512], in_=w_o[:, n * 512:(n + 1) * 512].rearrange("(k p) c -> p k c", p=128)),
                    writes=[f"wo{n}"])
            ln1 = sb(st, "ln1", [128, 2, D])
            wr = sb(st, "wr", [128, KD, E])
            brb = sb(st, "brb", [128, E])
            T.dma("sp", "c_ln1", lambda q: q.dma_start(out=ln1[:, :, :], in_=ln1_d[:, :, :]), writes=["ln1"])
            T.dma("sp", "c_wr", lambda q: q.dma_start(out=wr[:, :, :], in_=wr_d[:, :, :]), writes=["wr"])
            T.dma("sp", "c_brb", lambda q: q.dma_start(out=brb[:, :], in_=brb_d[:, :]), writes=["brb"])
            mt = [sb(st, f"mt{i}", [128, KD, 512], BF16) for i in range(2)]
            xin2 = [sb(st, f"xin2_{i}", [128, D]) for i in range(3)]
            r = [sb(st, f"r{i}", [128, D]) for i in range(2)]
            xrow = [sb(st, f"xrow{i}", [128, D + E]) for i in range(2)]
            x1T = sb(st, "x1T", [128, KD, 128])
            st1 = sb(st, "st1", [128, 4, 6])
            mv1 = sb(st, "mv1", [128, 8])
            lg = sb(st, "lg", [128, E])
            top8 = sb(st, "top8", [128, 8])
            mask = sb(st, "mask", [128, E])
            ex = sb(st, "ex", [128, E])
            sm = sb(st, "sm", [128, 4])
            run = sb(st, "run", [128, E])
            aa = sb(st, "aa", [128, E])
            ok = sb(st, "ok", [128, E])
            svn = sb(st, "svn", [128, E])
            top8s = sb(st, "top8s", [128, 8])
            psm = [ps_(st, f"psM{i}", [128, 512]) for i in range(4)]
            pst = [ps_(st, f"psX{i}", [128, 4, 128]) for i in range(2)]
            psl = ps_(st, "psL", [128, 512])
            psc = ps_(st, "psC", [128, 512])
            T.op("pool", lambda e: e.memset(run[:, :], 0.0), writes=["run"])
            BIG = float(NSLOT)

            negcap = sb(st, "negcap", [128, E])
            T.op("dve", lambda e: e.tensor_scalar(negcap[:, :], ecap, -1.0, BIG, ALU.mult, ALU.add), reads=["cst"], writes=["negcap"])

            def load_mt(ti):
                mtt, mtr = mt[ti % 2], f"mt{ti % 2}"
                T.dma("sp", mtr, lambda q: q.dma_start(out=mtt[:, :, :].rearrange("p a b -> p (a b)"), in_=MT[ti, :, :]),
                      reads=["MT"], writes=[mtr])

            def loads(blk):
                par = blk % 3
                xi, xir = xin2[par], f"xin2_{par}"
                T.dma("sp", xir, lambda q: q.dma_start(out=xi[:, :], in_=xs[blk * 128:(blk + 1) * 128, :]), writes=[xir])

            def s1a(blk):
                ti, b = blk // 4, blk % 4
                mtt, mtr = mt[ti % 2], f"mt{ti % 2}"
                par = blk % 2
                xi, xir = xin2[blk % 3], f"xin2_{blk % 3}"
                rr, rres = r[par], f"r{par}"
                for n in range(4):
                    mms = [(lambda pe, k=k: pe.matmul(psm[n][:, :], lhsT=mtt[:, k, b * 128:(b + 1) * 128], rhs=wo[:, k, n * 512:(n + 1) * 512],
                                                      start=(k == 0), stop=(k == KD - 1))) for k in range(KD)]
                    T.mm_group(mms, [[mtr, f"wo{n}"]] * KD, [f"psM{n}"])
                    T.op("dve", lambda e, n=n: e.scalar_tensor_tensor(rr[:, n * 512:(n + 1) * 512], xi[:, n * 512:(n + 1) * 512], ALPHA,
                                                                     psm[n][:, :], ALU.mult, ALU.add),
                         reads=[xir, f"psM{n}"], writes=[rres])

            def s1b_stats(blk):
                par = blk % 2
                rr, rres = r[par], f"r{par}"
                for n in range(4):
                    T.op("dve", lambda e, n=n: e.bn_stats(st1[:, n, :], rr[:, n * 512:(n + 1) * 512]), reads=[rres], writes=["st1"])
                T.op("dve", lambda e: e.bn_aggr(mv1[:, 0:2], st1[:, :, :].rearrange("p a b -> p (a b)")), reads=["st1"], writes=["mv1"])
                T.op("act", lambda e: e.activation(mv1[:, 2:3], mv1[:, 1:2], AF.Ln, bias=epsT[:, 0:1], scale=1.0),
                     reads=["mv1", "epsT"], writes=["mv1"])
                T.op("act", lambda e: e.activation(mv1[:, 3:4], mv1[:, 2:3], AF.Exp, scale=-0.5), reads=["mv1"], writes=["mv1"])

            def s1b_norm(blk):
                par = blk % 2
                rr, rres = r[par], f"r{par}"
                xr, xres = xrow[par], f"xrow{par}"
                T.op("dve", lambda e: e.tensor_scalar(mv1[:, 4:5], mv1[:, 0:1], -1.0, mv1[:, 3:4], ALU.mult, ALU.mult),
                     reads=["mv1"], writes=["mv1"])
                T.op("act", lambda e: e.activation(rr[:, :], rr[:, :], AF.Identity, bias=mv1[:, 4:5], scale=mv1[:, 3:4]),
                     reads=[rres, "mv1"], writes=[rres])
                T.op("dve", lambda e: e.tensor_tensor(rr[:, :], rr[:, :], ln1[:, 0, :], ALU.mult), reads=[rres, "ln1"], writes=[rres])
                T.op("dve", lambda e: e.tensor_tensor(xr[:, 0:D], rr[:, :], ln1[:, 1, :], ALU.add), reads=[rres, "ln1"], writes=[xres])

            def s2(blk):
                par = blk % 2
                xr, xres = xrow[par], f"xrow{par}"
                for qd in range(4):
                    pt, ptr = pst[qd % 2], f"psX{qd % 2}"
                    mms = [(lambda pe, a=a: pe.transpose(pt[:, a, :], xr[:, (qd * 4 + a) * 128:(qd * 4 + a + 1) * 128], idf))
                           for a in range(4)]
                    T.mm_group(mms, [[xres, "cst"]] * 4, [ptr])
                    T.op("act", lambda e: e.copy(x1T[:, qd * 4:(qd + 1) * 4, :], pt[:, :, :]), reads=[ptr], writes=["x1T"])
                mms = [(lambda pe, k=k: pe.matmul(psl[:, 0:E], lhsT=x1T[:, k, :], rhs=wr[:, k, :], start=(k == 0), stop=(k == KD - 1)))
                       for k in range(KD)]
                T.mm_group(mms, [["x1T", "wr"]] * KD, ["psL"])

            def s3a(blk):
                T.op("dve", lambda e: e.tensor_tensor(lg[:, :], psl[:, 0:E], brb[:, :], ALU.add), reads=["psL", "brb"], writes=["lg"])
                T.op("dve", lambda e: e.max(out=top8[:, :], in_=lg[:, :]), reads=["lg"], writes=["top8"])
                T.op("dve", lambda e: e.tensor_scalar(mask[:, :], lg[:, :], top8[:, 3:4], None, ALU.is_ge), reads=["lg", "top8"], writes=["mask"])
                T.op("dve", lambda e: e.tensor_scalar(sm[:, 0:1], top8[:, 0:1], -1.0, None, ALU.mult), reads=["top8"], writes=["sm"])
                T.op("act", lambda e: e.activation(ex[:, :], lg[:, :], AF.Exp, bias=sm[:, 0:1], scale=1.0), reads=["lg", "sm"], writes=["ex"])
                mms = [lambda pe: pe.matmul(psc[:, 0:E], lhsT=Uf, rhs=mask[:, :], start=True, stop=False),
                       lambda pe: pe.matmul(psc[:, 0:E], lhsT=ones[:, :], rhs=run[:, :], start=False, stop=True)]
                T.mm_group(mms, [["cst", "mask"], ["ones", "run"]], ["psC"])

            def s3b(blk):
                par = blk % 2
                xr, xres = xrow[par], f"xrow{par}"
                T.op("dve", lambda e: e.scalar_tensor_tensor(aa[:, :], psc[:, 0:E], -1.0, negcap[:, :], ALU.mult, ALU.add),
                     reads=["psC", "negcap"], writes=["aa"])
                T.op("dve", lambda e: e.scalar_tensor_tensor(ok[:, :], psc[:, 0:E], float(CAP), mask[:, :], ALU.is_lt, ALU.mult),
                     reads=["psC", "mask"], writes=["ok"])
                T.op("dve", lambda e: e.tensor_tensor(run[:, :], run[:, :], mask[:, :], ALU.add), reads=["run", "mask"], writes=["run"])
                T.op("dve", lambda e: e.tensor_tensor(svn[:, :], aa[:, :], ok[:, :], ALU.mult), reads=["aa", "ok"], writes=["svn"])
                T.op("dve", lambda e: e.max(out=top8s[:, :], in_=svn[:, :]), reads=["svn"], writes=["top8s"])
                T.op("dve", lambda e: e.tensor_scalar(slots_all[:, blk, :], top8s[:, 0:TOPK], -1.0, BIG, ALU.mult, ALU.add),
                     reads=["top8s"], writes=[f"slots{blk % 2}"])
                for k in range(TOPK):
                    T.dma("pool", "scat", lambda q, k=k: q.indirect_dma_start(
                        out=TT[:, :], out_offset=bass.IndirectOffsetOnAxis(ap=slots_all[:, blk, k:k + 1], axis=0),
                        in_=tokid[:, blk, :], in_offset=None),
                        reads=[f"slots{blk % 2}", "tokid", "TTinit"], writes=["TT"], order_writes=False)
                T.op("dve", lambda e: e.tensor_tensor(ex[:, :], ex[:, :], mask[:, :], ALU.mult), reads=["ex", "mask"], writes=["ex"])
                T.op("dve", lambda e: e.reduce_sum(sm[:, 1:2], ex[:, :], axis=mybir.AxisListType.X), reads=["ex"], writes=["sm"])
                T.op("dve", lambda e: e.reciprocal(sm[:, 2:3], sm[:, 1:2]), reads=["sm"], writes=["sm"])
                T.op("dve", lambda e: e.tensor_scalar(xr[:, D:D + E], ex[:, :], sm[:, 2:3], None, ALU.mult), reads=["ex", "sm"], writes=[xres])
                T.dma("sp", f"x1s{par}", lambda q: q.dma_start(out=X1[blk * 128:(blk + 1) * 128, :], in_=xr[:, :]),
                      reads=[xres], writes=["X1"], order_writes=False)

            load_mt(0)
            loads(0)
            if NTB > 1:
                loads(1)
            for blk in range(NTB):
                if blk + 2 < NTB:
                    loads(blk + 2)
                if blk % 4 == 1 and blk // 4 + 1 < NTT:
                    load_mt(blk // 4 + 1)
                s1a(blk)
                if blk > 0:
                    s2(blk - 1)
                s1b_stats(blk)
                if blk > 0:
                    s3a(blk - 1)
                s1b_norm(blk)
                if blk > 0:
                    s3b(blk - 1)
            s2(NTB - 1)
            s3a(NTB - 1)
            s3b(NTB - 1)
            T.barrier()

        with ExitStack() as st:
            NW = 5
            wp = [sb(st, f"wpB{i}", [128, 16, 512], BF16) for i in range(NW)]
            wctr = [0]

            def wloadB(src2d, K):
                s = wctr[0] % NW
                wctr[0] += 1
                T.dma("pool", f"wpB{s}",
                      lambda q: q.dma_start(out=wp[s][:, 0:K, :], in_=src2d.rearrange("(k p) c -> p k c", p=128)),
                      writes=[f"wpB{s}"])
                return wp[s], f"wpB{s}"

            psb = [ps_(st, f"psB{i}", [128, 512]) for i in range(6)]
            psTs = [ps_(st, f"psTB{i}", [128, 8, 128], BF16) for i in range(2)]
            pctr = [0]
            tctr = [0]

            def nextpsB():
                i = pctr[0] % 6
                pctr[0] += 1
                return psb[i], f"psB{i}"

            bup = sb(st, "bup", [128, E, 2 * KF])
            T.dma("sp", "c_bup", lambda q: q.dma_start(out=bup[:, :, :], in_=bup_d[:, :, :]), writes=["bup"])
            idx = [sb(st, f"idx{i}", [128, NB, 2], I32) for i in range(2)]
            xg = [sb(st, f"xg{i}", [128, D + E]) for i in range(2)]
            xgbs = [sb(st, f"xgb{i}", [128, D], BF16) for i in range(NB)]
            xgT = sb(st, "xgT", [128, KD, CAP], BF16)
            gcol = [sb(st, f"gcol{i}", [128, NB]) for i in range(2)]
            hT = sb(st, "hT", [128, KF, CAP], BF16)
            gl = sb(st, "gl", [128, CAP])
            sg = sb(st, "sg", [128, CAP])
            ln_ = sb(st, "ln_", [128, CAP])
            bdn = [sb(st, f"bdn{i}", [128, D]) for i in range(2)]
            osb = [sb(st, f"osb{i}", [128, 512]) for i in range(2)]
            osc = [sb(st, f"osc{i}", [128, 512]) for i in range(2)]
            octr = [0]
            gctr = [0]

            def prep_head(e_):
                ep = e_ % 2
                ix, ixr = idx[ep], f"idx{ep}"
                bd, bdr = bdn[ep], f"bdn{ep}"
                T.dma("sp", ixr, lambda q: q.dma_start(out=ix[:, :, :], in_=TT[e_ * CAP:(e_ + 1) * CAP, :].rearrange("(j p) b -> p j b", p=128)),
                      reads=["TT", "TTinit"], writes=[ixr])
                T.dma("sp", bdr, lambda q: q.dma_start(out=bd[:, :], in_=b_dn[e_:e_ + 1, :].to_broadcast([128, D])), writes=[bdr])

            def prep_gather(e_, j):
                ep = e_ % 2
                ix, ixr = idx[ep], f"idx{ep}"
                gc, gcr = gcol[ep], f"gcol{ep}"
                gp_ = gctr[0] % 2
                gctr[0] += 1
                xgt, xgr = xg[gp_], f"xg{gp_}"
                xgb, xbr = xgbs[j], f"xgb{j}"
                T.dma("pool", xgr, lambda q: q.indirect_dma_start(
                    out=xgt[:, :], out_offset=None, in_=X1[:, :],
                    in_offset=bass.IndirectOffsetOnAxis(ap=ix[:, j, 0:1], axis=0)),
                    reads=[ixr, "X1", "X1z"], writes=[xgr])
                T.op("act", lambda e: e.copy(xgb[:, :], xgt[:, 0:D]), reads=[xgr], writes=[xbr])
                T.op("dve", lambda e: e.tensor_copy(gc[:, j:j + 1], xgt[:, D + e_:D + e_ + 1]), reads=[xgr], writes=[gcr])

            def prep_transpose(e_, j):
                xgb, xbr = xgbs[j], f"xgb{j}"
                for half in range(2):
                    tp = tctr[0] % 2
                    tctr[0] += 1
                    psT, ptr_ = psTs[tp], f"psTB{tp}"
                    mms = [(lambda pe, a=a: pe.transpose(psT[:, a, :], xgb[:, (half * 8 + a) * 128:(half * 8 + a + 1) * 128], idb[:, :]))
                           for a in range(8)]
                    T.mm_group(mms, [[xbr, "idb"]] * 8, [ptr_])
                    if half == 0:
                        T.op("dve", lambda e: e.tensor_copy(xgT[:, 0:8, j * 128:(j + 1) * 128], psT[:, :, :]), reads=[ptr_], writes=["xgT"])
                    else:
                        T.op("act", lambda e: e.copy(xgT[:, 8:16, j * 128:(j + 1) * 128], psT[:, :, :]), reads=[ptr_], writes=["xgT"])

            def up(e_, hook=None):
                for g in range(F // 512):
                    Wg_, rg_ = wloadB(w_up[e_ * D:(e_ + 1) * D, g * 512:(g + 1) * 512], KD)
                    Wl_, rl_ = wloadB(w_up[e_ * D:(e_ + 1) * D, F + g * 512:F + (g + 1) * 512], KD)
                    for cc in range(4):
                        c = 4 * g + cc
                        pA, rA = nextpsB()
                        pB, rB = nextpsB()
                        pTl, rTl = (nextpsB() if len(colr) > 1 else (None, None))
                        for part, (W_, wr_, pm) in enumerate(((Wg_, rg_, pA), (Wl_, rl_, pB))):
                            for ci, (a0, a1) in enumerate(colr):
                                if ci == 0:
                                    dst, dres = pm[:, 0:a1 - a0], (rA if part == 0 else rB)
                                else:
                                    w_ = a1 - a0
                                    dst, dres = pTl[:, part * w_:(part + 1) * w_], rTl
                                mms = [(lambda pe, k=k, dst=dst, W_=W_, a0=a0, a1=a1: pe.matmul(
                                    dst, lhsT=W_[:, k, cc * 128:(cc + 1) * 128], rhs=xgT[:, k, a0:a1],
                                    start=(k == 0), stop=(k == KD - 1))) for k in range(KD)]
                                T.mm_group(mms, [[wr_, "xgT"]] * KD, [dres])
                        for ci, (a0, a1) in enumerate(colr):
                            w_ = a1 - a0
                            srcg = pA[:, 0:w_] if ci == 0 else pTl[:, 0:w_]
                            rgs = rA if ci == 0 else rTl
                            T.op("dve", lambda e, srcg=srcg, a0=a0, a1=a1: e.tensor_scalar(
                                gl[:, a0:a1], srcg, bup[:, e_, c:c + 1], SW_LIM, ALU.add, ALU.min),
                                reads=[rgs, "bup"], writes=["gl"])
                        T.op("act", lambda e: e.activation(sg[:, :], gl[:, :], AF.Silu, scale=SW_ALPHA), reads=["gl"], writes=["sg"])
                        for ci, (a0, a1) in enumerate(colr):
                            w_ = a1 - a0
                            srcl = pB[:, 0:w_] if ci == 0 else pTl[:, w_:2 * w_]
                            rls = rB if ci == 0 else rTl
                            T.op("dve", lambda e, srcl=srcl, a0=a0, a1=a1: e.tensor_scalar(
                                ln_[:, a0:a1], srcl, bup[:, e_, KF + c:KF + c + 1], -SW_LIM, ALU.add, ALU.max),
                                reads=[rls, "bup"], writes=["ln_"])
                        T.op("dve", lambda e: e.tensor_scalar(ln_[:, :], ln_[:, :], SW_LIM, 1.0, ALU.min, ALU.add), reads=["ln_"], writes=["ln_"])
                        T.op("dve", lambda e: e.scalar_tensor_tensor(hT[:, c, :], sg[:, :], 1.0 / SW_ALPHA, ln_[:, :], ALU.mult, ALU.mult),
                             reads=["sg", "ln_"], writes=["hT"])
                        if hook is not None:
                            hook(c)

            def down(e_, hook=None):
                ep = e_ % 2
                gc, gcr = gcol[ep], f"gcol{ep}"
                bd, bdr = bdn[ep], f"bdn{ep}"
                for n in range(D // 512):
                    Wd_, rd_ = wloadB(w_dn[e_ * F:(e_ + 1) * F, n * 512:(n + 1) * 512], KF)
                    for sbk in range(NB):
                        pO, rO = nextpsB()
                        mms = [(lambda pe, k=k: pe.matmul(pO[:, :], lhsT=hT[:, k, sbk * 128:(sbk + 1) * 128], rhs=Wd_[:, k, :],
                                                          start=(k == 0), stop=(k == KF - 1))) for k in range(KF)]
                        T.mm_group(mms, [["hT", rd_]] * KF, [rO])
                        op_ = octr[0] % 2
                        octr[0] += 1
                        o1, o1r = osb[op_], f"osb{op_}"
                        o2, o2r = osc[op_], f"osc{op_}"
                        T.op("dve", lambda e: e.tensor_tensor(o1[:, :], pO[:, :], bd[:, n * 512:(n + 1) * 512], ALU.add),
                             reads=[rO, bdr], writes=[o1r])
                        T.op("act", lambda e: e.mul(o2[:, :], o1[:, :], gc[:, sbk:sbk + 1]), reads=[o1r, gcr], writes=[o2r])
                        T.dma("sp", o2r, lambda q: q.dma_start(
                            out=OD[e_ * CAP + sbk * 128:e_ * CAP + (sbk + 1) * 128, n * 512:(n + 1) * 512], in_=o2[:, :]),
                            reads=[o2r], writes=["OD"], order_writes=False)
                    if hook is not None:
                        hook(n)

            NCH = KF
            gsched = {}
            for j in range(NB):
                gsched.setdefault(min(NCH - 1, (j * NCH) // NB), []).append(j)
            NDG = D // 512
            tsched = {}
            for j in range(NB):
                tsched.setdefault(min(NDG - 1, (j * NDG) // NB), []).append(j)
            prep_head(0)
            for j in range(NB):
                prep_gather(0, j)
            for j in range(NB):
                prep_transpose(0, j)
            for e_ in range(E):
                nxt = e_ + 1
                if nxt < E:
                    prep_head(nxt)
                    up(e_, hook=lambda c: [prep_gather(nxt, j) for j in gsched.get(c, [])])
                    down(e_, hook=lambda n: [prep_transpose(nxt, j) for j in tsched.get(n, [])])
                else:
                    up(e_)
                    down(e_)
            T.barrier()

        with ExitStack() as st:
            ln2 = sb(st, "ln2", [128, 2, D])
            T.dma("sp", "c_ln2", lambda q: q.dma_start(out=ln2[:, :, :], in_=ln2_d[:, :, :]), writes=["ln2"])
            og = [[sb(st, f"og{p}_{k}", [128, D]) for k in range(TOPK)] for p in range(2)]
            xr2 = [sb(st, f"xr2_{p}", [128, D]) for p in range(2)]
            acc = [sb(st, f"acc{p}", [128, D]) for p in range(2)]
            acc2 = sb(st, "acc2", [128, D])
            yy = [sb(st, f"yy{p}", [128, D]) for p in range(2)]
            st2 = sb(st, "st2", [128, 4, 6])
            mv2s = [sb(st, f"mv2_{i}", [128, 8]) for i in range(4)]

            def cL(i):
                p = i % 2
                T.dma("sp", f"xr2_{p}", lambda q: q.dma_start(out=xr2[p][:, :], in_=X1[i * 128:(i + 1) * 128, 0:D]),
                      reads=["X1"], writes=[f"xr2_{p}"])
                for k in range(TOPK):
                    T.dma("pool", f"og{p}_{k}", lambda q, k=k: q.indirect_dma_start(
                        out=og[p][k][:, :], out_offset=None, in_=OD[:, :],
                        in_offset=bass.IndirectOffsetOnAxis(ap=slots_all[:, i, k:k + 1], axis=0)),
                        reads=["slots0", "slots1", "OD", "ODz"], writes=[f"og{p}_{k}"])

            def cA(i):
                p = i % 2
                mvt, mvr = mv2s[i % 4], f"mv2_{i % 4}"
                a_, ar = acc[p], f"acc{p}"
                T.op("dve", lambda e: e.scalar_tensor_tensor(a_[:, :], xr2[p][:, :], ALPHA, og[p][0][:, :], ALU.mult, ALU.add),
                     reads=[f"xr2_{p}", f"og{p}_0"], writes=[ar])
                T.op("dve", lambda e: e.tensor_tensor(acc2[:, :], og[p][1][:, :], og[p][2][:, :], ALU.add),
                     reads=[f"og{p}_1", f"og{p}_2"], writes=["acc2"])
                T.op("dve", lambda e: e.tensor_tensor(a_[:, :], a_[:, :], og[p][3][:, :], ALU.add), reads=[ar, f"og{p}_3"], writes=[ar])
                T.op("dve", lambda e: e.tensor_tensor(a_[:, :], a_[:, :], acc2[:, :], ALU.add), reads=[ar, "acc2"], writes=[ar])
                for n in range(4):
                    T.op("dve", lambda e, n=n: e.bn_stats(st2[:, n, :], a_[:, n * 512:(n + 1) * 512]), reads=[ar], writes=["st2"])
                T.op("dve", lambda e: e.bn_aggr(mvt[:, 0:2], st2[:, :, :].rearrange("p a b -> p (a b)")), reads=["st2"], writes=[mvr])
                T.op("act", lambda e: e.activation(mvt[:, 2:3], mvt[:, 1:2], AF.Ln, bias=epsT[:, 0:1], scale=1.0),
                     reads=[mvr, "epsT"], writes=[mvr])
                T.op("act", lambda e: e.activation(mvt[:, 3:4], mvt[:, 2:3], AF.Exp, scale=-0.5), reads=[mvr], writes=[mvr])

            def cB(i):
                p = i % 2
                mvt, mvr = mv2s[i % 4], f"mv2_{i % 4}"
                a_, ar = acc[p], f"acc{p}"
                y_, yr = yy[p], f"yy{p}"
                T.op("dve", lambda e: e.tensor_scalar(mvt[:, 4:5], mvt[:, 0:1], -1.0, mvt[:, 3:4], ALU.mult, ALU.mult),
                     reads=[mvr], writes=[mvr])
                T.op("act", lambda e: e.activation(y_[:, :], a_[:, :], AF.Identity, bias=mvt[:, 4:5], scale=mvt[:, 3:4]),
                     reads=[ar, mvr], writes=[yr])

            def cC(i):
                p = i % 2
                y_, yr = yy[p], f"yy{p}"
                T.op("dve", lambda e: e.tensor_tensor(y_[:, :], y_[:, :], ln2[:, 0, :], ALU.mult), reads=[yr, "ln2"], writes=[yr])
                T.op("dve", lambda e: e.tensor_tensor(y_[:, :], y_[:, :], ln2[:, 1, :], ALU.add), reads=[yr, "ln2"], writes=[yr])
                T.dma("sp", f"out{p}", lambda q: q.dma_start(out=out[i * 128:(i + 1) * 128, :], in_=y_[:, :]),
                      reads=[yr], writes=["out"], order_writes=False)

            cL(0)
            for t in range(NTB + 2):
                if t + 1 < NTB:
                    cL(t + 1)
                if t < NTB:
                    cA(t)
                if 0 <= t - 1 < NTB:
                    cB(t - 1)
                if 0 <= t - 2 < NTB:
                    cC(t - 2)
            T.barrier()
    return nc


def prep_shared(inp, CAP):
    f = lambda a: np.ascontiguousarray(np.asarray(a, dtype=np.float32))
    rep = lambda vec: np.broadcast_to(np.asarray(vec, np.float32)[None, :], (128, len(vec)))
    F = inp["w_down"].shape[2]
    KF = F // 128
    d = {}
    d["w_in"] = f(inp["w_in"][0])
    d["cw"] = f(np.transpose(np.asarray(inp["conv_w"][0]).reshape(3, 8, 128), (2, 1, 0)))
    d["w_a"] = f(inp["w_a_out"][0])
    d["w_b"] = f(inp["w_b_out"][0])
    d["lnv"] = f(np.stack([rep(inp["ln_v_g"][0]), rep(inp["ln_v_b"][0])], axis=1))
    d["wsT"] = f(np.transpose(np.asarray(inp["w_s"][0]), (2, 0, 1)))
    d["bsb"] = f(np.broadcast_to(np.asarray(inp["b_s"][0])[None], (128, 8, 128)))
    d["bg"] = f(np.asarray(inp["b_gate"][0]).reshape(32, 128).T)
    d["w_o"] = f(inp["w_o"][0])
    d["ln1"] = f(np.stack([rep(inp["ln1_g"][0]), rep(inp["ln1_b"][0])], axis=1))
    d["ln2"] = f(np.stack([rep(inp["ln2_g"][0]), rep(inp["ln2_b"][0])], axis=1))
    d["wr"] = f(np.transpose(np.asarray(inp["w_router"][0]).reshape(KD, 128, E), (1, 0, 2)))
    d["brb"] = f(rep(inp["b_router"][0]))
    d["w_up"] = f(np.asarray(inp["w_up"][0]).reshape(E * D, 2 * F))
    d["bup"] = f(np.transpose(np.asarray(inp["b_up"][0]).reshape(E, 2 * KF, 128), (2, 0, 1)))
    d["w_dn"] = f(np.asarray(inp["w_down"][0]).reshape(E * F, D))
    d["b_dn"] = f(inp["b_down"][0])
    cst = np.zeros((128, 256 + E), np.float32)
    cst[:, 0:128] = np.eye(128, dtype=np.float32)
    cst[:, 128:256] = np.triu(np.ones((128, 128), np.float32), k=1)
    cst[:, 256:] = (np.arange(E, dtype=np.float32) * CAP)[None, :]
    d["cst"] = cst
    return d


def run(inp, n_cores, CAP):
    x = np.asarray(inp["x"], dtype=np.float32)
    B, S, _ = x.shape
    xf = x.reshape(B * S, D)
    NT = (B * S) // n_cores
    F = inp["w_down"].shape[2]
    shared = prep_shared(inp, CAP)
    nc = build_nc(NT, F, CAP)
    in_maps = []
    for c in range(n_cores):
        t0 = c * NT
        m = dict(shared)
        m["xs"] = np.ascontiguousarray(xf[t0:t0 + NT])
        halo = np.zeros((2, D), np.float32)
        if t0 % S != 0:
            halo = xf[t0 - 2:t0]
        m["xhT"] = np.ascontiguousarray(np.transpose(halo.reshape(2, KD, 128), (2, 1, 0)))
        in_maps.append(m)
    res = run_bass_kernel_spmd(nc, in_maps, core_ids=list(range(n_cores)))
    outs = [np.asarray(res.results[c]["out"]) for c in range(n_cores)]
    return np.concatenate(outs, axis=0).reshape(B, S, D).astype(np.float32)


def kernel(**inputs):
    return run(inputs, 8, 640)
```

```python
from contextlib import ExitStack
import numpy as np
import concourse.bass as bass
import concourse.mybir as mybir
from concourse.bass_utils import run_bass_kernel_spmd

F32 = mybir.dt.float32
BF16 = mybir.dt.bfloat16
I32 = mybir.dt.int32
AF = mybir.ActivationFunctionType
ALU = mybir.AluOpType

D = 2048
KD = D // 128
CW = 1024
E = 32
TOPK = 4
LN_EPS = 1e-5
ALPHA = (2.0 * 1) ** 0.25
SW_ALPHA = 1.702
SW_LIM = 7.0
IN_COLS = 3 * CW + 2 * CW + 2 * D


class Tracker:
    def __init__(self, nc, stack):
        self.nc = nc
        self.stack = stack
        self.eng = {"pe": nc.tensor, "act": nc.scalar, "dve": nc.vector, "pool": nc.gpsimd, "sp": nc.sync}
        self.sem = {}
        self.cnt = {}
        for e in ("pe", "act", "dve", "pool"):
            self.sem[e] = stack.enter_context(nc.semaphore("sem_" + e))
            self.cnt[e] = 0
        self.waited = {e: {} for e in self.eng}
        self.lastw = {}
        self.readers = {}
        self.chan = {}

    def _deps(self, reads, writes, order_writes=True):
        deps = []
        for r in reads:
            deps.extend(self.lastw.get(r, []))
        for w in writes:
            if order_writes:
                deps.extend(self.lastw.get(w, []))
            deps.extend(self.readers.get(w, []))
        return deps

    def _wait(self, e, deps):
        best = {}
        for (key, sem, val) in deps:
            if key not in best or best[key][1] < val:
                best[key] = (sem, val)
        for key, (sem, val) in best.items():
            if self.waited[e].get(key, 0) < val:
                self.eng[e].wait_ge(sem, val)
                self.waited[e][key] = val

    def _record(self, tok, reads, writes, order_writes=True):
        for r in reads:
            self.readers.setdefault(r, []).append(tok)
        for w in writes:
            if order_writes:
                self.lastw[w] = [tok]
                self.readers[w] = []
            else:
                self.lastw.setdefault(w, []).append(tok)

    def op(self, e, fn, reads=(), writes=()):
        self._wait(e, self._deps(reads, writes))
        ins = fn(self.eng[e])
        self.cnt[e] += 1
        ins.then_inc(self.sem[e], 1)
        tok = (e, self.sem[e], self.cnt[e])
        self._record(tok, reads, writes)
        return tok

    def mm_group(self, mms, reads_list, writes):
        tok = ("pe", self.sem["pe"], self.cnt["pe"] + 1)
        n = len(mms)
        for i, fn in enumerate(mms):
            self._wait("pe", self._deps(reads_list[i], writes if i == 0 else ()))
            ins = fn(self.eng["pe"])
            if i == n - 1:
                ins.then_inc(self.sem["pe"], 1)
        self.cnt["pe"] += 1
        allreads = set()
        for r in reads_list:
            allreads.update(r)
        self._record(tok, list(allreads), writes)
        return tok

    def dma(self, q, chan, fn, reads=(), writes=(), order_writes=True):
        if chan not in self.chan:
            self.chan[chan] = [self.stack.enter_context(self.nc.semaphore("ds_" + chan)), 0]
        self._wait(q, self._deps(reads, writes, order_writes))
        ins = fn(self.eng[q])
        c = self.chan[chan]
        c[1] += 16
        ins.then_inc(c[0], 16)
        tok = ("d_" + chan, c[0], c[1])
        self._record(tok, reads, writes, order_writes)
        return tok

    def barrier(self):
        toks = [(e, self.sem[e], self.cnt[e]) for e in self.sem if self.cnt[e] > 0]
        toks += [("d_" + c, sv[0], sv[1]) for c, sv in self.chan.items() if sv[1] > 0]
        for e in self.eng:
            self._wait(e, toks)

    def wait_all(self, e, resources):
        deps = []
        for r in resources:
            deps.extend(self.lastw.get(r, []))
            deps.extend(self.readers.get(r, []))
        self._wait(e, deps)


def build_nc(NT, F, CAP):
    NTT = NT // 512
    NTB = NT // 128
    KF = F // 128
    NB = CAP // 128
    NSLOT = E * CAP
    colr = [(0, min(CAP, 512))] + ([(512, CAP)] if CAP > 512 else [])

    nc = bass.Bass("TRN2", target_bir_lowering=False)

    def din(name, shape, dt=F32):
        return nc.dram_tensor(name, list(shape), dt, kind="ExternalInput").ap()

    xs = din("xs", [NT, D])
    xhT = din("xhT", [128, KD, 2])
    w_in = din("w_in", [D, IN_COLS])
    cw_d = din("cw", [128, 8, 3])
    w_a = din("w_a", [CW, D])
    w_b = din("w_b", [CW, D])
    lnv_d = din("lnv", [128, 2, CW])
    wsT_d = din("wsT", [128, 8, 128])
    bsb_d = din("bsb", [128, 8, 128])
    bg_d = din("bg", [128, 32])
    w_o = din("w_o", [D, D])
    ln1_d = din("ln1", [128, 2, D])
    ln2_d = din("ln2", [128, 2, D])
    wr_d = din("wr", [128, KD, E])
    brb_d = din("brb", [128, E])
    w_up = din("w_up", [E * D, 2 * F])
    bup_d = din("bup", [128, E, 2 * KF])
    w_dn = din("w_dn", [E * F, D])
    b_dn = din("b_dn", [E, D])
    cst_d = din("cst", [128, 128 + 128 + E])
    out = nc.dram_tensor("out", [NT, D], F32, kind="ExternalOutput").ap()

    MT = nc.dram_tensor("MT", [NTT, 128, KD * 512], BF16, kind="Internal").ap()
    X1 = nc.dram_tensor("X1", [NT + 1, D + E], F32, kind="Internal").ap()
    TT = nc.dram_tensor("TT", [NSLOT + 1, 2], I32, kind="Internal").ap()
    OD = nc.dram_tensor("OD", [NSLOT + 1, D], F32, kind="Internal").ap()

    with ExitStack() as top:
        T = Tracker(nc, top)
        sb = lambda st, name, shape, dt=F32: st.enter_context(nc.sbuf_tensor("s_" + name, list(shape), dt))
        ps_ = lambda st, name, shape, dt=F32: st.enter_context(nc.psum_tensor("p_" + name, list(shape), dt))

        cst = sb(top, "cst", [128, 128 + 128 + E])
        idb = sb(top, "idb", [128, 128], BF16)
        ones = sb(top, "ones", [128, 128])
        epsT = sb(top, "epsT", [128, 1])
        slots_all = sb(top, "slots_all", [128, NTB, TOPK], I32)
        tokid = sb(top, "tokid", [128, NTB, 2], I32)
        T.dma("sp", "c_cst", lambda q: q.dma_start(out=cst[:, :], in_=cst_d[:, :]), writes=["cst"])
        T.op("act", lambda e: e.copy(idb[:, :], cst[:, 0:128]), reads=["cst"], writes=["idb"])
        T.op("pool", lambda e: e.memset(ones[:, :], 1.0), writes=["ones"])
        T.op("pool", lambda e: e.memset(epsT[:, :], LN_EPS), writes=["epsT"])
        T.op("pool", lambda e: e.iota(tokid[:, :, :], pattern=[[128, NTB], [0, 2]], base=0, channel_multiplier=1),
             writes=["tokid"])
        idf = cst[:, 0:128]
        Uf = cst[:, 128:256]
        ecap = cst[:, 256:256 + E]

        with ExitStack() as st0:
            zrow = sb(st0, "zrow", [1, D + E])
            tinit = sb(st0, "tinit", [128, NSLOT * 2 // 128], I32)
            T.op("pool", lambda e: e.memset(zrow[:, :], 0.0), writes=["zrow"])
            T.op("pool", lambda e: e.memset(tinit[:, :], NT), writes=["tinit"])
            T.dma("sp", "i_x1", lambda q: q.dma_start(out=X1[NT:NT + 1, :], in_=zrow[:, :]), reads=["zrow"], writes=["X1z"])
            T.dma("sp", "i_od", lambda q: q.dma_start(out=OD[NSLOT:NSLOT + 1, :], in_=zrow[:, 0:D]), reads=["zrow"], writes=["ODz"])
            T.dma("sp", "i_tt", lambda q: q.dma_start(out=TT[0:NSLOT, :].rearrange("(p a) b -> p (a b)", p=128), in_=tinit[:, :]),
                  reads=["tinit"], writes=["TTinit"])
            T.barrier()

        with ExitStack() as st:
            NW = 5
            wp = [sb(st, f"wpA{i}", [128, 16, 512], BF16) for i in range(NW)]
            wctr = [0]

            def wload(src2d, K):
                s = wctr[0] % NW
                wctr[0] += 1
                T.dma("pool", f"wpA{s}",
                      lambda q: q.dma_start(out=wp[s][:, 0:K, :], in_=src2d.rearrange("(k p) c -> p k c", p=128)),
                      writes=[f"wpA{s}"])
                return wp[s], f"wpA{s}"

            NPA = 5
            psb = [ps_(st, f"psA{i}", [128, 512]) for i in range(NPA)]
            psH = ps_(st, "psH", [128, 512])
            psTA = [ps_(st, f"psTA{i}", [128, 8, 128], BF16) for i in range(2)]
            tctrA = [0]
            pctr = [0]

            def nextps():
                i = pctr[0] % NPA
                pctr[0] += 1
                return psb[i], f"psA{i}"

            xbs = [sb(st, f"xb{i}", [128, D], BF16) for i in range(2)]
            xissued = set()

            def xload(blk):
                if blk in xissued or blk >= NTB:
                    return
                xissued.add(blk)
                bb = blk % 2
                T.dma("pool", f"xb{bb}", lambda q: q.dma_start(out=xbs[bb][:, :], in_=xs[blk * 128:(blk + 1) * 128, :]),
                      writes=[f"xb{bb}"])
            xT = sb(st, "xT", [128, KD, 514], BF16)
            cw = sb(st, "cw", [128, 8, 3])
            lnv = sb(st, "lnv", [128, 2, CW])
            wsT = sb(st, "wsT", [128, 8, 128], BF16)
            bsb = sb(st, "bsb", [128, 8, 128])
            bg = sb(st, "bg", [128, 32])
            pre_sb = sb(st, "pre_sb", [128, 514])
            zb = sb(st, "zb", [128, 514])
            zc = sb(st, "zc", [128, 8, 2])
            c1 = sb(st, "c1", [128, 512])
            c2 = sb(st, "c2", [128, 512])
            actA = sb(st, "actA", [128, 8, 512], BF16)
            actB = sb(st, "actB", [128, 8, 512], BF16)
            u = sb(st, "u", [128, 8, 512], BF16)
            v = sb(st, "v", [128, 4, CW], BF16)
            vg = sb(st, "vg", [128, CW])
            stt = sb(st, "stt", [128, 4, 6])
            mv = sb(st, "mv", [128, 8])
            tmpB = sb(st, "tmpB", [128, 512])
            gsb = [sb(st, f"gsb{i}", [128, 512]) for i in range(2)]
            m2 = sb(st, "m2", [128, 512])
            mtmp = sb(st, "mtmp", [128, 4, 512])
            mst = [sb(st, f"mst{i}", [128, 4, 512], BF16) for i in range(2)]

            T.dma("sp", "c_cw", lambda q: q.dma_start(out=cw[:, :, :], in_=cw_d[:, :, :]), writes=["cw"])
            T.dma("sp", "c_lnv", lambda q: q.dma_start(out=lnv[:, :, :], in_=lnv_d[:, :, :]), writes=["lnv"])
            T.dma("sp", "c_bsb", lambda q: q.dma_start(out=bsb[:, :, :], in_=bsb_d[:, :, :]), writes=["bsb"])
            T.dma("sp", "c_bg", lambda q: q.dma_start(out=bg[:, :], in_=bg_d[:, :]), writes=["bg"])
            T.dma("pool", "c_ws", lambda q: q.dma_start(out=wsT[:, :, :], in_=wsT_d[:, :, :]), writes=["wsT"])
            T.op("pool", lambda e: e.memset(wsT[64:128, :, 0:64], 0.0), reads=["wsT"], writes=["wsT"])

            mctr = [0]
            conv_pref = [None]
            for ti in range(NTT):
                if ti == 0:
                    T.dma("pool", "c_xh", lambda q: q.dma_start(out=xT[:, :, 0:2], in_=xhT[:, :, :]), writes=["xT"])
                for b in range(4):
                    blk = ti * 4 + b
                    xload(blk)
                    xb, xbr = xbs[blk % 2], f"xb{blk % 2}"
                    for half in range(2):
                        tp = tctrA[0] % 2
                        tctrA[0] += 1
                        psT, ptr_ = psTA[tp], f"psTA{tp}"
                        mms = [(lambda pe, a=a, half=half: pe.transpose(psT[:, a, :], xb[:, (half * 8 + a) * 128:(half * 8 + a + 1) * 128], idb[:, :]))
                               for a in range(8)]
                        T.mm_group(mms, [[xbr, "idb"]] * 8, [ptr_])
                        if half == 0:
                            T.op("dve", lambda e: e.tensor_copy(xT[:, 0:8, 2 + b * 128:2 + (b + 1) * 128], psT[:, :, :]), reads=[ptr_], writes=["xT"])
                        else:
                            T.op("act", lambda e: e.copy(xT[:, 8:16, 2 + b * 128:2 + (b + 1) * 128], psT[:, :, :]), reads=[ptr_], writes=["xT"])
                    if b + 2 < 4:
                        xload(blk + 2)

                def lin_fm(W, wres, cc, rhs_of_k, K, rres, pst, pres, c0=0, c1_=512):
                    mms = [(lambda pe, k=k: pe.matmul(pst[:, c0:c1_], lhsT=W[:, k, cc * 128:(cc + 1) * 128], rhs=rhs_of_k(k),
                                                      start=(k == 0), stop=(k == K - 1))) for k in range(K)]
                    T.mm_group(mms, [[wres] + rres] * K, [pres])

                for h in range(2):
                    if h == 0 and conv_pref[0] is not None:
                        (Wpre, rpre), (Whid, rhid), (Wpost, rpost) = conv_pref[0]
                        conv_pref[0] = None
                    else:
                        Wpre, rpre = wload(w_in[:, h * 512:(h + 1) * 512], KD)
                        Whid, rhid = wload(w_in[:, CW + h * 512:CW + (h + 1) * 512], KD)
                        Wpost, rpost = wload(w_in[:, 2 * CW + h * 512:2 * CW + (h + 1) * 512], KD)
                    for cc in range(4):
                        j = 4 * h + cc
                        pP, rP = nextps()
                        pH, rH = nextps()
                        pO, rO = nextps()
                        pX, rX = psH, "psH"
                        main = lambda k: xT[:, k, 2:514]
                        halo = lambda k: xT[:, k, 0:2]
                        lin_fm(Wpre, rpre, cc, main, KD, ["xT"], pP, rP)
                        if ti == 0:
                            lin_fm(Wpre, rpre, cc, halo, KD, ["xT"], pX, rX, 0, 2)
                        lin_fm(Whid, rhid, cc, main, KD, ["xT"], pH, rH)
                        if ti == 0:
                            lin_fm(Whid, rhid, cc, halo, KD, ["xT"], pX, rX, 2, 4)
                        lin_fm(Wpost, rpost, cc, main, KD, ["xT"], pO, rO)
                        T.op("act", lambda e: e.copy(pre_sb[:, 2:514], pP[:, :]), reads=[rP], writes=["pre_sb"])
                        if ti == 0:
                            T.op("act", lambda e: e.copy(pre_sb[:, 0:2], pX[:, 0:2]), reads=[rX], writes=["pre_sb"])
                            T.op("dve", lambda e: e.tensor_tensor(zb[:, 0:2], pre_sb[:, 0:2], pX[:, 2:4], ALU.mult),
                                 reads=["pre_sb", rX], writes=["zb"])
                        else:
                            T.op("dve", lambda e: e.tensor_copy(zb[:, 0:2], zc[:, j, :]), reads=["zc"], writes=["zb"])
                        T.op("dve", lambda e: e.tensor_tensor(zb[:, 2:514], pre_sb[:, 2:514], pH[:, :], ALU.mult),
                             reads=["pre_sb", rH], writes=["zb"])
                        T.op("dve", lambda e: e.tensor_copy(zc[:, j, :], zb[:, 512:514]), reads=["zb"], writes=["zc"])
                        T.op("dve", lambda e: e.tensor_scalar(c1[:, :], zb[:, 0:512], cw[:, j, 0:1], None, ALU.mult),
                             reads=["zb", "cw"], writes=["c1"])
                        T.op("dve", lambda e: e.scalar_tensor_tensor(c2[:, :], zb[:, 1:513], cw[:, j, 1:2], c1[:, :], ALU.mult, ALU.add),
                             reads=["zb", "cw", "c1"], writes=["c2"])
                        T.op("dve", lambda e: e.scalar_tensor_tensor(c1[:, :], zb[:, 2:514], cw[:, j, 2:3], c2[:, :], ALU.mult, ALU.add),
                             reads=["zb", "cw", "c2"], writes=["c1"])
                        T.op("dve", lambda e: e.tensor_tensor(actA[:, j, :], c1[:, :], pO[:, :], ALU.mult),
                             reads=["c1", rO], writes=["actA"])

                Wv = [wload(w_in[:, 4 * CW + hh * 512:4 * CW + (hh + 1) * 512], KD) for hh in range(2)]
                for b in range(4):
                    for hh in range(2):
                        pV, rV = nextps()
                        Wt, rw = Wv[hh]
                        mms = [(lambda pe, k=k: pe.matmul(pV[:, :], lhsT=xT[:, k, 2 + b * 128:2 + (b + 1) * 128], rhs=Wt[:, k, :],
                                                          start=(k == 0), stop=(k == KD - 1))) for k in range(KD)]
                        T.mm_group(mms, [["xT", rw]] * KD, [rV])
                        T.op("act", lambda e: e.activation(vg[:, hh * 512:(hh + 1) * 512], pV[:, :], AF.Gelu), reads=[rV], writes=["vg"])
                    for hh in range(2):
                        T.op("dve", lambda e, hh=hh: e.bn_stats(stt[:, hh, :], vg[:, hh * 512:(hh + 1) * 512]), reads=["vg"], writes=["stt"])
                    T.op("dve", lambda e: e.bn_aggr(mv[:, 0:2], stt[:, 0:2, :].rearrange("p a b -> p (a b)")), reads=["stt"], writes=["mv"])
                    T.op("act", lambda e: e.activation(mv[:, 2:3], mv[:, 1:2], AF.Sqrt, bias=epsT[:, 0:1], scale=1.0),
                         reads=["mv", "epsT"], writes=["mv"])
                    T.op("dve", lambda e: e.reciprocal(mv[:, 3:4], mv[:, 2:3]), reads=["mv"], writes=["mv"])
                    T.op("dve", lambda e: e.tensor_scalar(vg[:, :], vg[:, :], mv[:, 0:1], mv[:, 3:4], ALU.subtract, ALU.mult),
                         reads=["vg", "mv"], writes=["vg"])
                    T.op("dve", lambda e: e.tensor_tensor(vg[:, :], vg[:, :], lnv[:, 0, :], ALU.mult), reads=["vg", "lnv"], writes=["vg"])
                    T.op("dve", lambda e: e.tensor_tensor(v[:, b, :], vg[:, :], lnv[:, 1, :], ALU.add), reads=["vg", "lnv"], writes=["v"])

                for h in range(2):
                    Wu, ru = wload(w_in[:, 3 * CW + h * 512:3 * CW + (h + 1) * 512], KD)
                    for cc in range(4):
                        j = 4 * h + cc
                        pU, rU = nextps()
                        lin_fm(Wu, ru, cc, lambda k: xT[:, k, 2:514], KD, ["xT"], pU, rU)
                        T.op("act", lambda e: e.activation(u[:, j, :], pU[:, :], AF.Gelu), reads=[rU], writes=["u"])

                for g in range(8):
                    pS, rS = nextps()
                    mms = [(lambda pe, b=b: pe.matmul(pS[:, b * 128:(b + 1) * 128], lhsT=v[:, b, g * 128:(g + 1) * 128], rhs=wsT[:, g, :],
                                                      start=True, stop=True)) for b in range(4)]
                    T.mm_group(mms, [["v", "wsT"]] * 4, [rS])
                    for b in range(4):
                        T.op("dve", lambda e, b=b: e.tensor_tensor(tmpB[:, b * 128:(b + 1) * 128], pS[:, b * 128:(b + 1) * 128], bsb[:, g, :], ALU.add),
                             reads=[rS, "bsb"], writes=["tmpB"])
                    T.op("dve", lambda e: e.tensor_tensor(actB[:, g, :], tmpB[:, :], u[:, g, :], ALU.mult),
                         reads=["tmpB", "u"], writes=["actB"])

                xload((ti + 1) * 4)
                xload((ti + 1) * 4 + 1)
                for q in range(4):
                    ms = mst[mctr[0] % 2]
                    mres = f"mst{mctr[0] % 2}"
                    for br in range(2):
                        Wg, rg = wload(w_in[:, 5 * CW + br * D + q * 512:5 * CW + br * D + (q + 1) * 512], KD)
                        Wy, ry = wload((w_a if br == 0 else w_b)[:, q * 512:(q + 1) * 512], 8)
                        act_in, ares = (actA, "actA") if br == 0 else (actB, "actB")
                        for cc in range(4):
                            c = 4 * q + cc
                            pG, rG = nextps()
                            pY, rY = nextps()
                            lin_fm(Wg, rg, cc, lambda k: xT[:, k, 2:514], KD, ["xT"], pG, rG)
                            lin_fm(Wy, ry, cc, lambda k: act_in[:, k, :], 8, [ares], pY, rY)
                            gs = gsb[(c + br) % 2]
                            gres = f"gsb{(c + br) % 2}"
                            T.op("act", lambda e: e.activation(gs[:, :], pG[:, :], AF.Sigmoid, bias=bg[:, br * 16 + c:br * 16 + c + 1], scale=1.0),
                                 reads=[rG, "bg"], writes=[gres])
                            if br == 0:
                                T.op("dve", lambda e: e.tensor_tensor(mtmp[:, cc, :], gs[:, :], pY[:, :], ALU.mult),
                                     reads=[gres, rY], writes=["mtmp"])
                            else:
                                T.op("dve", lambda e: e.tensor_tensor(m2[:, :], gs[:, :], pY[:, :], ALU.mult),
                                     reads=[gres, rY], writes=["m2"])
                                T.op("dve", lambda e: e.tensor_tensor(ms[:, cc, :], mtmp[:, cc, :], m2[:, :], ALU.add),
                                     reads=["mtmp", "m2"], writes=[mres])
                    T.dma("sp", mres, lambda q_, q=q, ti=ti: q_.dma_start(
                        out=MT[ti, :, q * 4 * 512:(q + 1) * 4 * 512], in_=ms[:, :, :].rearrange("p a b -> p (a b)")),
                        reads=[mres], writes=["MT"], order_writes=False)
                    mctr[0] += 1
                if ti + 1 < NTT:
                    conv_pref[0] = [wload(w_in[:, 0:512], KD), wload(w_in[:, CW:CW + 512], KD), wload(w_in[:, 2 * CW:2 * CW + 512], KD)]
            T.barrier()

        with ExitStack() as st:
            wo = sb(st, "wo", [128, KD, D], BF16)
            for n in range(4):
                T.dma("pool", f"wo{n}", lambda q, n=n: q.dma_start(
                    out=wo[:, :, n * 512:(n + 1) * 512], in_=w_o[:, n * 512:(n + 1) * 512].rearrange("(k p) c -> p k c", p=128)),
                    writes=[f"wo{n}"])
            ln1 = sb(st, "ln1", [128, 2, D])
            wr = sb(st, "wr", [128, KD, E])
            brb = sb(st, "brb", [128, E])
            T.dma("sp", "c_ln1", lambda q: q.dma_start(out=ln1[:, :, :], in_=ln1_d[:, :, :]), writes=["ln1"])
            T.dma("sp", "c_wr", lambda q: q.dma_start(out=wr[:, :, :], in_=wr_d[:, :, :]), writes=["wr"])
            T.dma("sp", "c_brb", lambda q: q.dma_start(out=brb[:, :], in_=brb_d[:, :]), writes=["brb"])
            mt = [sb(st, f"mt{i}", [128, KD, 512], BF16) for i in range(2)]
            xin2 = [sb(st, f"xin2_{i}", [128, D]) for i in range(3)]
            r = [sb(st, f"r{i}", [128, D]) for i in range(2)]
            xrow = [sb(st, f"xrow{i}", [128, D + E]) for i in range(2)]
            x1T = sb(st, "x1T", [128, KD, 128])
            st1 = sb(st, "st1", [128, 4, 6])
            mv1 = sb(st, "mv1", [128, 8])
            lg = sb(st, "lg", [128, E])
            top8 = sb(st, "top8", [128, 8])
            mask = sb(st, "mask", [128, E])
            ex = sb(st, "ex", [128, E])
            sm = sb(st, "sm", [128, 4])
            run = sb(st, "run", [128, E])
            aa = sb(st, "aa", [128, E])
            ok = sb(st, "ok", [128, E])
            svn = sb(st, "svn", [128, E])
            top8s = sb(st, "top8s", [128, 8])
            psm = [ps_(st, f"psM{i}", [128, 512]) for i in range(4)]
            pst = [ps_(st, f"psX{i}", [128, 4, 128]) for i in range(2)]
            psl = ps_(st, "psL", [128, 512])
            psc = ps_(st, "psC", [128, 512])
            T.op("pool", lambda e: e.memset(run[:, :], 0.0), writes=["run"])
            BIG = float(NSLOT)

            negcap = sb(st, "negcap", [128, E])
            T.op("dve", lambda e: e.tensor_scalar(negcap[:, :], ecap, -1.0, BIG, ALU.mult, ALU.add), reads=["cst"], writes=["negcap"])

            def load_mt(ti):
                mtt, mtr = mt[ti % 2], f"mt{ti % 2}"
                T.dma("sp", mtr, lambda q: q.dma_start(out=mtt[:, :, :].rearrange("p a b -> p (a b)"), in_=MT[ti, :, :]),
                      reads=["MT"], writes=[mtr])

            def loads(blk):
                par = blk % 3
                xi, xir = xin2[par], f"xin2_{par}"
                T.dma("sp", xir, lambda q: q.dma_start(out=xi[:, :], in_=xs[blk * 128:(blk + 1) * 128, :]), writes=[xir])

            def s1a(blk):
                ti, b = blk // 4, blk % 4
                mtt, mtr = mt[ti % 2], f"mt{ti % 2}"
                par = blk % 2
                xi, xir = xin2[blk % 3], f"xin2_{blk % 3}"
                rr, rres = r[par], f"r{par}"
                for n in range(4):
                    mms = [(lambda pe, k=k: pe.matmul(psm[n][:, :], lhsT=mtt[:, k, b * 128:(b + 1) * 128], rhs=wo[:, k, n * 512:(n + 1) * 512],
                                                      start=(k == 0), stop=(k == KD - 1))) for k in range(KD)]
                    T.mm_group(mms, [[mtr, f"wo{n}"]] * KD, [f"psM{n}"])
                    T.op("dve", lambda e, n=n: e.scalar_tensor_tensor(rr[:, n * 512:(n + 1) * 512], xi[:, n * 512:(n + 1) * 512], ALPHA,
                                                                     psm[n][:, :], ALU.mult, ALU.add),
                         reads=[xir, f"psM{n}"], writes=[rres])

            def s1b_stats(blk):
                par = blk % 2
                rr, rres = r[par], f"r{par}"
                for n in range(4):
                    T.op("dve", lambda e, n=n: e.bn_stats(st1[:, n, :], rr[:, n * 512:(n + 1) * 512]), reads=[rres], writes=["st1"])
                T.op("dve", lambda e: e.bn_aggr(mv1[:, 0:2], st1[:, :, :].rearrange("p a b -> p (a b)")), reads=["st1"], writes=["mv1"])
                T.op("act", lambda e: e.activation(mv1[:, 2:3], mv1[:, 1:2], AF.Ln, bias=epsT[:, 0:1], scale=1.0),
                     reads=["mv1", "epsT"], writes=["mv1"])
                T.op("act", lambda e: e.activation(mv1[:, 3:4], mv1[:, 2:3], AF.Exp, scale=-0.5), reads=["mv1"], writes=["mv1"])

            def s1b_norm(blk):
                par = blk % 2
                rr, rres = r[par], f"r{par}"
                xr, xres = xrow[par], f"xrow{par}"
                T.op("dve", lambda e: e.tensor_scalar(mv1[:, 4:5], mv1[:, 0:1], -1.0, mv1[:, 3:4], ALU.mult, ALU.mult),
                     reads=["mv1"], writes=["mv1"])
                T.op("act", lambda e: e.activation(rr[:, :], rr[:, :], AF.Identity, bias=mv1[:, 4:5], scale=mv1[:, 3:4]),
                     reads=[rres, "mv1"], writes=[rres])
                T.op("dve", lambda e: e.tensor_tensor(rr[:, :], rr[:, :], ln1[:, 0, :], ALU.mult), reads=[rres, "ln1"], writes=[rres])
                T.op("dve", lambda e: e.tensor_tensor(xr[:, 0:D], rr[:, :], ln1[:, 1, :], ALU.add), reads=[rres, "ln1"], writes=[xres])

            def s2(blk):
                par = blk % 2
                xr, xres = xrow[par], f"xrow{par}"
                for qd in range(4):
                    pt, ptr = pst[qd % 2], f"psX{qd % 2}"
                    mms = [(lambda pe, a=a: pe.transpose(pt[:, a, :], xr[:, (qd * 4 + a) * 128:(qd * 4 + a + 1) * 128], idf))
                           for a in range(4)]
                    T.mm_group(mms, [[xres, "cst"]] * 4, [ptr])
                    T.op("act", lambda e: e.copy(x1T[:, qd * 4:(qd + 1) * 4, :], pt[:, :, :]), reads=[ptr], writes=["x1T"])
                mms = [(lambda pe, k=k: pe.matmul(psl[:, 0:E], lhsT=x1T[:, k, :], rhs=wr[:, k, :], start=(k == 0), stop=(k == KD - 1)))
                       for k in range(KD)]
                T.mm_group(mms, [["x1T", "wr"]] * KD, ["psL"])

            def s3a(blk):
                T.op("dve", lambda e: e.tensor_tensor(lg[:, :], psl[:, 0:E], brb[:, :], ALU.add), reads=["psL", "brb"], writes=["lg"])
                T.op("dve", lambda e: e.max(out=top8[:, :], in_=lg[:, :]), reads=["lg"], writes=["top8"])
                T.op("dve", lambda e: e.tensor_scalar(mask[:, :], lg[:, :], top8[:, 3:4], None, ALU.is_ge), reads=["lg", "top8"], writes=["mask"])
                T.op("dve", lambda e: e.tensor_scalar(sm[:, 0:1], top8[:, 0:1], -1.0, None, ALU.mult), reads=["top8"], writes=["sm"])
                T.op("act", lambda e: e.activation(ex[:, :], lg[:, :], AF.Exp, bias=sm[:, 0:1], scale=1.0), reads=["lg", "sm"], writes=["ex"])
                mms = [lambda pe: pe.matmul(psc[:, 0:E], lhsT=Uf, rhs=mask[:, :], start=True, stop=False),
                       lambda pe: pe.matmul(psc[:, 0:E], lhsT=ones[:, :], rhs=run[:, :], start=False, stop=True)]
                T.mm_group(mms, [["cst", "mask"], ["ones", "run"]], ["psC"])

            def s3b(blk):
                par = blk % 2
                xr, xres = xrow[par], f"xrow{par}"
                T.op("dve", lambda e: e.scalar_tensor_tensor(aa[:, :], psc[:, 0:E], -1.0, negcap[:, :], ALU.mult, ALU.add),
                     reads=["psC", "negcap"], writes=["aa"])
                T.op("dve", lambda e: e.scalar_tensor_tensor(ok[:, :], psc[:, 0:E], float(CAP), mask[:, :], ALU.is_lt, ALU.mult),
                     reads=["psC", "mask"], writes=["ok"])
                T.op("dve", lambda e: e.tensor_tensor(run[:, :], run[:, :], mask[:, :], ALU.add), reads=["run", "mask"], writes=["run"])
                T.op("dve", lambda e: e.tensor_tensor(svn[:, :], aa[:, :], ok[:, :], ALU.mult), reads=["aa", "ok"], writes=["svn"])
                T.op("dve", lambda e: e.max(out=top8s[:, :], in_=svn[:, :]), reads=["svn"], writes=["top8s"])
                T.op("dve", lambda e: e.tensor_scalar(slots_all[:, blk, :], top8s[:, 0:TOPK], -1.0, BIG, ALU.mult, ALU.add),
                     reads=["top8s"], writes=[f"slots{blk % 2}"])
                for k in range(TOPK):
                    T.dma("pool", "scat", lambda q, k=k: q.indirect_dma_start(
                        out=TT[:, :], out_offset=bass.IndirectOffsetOnAxis(ap=slots_all[:, blk, k:k + 1], axis=0),
                        in_=tokid[:, blk, :], in_offset=None),
                        reads=[f"slots{blk % 2}", "tokid", "TTinit"], writes=["TT"], order_writes=False)
                T.op("dve", lambda e: e.tensor_tensor(ex[:, :], ex[:, :], mask[:, :], ALU.mult), reads=["ex", "mask"], writes=["ex"])
                T.op("dve", lambda e: e.reduce_sum(sm[:, 1:2], ex[:, :], axis=mybir.AxisListType.X), reads=["ex"], writes=["sm"])
                T.op("dve", lambda e: e.reciprocal(sm[:, 2:3], sm[:, 1:2]), reads=["sm"], writes=["sm"])
                T.op("dve", lambda e: e.tensor_scalar(xr[:, D:D + E], ex[:, :], sm[:, 2:3], None, ALU.mult), reads=["ex", "sm"], writes=[xres])
                T.dma("sp", f"x1s{par}", lambda q: q.dma_start(out=X1[blk * 128:(blk + 1) * 128, :], in_=xr[:, :]),
                      reads=[xres], writes=["X1"], order_writes=False)

            load_mt(0)
            loads(0)
            if NTB > 1:
                loads(1)
            for blk in range(NTB):
                if blk + 2 < NTB:
                    loads(blk + 2)
                if blk % 4 == 1 and blk // 4 + 1 < NTT:
                    load_mt(blk // 4 + 1)
                s1a(blk)
                if blk > 0:
                    s2(blk - 1)
                s1b_stats(blk)
                if blk > 0:
                    s3a(blk - 1)
                s1b_norm(blk)
                if blk > 0:
                    s3b(blk - 1)
            s2(NTB - 1)
            s3a(NTB - 1)
            s3b(NTB - 1)
            T.barrier()

        with ExitStack() as st:
            NW = 5
            wp = [sb(st, f"wpB{i}", [128, 16, 512], BF16) for i in range(NW)]
            wctr = [0]

            def wloadB(src2d, K):
                s = wctr[0] % NW
                wctr[0] += 1
                T.dma("pool", f"wpB{s}",
                      lambda q: q.dma_start(out=wp[s][:, 0:K, :], in_=src2d.rearrange("(k p) c -> p k c", p=128)),
                      writes=[f"wpB{s}"])
                return wp[s], f"wpB{s}"

            psb = [ps_(st, f"psB{i}", [128, 512]) for i in range(6)]
            psTs = [ps_(st, f"psTB{i}", [128, 8, 128], BF16) for i in range(2)]
            pctr = [0]
            tctr = [0]

            def nextpsB():
                i = pctr[0] % 6
                pctr[0] += 1
                return psb[i], f"psB{i}"

            bup = sb(st, "bup", [128, E, 2 * KF])
            T.dma("sp", "c_bup", lambda q: q.dma_start(out=bup[:, :, :], in_=bup_d[:, :, :]), writes=["bup"])
            idx = [sb(st, f"idx{i}", [128, NB, 2], I32) for i in range(2)]
            xg = [sb(st, f"xg{i}", [128, D + E]) for i in range(2)]
            xgbs = [sb(st, f"xgb{i}", [128, D], BF16) for i in range(NB)]
            xgT = sb(st, "xgT", [128, KD, CAP], BF16)
            gcol = [sb(st, f"gcol{i}", [128, NB]) for i in range(2)]
            hT = sb(st, "hT", [128, KF, CAP], BF16)
            gl = sb(st, "gl", [128, CAP])
            sg = sb(st, "sg", [128, CAP])
            ln_ = sb(st, "ln_", [128, CAP])
            bdn = [sb(st, f"bdn{i}", [128, D]) for i in range(2)]
            osb = [sb(st, f"osb{i}", [128, 512]) for i in range(2)]
            osc = [sb(st, f"osc{i}", [128, 512]) for i in range(2)]
            octr = [0]
            gctr = [0]

            def prep_head(e_):
                ep = e_ % 2
                ix, ixr = idx[ep], f"idx{ep}"
                bd, bdr = bdn[ep], f"bdn{ep}"
                T.dma("sp", ixr, lambda q: q.dma_start(out=ix[:, :, :], in_=TT[e_ * CAP:(e_ + 1) * CAP, :].rearrange("(j p) b -> p j b", p=128)),
                      reads=["TT", "TTinit"], writes=[ixr])
                T.dma("sp", bdr, lambda q: q.dma_start(out=bd[:, :], in_=b_dn[e_:e_ + 1, :].to_broadcast([128, D])), writes=[bdr])

            def prep_gather(e_, j):
                ep = e_ % 2
                ix, ixr = idx[ep], f"idx{ep}"
                gc, gcr = gcol[ep], f"gcol{ep}"
                gp_ = gctr[0] % 2
                gctr[0] += 1
                xgt, xgr = xg[gp_], f"xg{gp_}"
                xgb, xbr = xgbs[j], f"xgb{j}"
                T.dma("pool", xgr, lambda q: q.indirect_dma_start(
                    out=xgt[:, :], out_offset=None, in_=X1[:, :],
                    in_offset=bass.IndirectOffsetOnAxis(ap=ix[:, j, 0:1], axis=0)),
                    reads=[ixr, "X1", "X1z"], writes=[xgr])
                T.op("act", lambda e: e.copy(xgb[:, :], xgt[:, 0:D]), reads=[xgr], writes=[xbr])
                T.op("dve", lambda e: e.tensor_copy(gc[:, j:j + 1], xgt[:, D + e_:D + e_ + 1]), reads=[xgr], writes=[gcr])

            def prep_transpose(e_, j):
                xgb, xbr = xgbs[j], f"xgb{j}"
                for half in range(2):
                    tp = tctr[0] % 2
                    tctr[0] += 1
                    psT, ptr_ = psTs[tp], f"psTB{tp}"
                    mms = [(lambda pe, a=a: pe.transpose(psT[:, a, :], xgb[:, (half * 8 + a) * 128:(half * 8 + a + 1) * 128], idb[:, :]))
                           for a in range(8)]
                    T.mm_group(mms, [[xbr, "idb"]] * 8, [ptr_])
                    if half == 0:
                        T.op("dve", lambda e: e.tensor_copy(xgT[:, 0:8, j * 128:(j + 1) * 128], psT[:, :, :]), reads=[ptr_], writes=["xgT"])
                    else:
                        T.op("act", lambda e: e.copy(xgT[:, 8:16, j * 128:(j + 1) * 128], psT[:, :, :]), reads=[ptr_], writes=["xgT"])

            def up(e_, hook=None):
                for g in range(F // 512):
                    Wg_, rg_ = wloadB(w_up[e_ * D:(e_ + 1) * D, g * 512:(g + 1) * 512], KD)
                    Wl_, rl_ = wloadB(w_up[e_ * D:(e_ + 1) * D, F + g * 512:F + (g + 1) * 512], KD)
                    for cc in range(4):
                        c = 4 * g + cc
                        pA, rA = nextpsB()
                        pB, rB = nextpsB()
                        pTl, rTl = (nextpsB() if len(colr) > 1 else (None, None))
                        for part, (W_, wr_, pm) in enumerate(((Wg_, rg_, pA), (Wl_, rl_, pB))):
                            for ci, (a0, a1) in enumerate(colr):
                                if ci == 0:
                                    dst, dres = pm[:, 0:a1 - a0], (rA if part == 0 else rB)
                                else:
                                    w_ = a1 - a0
                                    dst, dres = pTl[:, part * w_:(part + 1) * w_], rTl
                                mms = [(lambda pe, k=k, dst=dst, W_=W_, a0=a0, a1=a1: pe.matmul(
                                    dst, lhsT=W_[:, k, cc * 128:(cc + 1) * 128], rhs=xgT[:, k, a0:a1],
                                    start=(k == 0), stop=(k == KD - 1))) for k in range(KD)]
                                T.mm_group(mms, [[wr_, "xgT"]] * KD, [dres])
                        for ci, (a0, a1) in enumerate(colr):
                            w_ = a1 - a0
                            srcg = pA[:, 0:w_] if ci == 0 else pTl[:, 0:w_]
                            rgs = rA if ci == 0 else rTl
                            T.op("dve", lambda e, srcg=srcg, a0=a0, a1=a1: e.tensor_scalar(
                                gl[:, a0:a1], srcg, bup[:, e_, c:c + 1], SW_LIM, ALU.add, ALU.min),
                                reads=[rgs, "bup"], writes=["gl"])
                        T.op("act", lambda e: e.activation(sg[:, :], gl[:, :], AF.Silu, scale=SW_ALPHA), reads=["gl"], writes=["sg"])
                        for ci, (a0, a1) in enumerate(colr):
                            w_ = a1 - a0
                            srcl = pB[:, 0:w_] if ci == 0 else pTl[:, w_:2 * w_]
                            rls = rB if ci == 0 else rTl
                            T.op("dve", lambda e, srcl=srcl, a0=a0, a1=a1: e.tensor_scalar(
                                ln_[:, a0:a1], srcl, bup[:, e_, KF + c:KF + c + 1], -SW_LIM, ALU.add, ALU.max),
                                reads=[rls, "bup"], writes=["ln_"])
                        T.op("dve", lambda e: e.tensor_scalar(ln_[:, :], ln_[:, :], SW_LIM, 1.0, ALU.min, ALU.add), reads=["ln_"], writes=["ln_"])
                        T.op("dve", lambda e: e.scalar_tensor_tensor(hT[:, c, :], sg[:, :], 1.0 / SW_ALPHA, ln_[:, :], ALU.mult, ALU.mult),
                             reads=["sg", "ln_"], writes=["hT"])
                        if hook is not None:
                            hook(c)

            def down(e_, hook=None):
                ep = e_ % 2
                gc, gcr = gcol[ep], f"gcol{ep}"
                bd, bdr = bdn[ep], f"bdn{ep}"
                for n in range(D // 512):
                    Wd_, rd_ = wloadB(w_dn[e_ * F:(e_ + 1) * F, n * 512:(n + 1) * 512], KF)
                    for sbk in range(NB):
                        pO, rO = nextpsB()
                        mms = [(lambda pe, k=k: pe.matmul(pO[:, :], lhsT=hT[:, k, sbk * 128:(sbk + 1) * 128], rhs=Wd_[:, k, :],
                                                          start=(k == 0), stop=(k == KF - 1))) for k in range(KF)]
                        T.mm_group(mms, [["hT", rd_]] * KF, [rO])
                        op_ = octr[0] % 2
                        octr[0] += 1
                        o1, o1r = osb[op_], f"osb{op_}"
                        o2, o2r = osc[op_], f"osc{op_}"
                        T.op("dve", lambda e: e.tensor_tensor(o1[:, :], pO[:, :], bd[:, n * 512:(n + 1) * 512], ALU.add),
                             reads=[rO, bdr], writes=[o1r])
                        T.op("act", lambda e: e.mul(o2[:, :], o1[:, :], gc[:, sbk:sbk + 1]), reads=[o1r, gcr], writes=[o2r])
                        T.dma("sp", o2r, lambda q: q.dma_start(
                            out=OD[e_ * CAP + sbk * 128:e_ * CAP + (sbk + 1) * 128, n * 512:(n + 1) * 512], in_=o2[:, :]),
                            reads=[o2r], writes=["OD"], order_writes=False)
                    if hook is not None:
                        hook(n)

            NCH = KF
            gsched = {}
            for j in range(NB):
                gsched.setdefault(min(NCH - 1, (j * NCH) // NB), []).append(j)
            NDG = D // 512
            tsched = {}
            for j in range(NB):
                tsched.setdefault(min(NDG - 1, (j * NDG) // NB), []).append(j)
            prep_head(0)
            for j in range(NB):
                prep_gather(0, j)
            for j in range(NB):
                prep_transpose(0, j)
            for e_ in range(E):
                nxt = e_ + 1
                if nxt < E:
                    prep_head(nxt)
                    up(e_, hook=lambda c: [prep_gather(nxt, j) for j in gsched.get(c, [])])
                    down(e_, hook=lambda n: [prep_transpose(nxt, j) for j in tsched.get(n, [])])
                else:
                    up(e_)
                    down(e_)
            T.barrier()

        with ExitStack() as st:
            ln2 = sb(st, "ln2", [128, 2, D])
            T.dma("sp", "c_ln2", lambda q: q.dma_start(out=ln2[:, :, :], in_=ln2_d[:, :, :]), writes=["ln2"])
            og = [[sb(st, f"og{p}_{k}", [128, D]) for k in range(TOPK)] for p in range(2)]
            xr2 = [sb(st, f"xr2_{p}", [128, D]) for p in range(2)]
            acc = [sb(st, f"acc{p}", [128, D]) for p in range(2)]
            psC = [[ps_(st, f"psCC{p}_{n}", [128, 512]) for n in range(4)] for p in range(2)]
            yy = [sb(st, f"yy{p}", [128, D]) for p in range(2)]
            st2 = sb(st, "st2", [128, 4, 6])
            mv2s = [sb(st, f"mv2_{i}", [128, 8]) for i in range(4)]

            def cL(i):
                p = i % 2
                T.dma("sp", f"xr2_{p}", lambda q: q.dma_start(out=xr2[p][:, :], in_=X1[i * 128:(i + 1) * 128, 0:D]),
                      reads=["X1"], writes=[f"xr2_{p}"])
                for k in range(TOPK):
                    T.dma("pool", f"og{p}_{k}", lambda q, k=k: q.indirect_dma_start(
                        out=og[p][k][:, :], out_offset=None, in_=OD[:, :],
                        in_offset=bass.IndirectOffsetOnAxis(ap=slots_all[:, i, k:k + 1], axis=0)),
                        reads=["slots0", "slots1", "OD", "ODz"], writes=[f"og{p}_{k}"])

            def cA(i):
                p = i % 2
                mvt, mvr = mv2s[i % 4], f"mv2_{i % 4}"
                a_, ar = acc[p], f"acc{p}"
                for n in range(4):
                    mms = [(lambda pe, k=k, n=n: pe.matmul(psC[p][n][:, :], lhsT=idf, rhs=og[p][k][:, n * 512:(n + 1) * 512],
                                                           start=(k == 1), stop=(k == TOPK - 1))) for k in range(1, TOPK)]
                    T.mm_group(mms, [["cst", f"og{p}_{k}"] for k in range(1, TOPK)], [f"psCC{p}_{n}"])
                T.op("dve", lambda e: e.scalar_tensor_tensor(a_[:, :], xr2[p][:, :], ALPHA, og[p][0][:, :], ALU.mult, ALU.add),
                     reads=[f"xr2_{p}", f"og{p}_0"], writes=[ar])
                for n in range(4):
                    T.op("dve", lambda e, n=n: e.tensor_tensor(a_[:, n * 512:(n + 1) * 512], a_[:, n * 512:(n + 1) * 512], psC[p][n][:, :], ALU.add),
                         reads=[ar, f"psCC{p}_{n}"], writes=[ar])
                for n in range(4):
                    T.op("dve", lambda e, n=n: e.bn_stats(st2[:, n, :], a_[:, n * 512:(n + 1) * 512]), reads=[ar], writes=["st2"])
                T.op("dve", lambda e: e.bn_aggr(mvt[:, 0:2], st2[:, :, :].rearrange("p a b -> p (a b)")), reads=["st2"], writes=[mvr])
                T.op("act", lambda e: e.activation(mvt[:, 2:3], mvt[:, 1:2], AF.Ln, bias=epsT[:, 0:1], scale=1.0),
                     reads=[mvr, "epsT"], writes=[mvr])
                T.op("act", lambda e: e.activation(mvt[:, 3:4], mvt[:, 2:3], AF.Exp, scale=-0.5), reads=[mvr], writes=[mvr])

            def cB(i):
                p = i % 2
                mvt, mvr = mv2s[i % 4], f"mv2_{i % 4}"
                a_, ar = acc[p], f"acc{p}"
                y_, yr = yy[p], f"yy{p}"
                T.op("dve", lambda e: e.tensor_scalar(mvt[:, 4:5], mvt[:, 0:1], -1.0, mvt[:, 3:4], ALU.mult, ALU.mult),
                     reads=[mvr], writes=[mvr])
                T.op("act", lambda e: e.activation(y_[:, :], a_[:, :], AF.Identity, bias=mvt[:, 4:5], scale=mvt[:, 3:4]),
                     reads=[ar, mvr], writes=[yr])

            def cC(i):
                p = i % 2
                y_, yr = yy[p], f"yy{p}"
                T.op("dve", lambda e: e.tensor_tensor(y_[:, :], y_[:, :], ln2[:, 0, :], ALU.mult), reads=[yr, "ln2"], writes=[yr])
                T.op("dve", lambda e: e.tensor_tensor(y_[:, :], y_[:, :], ln2[:, 1, :], ALU.add), reads=[yr, "ln2"], writes=[yr])
                T.dma("sp", f"out{p}", lambda q: q.dma_start(out=out[i * 128:(i + 1) * 128, :], in_=y_[:, :]),
                      reads=[yr], writes=["out"], order_writes=False)

            cL(0)
            for t in range(NTB + 2):
                if t + 1 < NTB:
                    cL(t + 1)
                if t < NTB:
                    cA(t)
                if 0 <= t - 1 < NTB:
                    cB(t - 1)
                if 0 <= t - 2 < NTB:
                    cC(t - 2)
            T.barrier()
    return nc


def prep_shared(inp, CAP):
    f = lambda a: np.ascontiguousarray(np.asarray(a, dtype=np.float32))
    rep = lambda vec: np.broadcast_to(np.asarray(vec, np.float32)[None, :], (128, len(vec)))
    F = inp["w_down"].shape[2]
    KF = F // 128
    d = {}
    d["w_in"] = f(inp["w_in"][0])
    d["cw"] = f(np.transpose(np.asarray(inp["conv_w"][0]).reshape(3, 8, 128), (2, 1, 0)))
    d["w_a"] = f(inp["w_a_out"][0])
    d["w_b"] = f(inp["w_b_out"][0])
    d["lnv"] = f(np.stack([rep(inp["ln_v_g"][0]), rep(inp["ln_v_b"][0])], axis=1))
    d["wsT"] = f(np.transpose(np.asarray(inp["w_s"][0]), (2, 0, 1)))
    d["bsb"] = f(np.broadcast_to(np.asarray(inp["b_s"][0])[None], (128, 8, 128)))
    d["bg"] = f(np.asarray(inp["b_gate"][0]).reshape(32, 128).T)
    d["w_o"] = f(inp["w_o"][0])
    d["ln1"] = f(np.stack([rep(inp["ln1_g"][0]), rep(inp["ln1_b"][0])], axis=1))
    d["ln2"] = f(np.stack([rep(inp["ln2_g"][0]), rep(inp["ln2_b"][0])], axis=1))
    d["wr"] = f(np.transpose(np.asarray(inp["w_router"][0]).reshape(KD, 128, E), (1, 0, 2)))
    d["brb"] = f(rep(inp["b_router"][0]))
    d["w_up"] = f(np.asarray(inp["w_up"][0]).reshape(E * D, 2 * F))
    d["bup"] = f(np.transpose(np.asarray(inp["b_up"][0]).reshape(E, 2 * KF, 128), (2, 0, 1)))
    d["w_dn"] = f(np.asarray(inp["w_down"][0]).reshape(E * F, D))
    d["b_dn"] = f(inp["b_down"][0])
    cst = np.zeros((128, 256 + E), np.float32)
    cst[:, 0:128] = np.eye(128, dtype=np.float32)
    cst[:, 128:256] = np.triu(np.ones((128, 128), np.float32), k=1)
    cst[:, 256:] = (np.arange(E, dtype=np.float32) * CAP)[None, :]
    d["cst"] = cst
    return d


def run(inp, n_cores, CAP):
    x = np.asarray(inp["x"], dtype=np.float32)
    B, S, _ = x.shape
    xf = x.reshape(B * S, D)
    NT = (B * S) // n_cores
    F = inp["w_down"].shape[2]
    shared = prep_shared(inp, CAP)
    nc = build_nc(NT, F, CAP)
    in_maps = []
    for c in range(n_cores):
        t0 = c * NT
        m = dict(shared)
        m["xs"] = np.ascontiguousarray(xf[t0:t0 + NT])
        halo = np.zeros((2, D), np.float32)
        if t0 % S != 0:
            halo = xf[t0 - 2:t0]
        m["xhT"] = np.ascontiguousarray(np.transpose(halo.reshape(2, KD, 128), (2, 1, 0)))
        in_maps.append(m)
    res = run_bass_kernel_spmd(nc, in_maps, core_ids=list(range(n_cores)))
    outs = [np.asarray(res.results[c]["out"]) for c in range(n_cores)]
    return np.concatenate(outs, axis=0).reshape(B, S, D).astype(np.float32)


def kernel(**inputs):
    return run(inputs, 8, 640)
```
